# Optimizing a Trainium2 kernel written in Bass

```python
import math
import jax
import jax.numpy as jnp
from jax import lax
import numpy as np

D_MODEL = 4096
BATCH = 4
SEQ = 4096
DEPTH = 1

GRID_W = 64
CTX_LEN = 256
NORM_EPS = 1e-6
N_MOD = 6

D_MIX = D_MODEL
DA_HEAD_DIM = 128
DA_WIDTH = D_MIX // 2
DA_HEADS = DA_WIDTH // (2 * DA_HEAD_DIM)
ROPE_THETA = 10000.0
Q_BLOCK = 128
RW_HEAD = 64
RW_WIDTH = D_MIX - DA_WIDTH
RW_HEADS = RW_WIDTH // RW_HEAD
LORA_W = 128
LORA_A = 128
LORA_G = 480
GN_EPS = 64e-5
RW_COLS = 3 * RW_WIDTH + LORA_W + LORA_A + LORA_G
RW_SPLITS = (RW_WIDTH, 2 * RW_WIDTH, 3 * RW_WIDTH, 3 * RW_WIDTH + LORA_W, 3 * RW_WIDTH + LORA_W + LORA_A)
IN_COLS = 3 * DA_WIDTH + RW_COLS
N_EXPERTS = 64
TOP_K = 6
N_GROUPS = 8
TOPK_GROUPS = 4
EXPERT_FF = 512
SHARED_FF = 512
ROUTED_SCALE = 2.5
EXPERT_BLOCK = 128

kernel_name = "hybrid_diffattn_rwkv7_moe_dit_layer"


def rms_norm(x, g):
    xf = x.astype(jnp.float32)
    y = xf * lax.rsqrt(jnp.mean(xf * xf, axis=-1, keepdims=True) + NORM_EPS)
    return (y * g).astype(x.dtype)


def modulation(cv, w_mod, b_mod, n_chunks):
    n = n_chunks * D_MODEL
    m = jax.nn.silu(cv)[..., None, :] @ w_mod[:, :n] + b_mod[:n]
    return jnp.split(m, n_chunks, axis=-1)


def modulate(h, shift, scale):
    return h * (1.0 + scale) + shift


def axial_rope_tables(rows):
    row = jnp.repeat(jnp.arange(rows, dtype=jnp.float32), GRID_W)
    col = jnp.tile(jnp.arange(GRID_W, dtype=jnp.float32), rows)
    n_pairs = DA_HEAD_DIM // 4
    inv = jnp.power(jnp.float32(ROPE_THETA), -jnp.arange(n_pairs, dtype=jnp.float32) / n_pairs)
    ang = jnp.concatenate([row[:, None] * inv, col[:, None] * inv], axis=-1)
    return jnp.cos(ang), jnp.sin(ang)


def apply_rope(x, cos, sin):
    xp = x.reshape(*x.shape[:-1], -1, 2)
    x0, x1 = xp[..., 0], xp[..., 1]
    cb, sb = cos[None, :, None, None, :], sin[None, :, None, None, :]
    out = jnp.stack([x0 * cb - x1 * sb, x0 * sb + x1 * cb], axis=-1)
    return out.reshape(x.shape).astype(x.dtype)


def da_heads(p):
    q, k, v = jnp.split(p, 3, axis=-1)
    B, L = p.shape[:2]
    q = q.reshape(B, L, DA_HEADS, 2, DA_HEAD_DIM)
    k = k.reshape(B, L, DA_HEADS, 2, DA_HEAD_DIM)
    v = v.reshape(B, L, DA_HEADS, 2 * DA_HEAD_DIM)
    return q, k, v


def lambda_full(lam_vecs, lam_init):
    lv = lam_vecs.astype(jnp.float32)
    return jnp.exp(jnp.sum(lv[0] * lv[1])) - jnp.exp(jnp.sum(lv[2] * lv[3])) + lam_init


def diff_attend(q, k, v, lam):
    s = jnp.einsum('bqhmd,bkhmd->bhmqk', q, k).astype(jnp.float32) * (DA_HEAD_DIM ** -0.5)
    p = jax.nn.softmax(s, axis=-1)
    a = p[:, :, 0] - lam * p[:, :, 1]
    return jnp.einsum('bhqk,bkhe->bqhe', a.astype(v.dtype), v)


def block_sweep_attention(q, k, v, lam):
    B, S = q.shape[:2]
    nblk = S // Q_BLOCK
    qb = jnp.moveaxis(q.reshape(B, nblk, Q_BLOCK, *q.shape[2:]), 1, 0)
    ob = lax.map(lambda qq: diff_attend(qq, k, v, lam), qb)
    return jnp.moveaxis(ob, 0, 1).reshape(B, S, DA_HEADS, 2 * DA_HEAD_DIM)


def da_output(o, subln, lam_init):
    B, L = o.shape[:2]
    return (rms_norm(o, subln) * (1.0 - lam_init)).reshape(B, L, DA_WIDTH)


def centred_shift(p, mu_prev, mu_next):
    prev = jnp.pad(p[:, :-1], ((0, 0), (1, 0), (0, 0)))
    nxt = jnp.pad(p[:, 1:], ((0, 0), (0, 1), (0, 0)))
    return p + mu_prev * (prev - p) + mu_next * (nxt - p)


def rwkv_features(p_rw, k_k, k_a, w0, w2, a0, a2):
    r, k, v, xw, xa, xg = jnp.split(p_rw, RW_SPLITS, axis=-1)
    B, L = r.shape[:2]
    heads = lambda t: t.reshape(B, L, RW_HEADS, RW_HEAD)
    kk = heads(k * k_k).astype(jnp.float32)
    kk = kk / jnp.maximum(jnp.sqrt(jnp.sum(kk * kk, axis=-1, keepdims=True)), 1e-12)
    per_dir = []
    for d in range(2):
        wlog = -jax.nn.softplus(-(w0[d] + jnp.tanh(xw) @ w2[d])) - 0.5
        decay = jnp.exp(-jnp.exp(wlog.astype(jnp.float32)))
        a = jax.nn.sigmoid(a0[d] + xa @ a2[d])
        kd = k * (1.0 + (a - 1.0) * k_a)
        per_dir.append((heads(decay), heads(a), heads(kd)))
    return heads(r), heads(v), kk, xg, per_dir


def wkv7_scan(state0, decay, a, k, v, kk, r, reverse):
    seq_first = lambda t: jnp.moveaxis(t.astype(jnp.float32), 1, 0)
    xs = (seq_first(decay), seq_first(a), seq_first(k), seq_first(v), seq_first(kk))
    if r is not None:
        xs = xs + (seq_first(r),)

    def step(S, inp):
        w_t, a_t, k_t, v_t, kk_t = inp[:5]
        sa = jnp.einsum('bhvk,bhk->bhv', S, -kk_t)
        S = (S * w_t[:, :, None, :] + sa[..., None] * (kk_t * a_t)[:, :, None, :]
             + v_t[..., None] * k_t[:, :, None, :])
        y = None if r is None else jnp.einsum('bhvk,bhk->bhv', S, inp[5])
        return S, y

    S, ys = lax.scan(step, state0, xs, reverse=reverse)
    return S, (None if r is None else jnp.moveaxis(ys, 0, 1))


def rwkv_output(y, r, v, per_dir, r_k, ln_w, ln_b, xg, g2):
    B, L = y.shape[:2]
    mu = jnp.mean(y, axis=-1, keepdims=True)
    var = jnp.mean(jnp.square(y - mu), axis=-1, keepdims=True)
    yn = ((y - mu) * lax.rsqrt(var + GN_EPS)).reshape(B, L, RW_WIDTH) * ln_w + ln_b
    rf, vf = r.astype(jnp.float32), v.astype(jnp.float32)
    bonus = sum(jnp.sum(rf * kd.astype(jnp.float32) * r_k, axis=-1, keepdims=True) * vf
                for (_, _, kd) in per_dir)
    g = jax.nn.sigmoid(xg) @ g2
    return (yn + bonus.reshape(B, L, RW_WIDTH)) * g


def swiglu(t, w1, w3, w2):
    return (jax.nn.silu(t @ w1) * (t @ w3)) @ w2


def moe_ffn(h, router_w, router_bias, w1, w3, w2, sw1, sw3, sw2):
    B, L, D = h.shape
    T = B * L
    t = h.reshape(T, D)
    scores = jax.nn.sigmoid((t @ router_w).astype(jnp.float32))
    biased = scores + router_bias.astype(jnp.float32)
    grp = biased.reshape(T, N_GROUPS, N_EXPERTS // N_GROUPS)
    grp_score = jnp.sum(lax.top_k(grp, 2)[0], axis=-1)
    _, grp_idx = lax.top_k(grp_score, TOPK_GROUPS)
    grp_mask = jnp.sum(jax.nn.one_hot(grp_idx, N_GROUPS, dtype=jnp.float32), axis=1) > 0
    exp_mask = jnp.repeat(grp_mask, N_EXPERTS // N_GROUPS, axis=1)
    _, e_idx = lax.top_k(jnp.where(exp_mask, biased, -jnp.inf), TOP_K)
    gates = jnp.take_along_axis(scores, e_idx, axis=1)
    gates = gates / jnp.sum(gates, axis=-1, keepdims=True) * ROUTED_SCALE
    TK = T * TOP_K
    nb = -(-TK // EXPERT_BLOCK) + N_EXPERTS
    P = nb * EXPERT_BLOCK
    e_flat = e_idx.reshape(-1).astype(jnp.int32)
    tok_flat = jnp.repeat(jnp.arange(T, dtype=jnp.int32), TOP_K)
    w_flat = gates.reshape(-1)
    order = jnp.argsort(e_flat)
    e_s, tok_s, w_s = e_flat[order], tok_flat[order], w_flat[order]
    counts = jnp.bincount(e_flat, length=N_EXPERTS).astype(jnp.int32)
    starts = jnp.cumsum(counts) - counts
    padded = ((counts + EXPERT_BLOCK - 1) // EXPERT_BLOCK) * EXPERT_BLOCK
    pends = jnp.cumsum(padded)
    pstarts = pends - padded
    dest = pstarts[e_s] + (jnp.arange(TK, dtype=jnp.int32) - starts[e_s])
    tok_buf = jnp.full((P,), T, jnp.int32).at[dest].set(tok_s)
    w_buf = jnp.zeros((P,), t.dtype).at[dest].set(w_s.astype(t.dtype))
    blk_expert = jnp.clip(jnp.searchsorted(pends, jnp.arange(nb, dtype=jnp.int32) * EXPERT_BLOCK,
                                           side='right'), 0, N_EXPERTS - 1).astype(jnp.int32)
    t_pad = jnp.concatenate([t, jnp.zeros((1, D), t.dtype)], axis=0)

    def expert_block(acc, blk):
        e, toks, wv = blk
        xb = t_pad[toks]
        hb = jax.nn.silu(xb @ w1[e]) * (xb @ w3[e])
        return acc.at[toks].add(((hb @ w2[e]) * wv[:, None]).astype(acc.dtype)), None

    acc, _ = lax.scan(expert_block, jnp.zeros((T + 1, D), t.dtype),
                      (blk_expert, tok_buf.reshape(nb, EXPERT_BLOCK), w_buf.reshape(nb, EXPERT_BLOCK)))
    routed = acc[:T]
    shared = swiglu(t, sw1, sw3, sw2)
    return (routed + shared).reshape(B, L, D)


def setup_inputs(seed: int = 0) -> dict:
    key = jax.random.key(seed)
    ks = iter(jax.random.split(key, 48))
    L, D, E = DEPTH, D_MODEL, N_EXPERTS

    def nrm(shape, scale):
        return jax.random.normal(next(ks), shape, jnp.float32) * scale

    def gain(shape):
        return 1.0 + nrm(shape, 0.05)

    def unif(shape, lo, hi):
        return jax.random.uniform(next(ks), shape, jnp.float32, lo, hi)

    return {
        "x": nrm((BATCH, SEQ, D), 1.0),
        "c": nrm((BATCH, D), 1.0),
        "ctx": nrm((BATCH, CTX_LEN, D), 1.0),
        "c_ctx": nrm((D,), 1.0),
        "w_mod": nrm((L, D, N_MOD * D), 0.5 * D ** -0.5),
        "b_mod": nrm((L, N_MOD * D), 0.02),
        "g_pre_attn": gain((L, D)),
        "g_post_attn": gain((L, D)),
        "g_pre_ffn": gain((L, D)),
        "g_post_ffn": gain((L, D)),
        "w_in": nrm((L, D, IN_COLS), D ** -0.5),
        "w_out": nrm((L, D_MIX, D), D_MIX ** -0.5),
        "da_lambda": nrm((L, 4, DA_HEAD_DIM), 0.1),
        "da_subln": gain((L, 2 * DA_HEAD_DIM)),
        "rw_shift": unif((L, 2, RW_COLS), 0.0, 0.5),
        "rw_k_k": 0.85 + nrm((L, RW_WIDTH), 0.05),
        "rw_k_a": gain((L, RW_WIDTH)),
        "rw_r_k": nrm((L, RW_HEADS, RW_HEAD), 0.1),
        "rw_w0": unif((L, 2, RW_WIDTH), -6.0, -1.0),
        "rw_w2": nrm((L, 2, LORA_W, RW_WIDTH), 0.1 * LORA_W ** -0.5),
        "rw_a0": nrm((L, 2, RW_WIDTH), 0.1),
        "rw_a2": nrm((L, 2, LORA_A, RW_WIDTH), 0.5 * LORA_A ** -0.5),
        "rw_g2": nrm((L, LORA_G, RW_WIDTH), LORA_G ** -0.5),
        "rw_ln_w": gain((L, RW_WIDTH)),
        "rw_ln_b": nrm((L, RW_WIDTH), 0.02),
        "router_w": nrm((L, D, E), D ** -0.5),
        "router_bias": nrm((L, E), 0.01),
        "exp_w1": nrm((L, E, D, EXPERT_FF), D ** -0.5),
        "exp_w3": nrm((L, E, D, EXPERT_FF), D ** -0.5),
        "exp_w2": nrm((L, E, EXPERT_FF, D), EXPERT_FF ** -0.5),
        "sh_w1": nrm((L, D, SHARED_FF), D ** -0.5),
        "sh_w3": nrm((L, D, SHARED_FF), D ** -0.5),
        "sh_w2": nrm((L, SHARED_FF, D), SHARED_FF ** -0.5),
    }


def reference(x, c, ctx, c_ctx, w_mod, b_mod, g_pre_attn, g_post_attn, g_pre_ffn, g_post_ffn,
              w_in, w_out, da_lambda, da_subln, rw_shift, rw_k_k, rw_k_a, rw_r_k,
              rw_w0, rw_w2, rw_a0, rw_a2, rw_g2, rw_ln_w, rw_ln_b,
              router_w, router_bias, exp_w1, exp_w3, exp_w2, sh_w1, sh_w3, sh_w2):
    B, S = x.shape[:2]
    ROWS = S // GRID_W
    cos, sin = axial_rope_tables(ROWS)
    for l in range(DEPTH):
        update_ctx = l < DEPTH - 1
        mod = modulation(c, w_mod[l], b_mod[l], N_MOD)
        mod_c = modulation(c_ctx, w_mod[l], b_mod[l], N_MOD if update_ctx else 2)

        u_lat = modulate(rms_norm(x, g_pre_attn[l]), mod[0], mod[1])
        u_ctx = modulate(rms_norm(ctx, g_pre_attn[l]), mod_c[0], mod_c[1])
        p_lat = u_lat @ w_in[l]
        p_ctx = u_ctx @ w_in[l]

        lam_init = 0.8 - 0.6 * math.exp(-0.3 * l)
        lam = lambda_full(da_lambda[l], lam_init)
        q_l, k_l, v_l = da_heads(p_lat[..., :3 * DA_WIDTH])
        q_c, k_c, v_c = da_heads(p_ctx[..., :3 * DA_WIDTH])
        q_l = apply_rope(q_l, cos, sin)
        k_l = apply_rope(k_l, cos, sin)
        k_all = jnp.concatenate([k_c, k_l], axis=1)
        v_all = jnp.concatenate([v_c, v_l], axis=1)
        o_da = da_output(block_sweep_attention(q_l, k_all, v_all, lam), da_subln[l], lam_init)

        rw_lat = centred_shift(p_lat[..., 3 * DA_WIDTH:], rw_shift[l, 0], rw_shift[l, 1])
        rw_ctx = centred_shift(p_ctx[..., 3 * DA_WIDTH:], rw_shift[l, 0], rw_shift[l, 1])
        r_l, vr_l, kk_l, xg_l, dirs_l = rwkv_features(rw_lat, rw_k_k[l], rw_k_a[l], rw_w0[l], rw_w2[l], rw_a0[l], rw_a2[l])
        r_c, vr_c, kk_c, xg_c, dirs_c = rwkv_features(rw_ctx, rw_k_k[l], rw_k_a[l], rw_w0[l], rw_w2[l], rw_a0[l], rw_a2[l])
        state0 = jnp.zeros((B, RW_HEADS, RW_HEAD, RW_HEAD), jnp.float32)
        r_c_emit = r_c if update_ctx else None
        s_cf, yc_f = wkv7_scan(state0, *dirs_c[0], vr_c, kk_c, r_c_emit, reverse=False)
        s_cb, yc_b = wkv7_scan(state0, *dirs_c[1], vr_c, kk_c, r_c_emit, reverse=True)
        _, y_f = wkv7_scan(s_cf, *dirs_l[0], vr_l, kk_l, r_l, reverse=False)
        _, y_b = wkv7_scan(s_cb, *dirs_l[1], vr_l, kk_l, r_l, reverse=True)
        o_rw = rwkv_output(y_f + y_b, r_l, vr_l, dirs_l, rw_r_k[l], rw_ln_w[l], rw_ln_b[l], xg_l, rw_g2[l])

        o_lat = jnp.concatenate([o_da.astype(jnp.float32), o_rw.astype(jnp.float32)], axis=-1).astype(x.dtype) @ w_out[l]
        x_new = x + (mod[2] * rms_norm(o_lat, g_post_attn[l])).astype(x.dtype)

        h = modulate(rms_norm(x_new, g_pre_ffn[l]), mod[3], mod[4])
        y_moe = moe_ffn(h, router_w[l], router_bias[l], exp_w1[l], exp_w3[l], exp_w2[l], sh_w1[l], sh_w3[l], sh_w2[l])
        x_new = x_new + (mod[5] * rms_norm(y_moe, g_post_ffn[l])).astype(x.dtype)

        if update_ctx:
            o_da_c = da_output(diff_attend(q_c, k_c, v_c, lam), da_subln[l], lam_init)
            o_rw_c = rwkv_output(yc_f + yc_b, r_c, vr_c, dirs_c, rw_r_k[l], rw_ln_w[l], rw_ln_b[l], xg_c, rw_g2[l])
            o_c = jnp.concatenate([o_da_c.astype(jnp.float32), o_rw_c.astype(jnp.float32)], axis=-1).astype(ctx.dtype) @ w_out[l]
            ctx_new = ctx + (mod_c[2] * rms_norm(o_c, g_post_attn[l])).astype(ctx.dtype)
            h_c = modulate(rms_norm(ctx_new, g_pre_ffn[l]), mod_c[3], mod_c[4])
            y_c = moe_ffn(h_c, router_w[l], router_bias[l], exp_w1[l], exp_w3[l], exp_w2[l], sh_w1[l], sh_w3[l], sh_w2[l])
            ctx = ctx_new + (mod_c[5] * rms_norm(y_c, g_post_ffn[l])).astype(ctx.dtype)
        x = x_new
    return x
```

```python
import numpy as np
from contextlib import ExitStack
import concourse.bass as bass
import concourse.mybir as mybir
from concourse.bass_utils import run_bass_kernel_spmd

F32 = mybir.dt.float32
BF16 = mybir.dt.bfloat16
AF = mybir.ActivationFunctionType
ALU = mybir.AluOpType
AX = mybir.AxisListType

D = 4096
KC = D // 128
N_MOD = 6
DA_W = 2048
RW_W = 2048
RW_COLS = 3 * RW_W + 128 + 128 + 480
IN_COLS = 3 * DA_W + RW_COLS
NORM_EPS = 1e-6
GN_EPS = 64e-5
LAM_INIT = 0.2
N_EXP = 64
FF = 512

FULL_CFG = dict(B=4, S=4096, CTX=256, GW=64)


class Buf:
    __slots__ = ("name", "lw", "rd")

    def __init__(self, name):
        self.name = name
        self.lw = None
        self.rd = []


class Op:
    __slots__ = ("eng", "fn", "r", "w", "dma", "need", "sig", "eidx", "fn_deps")

    def __init__(self, eng, fn, r, w, dma):
        self.eng, self.fn, self.r, self.w, self.dma = eng, fn, r, w, dma
        self.need = False
        self.sig = None
        self.eidx = 0


class Prog:
    ENGS = ("pe", "act", "dve", "pool", "sp")
    RING = {"sp": 8, "pool": 6, "act": 4}

    def __init__(self, nc):
        self.nc = nc
        self.e = {"pe": nc.tensor, "act": nc.scalar, "dve": nc.vector, "pool": nc.gpsimd, "sp": nc.sync}
        self.es = ExitStack()
        self.sems = []

        def mk(name):
            h = self.es.enter_context(nc.semaphore(name))
            self.sems.append(h)
            return len(self.sems) - 1

        self.csem = {e: mk("c_" + e) for e in ("pe", "act", "dve", "pool")}
        self.ccnt = {e: 0 for e in self.csem}
        self.ring = {q: [mk("d_%s%d" % (q, i)) for i in range(n)] for q, n in self.RING.items()}
        self.ringval = {q: [0] * n for q, n in self.RING.items()}
        self.dcnt = {q: 0 for q in self.RING}
        self.waited = {e: {} for e in self.ENGS}
        self.ops = []
        self.reg = {}
        self.pstack = None
        self.ecount = {e: 0 for e in self.ENGS}
        self.n_inst = 0
        self.pid = 0

    def sb(self, name, shape, dtype):
        name = "p%d_%s" % (self.pid, name)
        t = self.pstack.enter_context(self.nc.sbuf_tensor(name, list(shape), dtype))
        self.reg[name] = Buf(name)
        return t

    def ps(self, name, shape, dtype=F32):
        name = "p%d_%s" % (self.pid, name)
        t = self.pstack.enter_context(self.nc.psum_tensor(name, list(shape), dtype))
        self.reg[name] = Buf(name)
        return t

    def _bufs(self, aps):
        out = []
        for a in aps:
            if a is None or isinstance(a, (int, float)):
                continue
            b = self.reg.get(a.tensor.name)
            if b is not None and b not in out:
                out.append(b)
        return out

    def add(self, eng, fn, outs, ins, dma=False):
        self.ops.append(Op(eng, fn, self._bufs(ins), self._bufs(outs), dma))

    def dma(self, q, out, in_, slow=False):
        e = self.e[q]
        if slow:
            self.add(q, lambda: e.dma_start(out=out, in_=in_, allow_slow_non_contiguous=True), [out], [in_], dma=True)
        else:
            self.add(q, lambda: e.dma_start(out=out, in_=in_), [out], [in_], dma=True)

    def mm(self, out, lhsT, rhs, start=True, stop=True):
        self.add("pe", lambda: self.nc.tensor.matmul(out, lhsT=lhsT, rhs=rhs, start=start, stop=stop), [out], [lhsT, rhs])

    def tr(self, out, in_, ident):
        self.add("pe", lambda: self.nc.tensor.transpose(out, in_, ident), [out], [in_, ident])

    def act(self, out, in_, func, bias=None, scale=None, accum_out=None):
        kw = {}
        if bias is not None:
            kw["bias"] = bias
        if scale is not None:
            kw["scale"] = scale
        if accum_out is not None:
            kw["accum_out"] = accum_out
        self.add("act", lambda: self.nc.scalar.activation(out=out, in_=in_, func=func, **kw),
                 [out, accum_out], [in_, bias, scale])

    def tt(self, eng, out, in0, in1, op):
        e = self.e[eng]
        self.add(eng, lambda: e.tensor_tensor(out=out, in0=in0, in1=in1, op=op), [out], [in0, in1])

    def ts(self, eng, out, in0, s1, op0, s2=None, op1=None, accum_out=None):
        e = self.e[eng]
        kw = {}
        if op1 is not None:
            kw["op1"] = op1
        if accum_out is not None:
            kw["accum_out"] = accum_out
        self.add(eng, lambda: e.tensor_scalar(out=out, in0=in0, scalar1=s1, scalar2=s2, op0=op0, **kw),
                 [out, accum_out], [in0, s1, s2])

    def stt(self, out, in0, scalar, in1, op0, op1):
        self.add("dve", lambda: self.nc.vector.scalar_tensor_tensor(out=out, in0=in0, scalar=scalar, in1=in1, op0=op0, op1=op1),
                 [out], [in0, scalar, in1])

    def copy(self, eng, out, in_):
        if eng == "act":
            self.add("act", lambda: self.nc.scalar.copy(out=out, in_=in_), [out], [in_])
        else:
            e = self.e[eng]
            self.add(eng, lambda: e.tensor_copy(out=out, in_=in_), [out], [in_])

    def red(self, out, in_, op, axis=None, negate=None):
        ax = AX.X if axis is None else axis
        kw = {}
        if negate is not None:
            kw["negate"] = negate
        self.add("dve", lambda: self.nc.vector.tensor_reduce(out=out, in_=in_, axis=ax, op=op, **kw), [out], [in_])

    def recip(self, out, in_):
        self.add("dve", lambda: self.nc.vector.reciprocal(out=out, in_=in_), [out], [in_])

    def memset(self, eng, ap, val):
        e = self.e[eng]
        self.add(eng, lambda: e.memset(ap, val), [ap], [])

    def scan(self, out, d0, d1, initial, op0, op1):
        self.add("dve", lambda: self.nc.vector.tensor_tensor_scan(out=out, data0=d0, data1=d1, initial=initial, op0=op0, op1=op1),
                 [out], [d0, d1, initial])

    def max8(self, out, in_):
        self.add("dve", lambda: self.nc.vector.max(out=out, in_=in_), [out], [in_])

    def phase(self):
        prog = self

        class _Ph:
            def __enter__(s):
                prog.pid += 1
                prog.pstack = ExitStack()
                prog.pstack.__enter__()
                return prog

            def __exit__(s, et, ev, tb):
                if et is None:
                    prog.flush()
                prog.pstack.__exit__(et, ev, tb)
                prog.pstack = None
                prog.reg = {}
                return False

        return _Ph()

    def _wait(self, eng, sem, val):
        if val <= 0:
            return
        if self.waited[eng].get(sem, 0) >= val:
            return
        self.e[eng].wait_ge(self.sems[sem], val)
        self.waited[eng][sem] = val
        self.n_inst += 1

    def flush(self):
        ops = self.ops
        self.ops = []
        ecnt = dict(self.ecount)
        last_compute = {}
        for op in ops:
            ecnt[op.eng] += 1
            op.eidx = ecnt[op.eng]
            deps = []
            for b in op.r:
                if b.lw is not None:
                    deps.append((b.lw, "raw"))
            for b in op.w:
                if b.lw is not None:
                    deps.append((b.lw, "waw"))
                for r in b.rd:
                    deps.append((r, "war"))
            keep = []
            for d, kind in deps:
                if d is op:
                    continue
                if d.eng == op.eng and not d.dma:
                    if op.dma:
                        pass
                    elif op.eng == "pe":
                        continue
                    elif kind != "raw":
                        continue
                    elif op.eidx - d.eidx > 6:
                        continue
                if d not in keep:
                    keep.append(d)
            best = {}
            final = []
            for d in keep:
                if d.dma:
                    final.append(d)
                elif d.eng not in best or d.eidx > best[d.eng].eidx:
                    best[d.eng] = d
            keep = final + list(best.values())
            for d in keep:
                d.need = True
            op.fn_deps = keep
            for b in op.r:
                b.rd.append(op)
            for b in op.w:
                b.lw = op
                b.rd = []
            if not op.dma:
                last_compute[op.eng] = op
        for op in last_compute.values():
            op.need = True
        for op in ops:
            eng = op.eng
            for d in op.fn_deps:
                sem, val = d.sig
                self._wait(eng, sem, val)
            if op.dma:
                n = len(self.ring[eng])
                i = self.dcnt[eng] % n
                self.dcnt[eng] += 1
                sem = self.ring[eng][i]
                self._wait(eng, sem, self.ringval[eng][i])
                inst = op.fn()
                self.ringval[eng][i] += 16
                inst.then_inc(self.sems[sem], 16)
                op.sig = (sem, self.ringval[eng][i])
            else:
                inst = op.fn()
                if op.need:
                    self.ccnt[eng] += 1
                    inst.then_inc(self.sems[self.csem[eng]], 1)
                    op.sig = (self.csem[eng], self.ccnt[eng])
            self.n_inst += 1
            self.ecount[eng] += 1
        self.barrier()

    def barrier(self):
        for eng in self.ENGS:
            for c in self.csem:
                if c != eng:
                    self._wait(eng, self.csem[c], self.ccnt[c])
            for q in self.ring:
                for i, sem in enumerate(self.ring[q]):
                    self._wait(eng, sem, self.ringval[q][i])

    def close(self):
        self.es.close()


def splits(n0, n1, w):
    out = []
    c = n0
    while c < n1:
        n = min(w, n1 - c)
        out.append((c, n))
        c += n
    return out


class Rot:
    def __init__(self, lst):
        self.lst = lst
        self.i = 0

    def __call__(self):
        r = self.lst[self.i % len(self.lst)]
        self.i += 1
        return r


EXPM05 = 0.6065306597126334
NPAR = 10
PK_K, PK_A, POMKA, PR_K, PLNW, PLNB, PW0, PA0 = 0, 1, 2, 3, 4, 5, 6, 8
NRT = 54


def build_program(cfg, debug=()):
    S, CTX = cfg["S"], cfg["CTX"]
    OWN = S // 2
    L = S + CTX
    NCH = L // 128
    NE = cfg.get("NE", N_EXP)
    NG = 8
    GS = NE // NG
    NE1 = NE + 1
    phases = cfg.get("phases", None)
    feed = cfg.get("feed", ())
    nc = bass.Bass("TRN2", target_bir_lowering=False)
    in_names = []
    in_cache = {}

    def on(ph):
        return phases is None or ph in phases

    def IN(name, shape, dt=F32):
        if name not in in_cache:
            in_names.append(name)
            in_cache[name] = nc.dram_tensor(name, list(shape), dt, kind="ExternalInput").ap()
        return in_cache[name]

    def scratch(name, shape, dt):
        if name in feed:
            return IN(name, shape, dt)
        kind = "ExternalOutput" if name in debug else "Internal"
        return nc.dram_tensor(name, list(shape), dt, kind=kind).ap()

    out_d = nc.dram_tensor("out", [OWN, D], F32, kind="ExternalOutput").ap()

    modscr = scratch("modscr", [2, N_MOD * D], F32)
    uT = scratch("uT", [D, L], BF16)
    qT = scratch("qT", [DA_W, OWN], BF16)
    kT = scratch("kT", [DA_W, L], BF16)
    vS = scratch("vS", [L, DA_W], BF16)
    rwT = scratch("rwT", [RW_COLS, L], BF16)
    oT = scratch("oT", [D, OWN], BF16)
    sgT = scratch("sgT", [512, OWN], BF16)
    AhT = [scratch("AhT%d" % d, [RW_W, L], BF16) for d in range(2)]
    RhT = [scratch("RhT%d" % d, [RW_W, L], BF16) for d in range(2)]
    BtT = [scratch("BtT%d" % d, [RW_W, L], BF16) for d in range(2)]
    KtT = [scratch("KtT%d" % d, [RW_W, L], BF16) for d in range(2)]
    Btok = [scratch("Btok%d" % d, [L, RW_W], BF16) for d in range(2)]
    Ktok = [scratch("Ktok%d" % d, [L, RW_W], BF16) for d in range(2)]
    Vtok = scratch("Vtok", [L, RW_W], BF16)
    Wc = [scratch("Wc%d" % d, [RW_W, NCH], F32) for d in range(2)]
    bonusT = scratch("bonusT", [RW_W, OWN], F32)
    Gs = [scratch("Gs%d" % d, [NCH, 128, 32 * 128], BF16) for d in range(2)]
    Ns = [scratch("Ns%d" % d, [NCH, 128, 32 * 128], BF16) for d in range(2)]
    Z0s = [scratch("Z0s%d" % d, [NCH, 128, 32 * 64], F32) for d in range(2)]
    Y0s = [scratch("Y0s%d" % d, [NCH, 128, 16 * 128], F32) for d in range(2)]
    ysc = [scratch("ysc%d" % d, [RW_W, OWN], F32) for d in range(2)]
    olat = scratch("olat", [OWN, D], F32)
    x1s = scratch("x1s", [OWN, D], F32)
    hTs = scratch("hTs", [D, OWN], BF16)
    gscT = scratch("gscT", [NE, OWN], F32)
    hbs = scratch("hbs", [NE1 * FF, OWN], BF16)
    ymoe = scratch("ymoe", [OWN, D], F32)

    P = Prog(nc)

    def load_ident(P):
        idf = P.sb("idf", [128, 128], F32)
        idb = P.sb("idb", [128, 128], BF16)
        P.dma("sp", idf[:], IN("ident", [128, 128])[:, :])
        P.copy("dve", idb[:], idf[:])
        return idf, idb

    def rms_stats(P, s, x, junk, n):
        P.act(junk, x, AF.Square, accum_out=s[:, 0:1])
        P.ts("dve", s[:, 1:2], s[:, 0:1], 1.0 / n, ALU.mult, NORM_EPS, ALU.add)
        P.act(s[:, 2:3], s[:, 1:2], AF.Sqrt)
        P.recip(s[:, 3:4], s[:, 2:3])

    if on(0):
        with P.phase():
            cv = P.sb("cv", [128, KC, 2], F32)
            cs = P.sb("cs", [128, KC, 2], F32)
            P.dma("sp", cv[:], IN("cvec", [128, KC, 2])[:, :, :])
            P.act(cs[:], cv[:], AF.Silu)
            w_mod = IN("w_mod", [D, N_MOD * D])
            b_mod = IN("b_mod", [1, N_MOD * D])
            wv = w_mod.rearrange("(k p) n -> p k n", p=128)
            NB = 256
            wts = [P.sb("wm%d" % i, [128, KC, NB], F32) for i in range(2)]
            pss = [P.ps("pm%d" % i, [128, 512]) for i in range(2)]
            bms = [P.sb("bm%d" % i, [2, NB], F32) for i in range(2)]
            mos = [P.sb("mo%d" % i, [2, NB], F32) for i in range(2)]
            for blk in range(N_MOD * D // NB):
                wt, ps, bm, mo = wts[blk % 2], pss[blk % 2], bms[blk % 2], mos[blk % 2]
                c0 = blk * NB
                P.dma("sp", wt[:], wv[:, :, c0:c0 + NB])
                P.dma("sp", bm[:], b_mod[0:1, c0:c0 + NB].partition_broadcast(2))
                for kc in range(KC):
                    P.mm(ps[0:2, 0:NB], cs[:, kc, :], wt[:, kc, :], start=(kc == 0), stop=(kc == KC - 1))
                P.tt("dve", mo[:], ps[0:2, 0:NB], bm[:], ALU.add)
                P.dma("sp", modscr[:, c0:c0 + NB], mo[:])

    if on(1):
        with P.phase():
            x_l = IN("x_l", [L, D])
            idf, idb = load_ident(P)
            A = P.sb("A", [128, D], F32)
            Sh = P.sb("Sh", [128, D], F32)
            gb = P.sb("gb", [128, D], F32)
            P.dma("sp", gb[:], IN("g_pre_attn", [1, D])[0:1, :].partition_broadcast(128))
            xs = [P.sb("x%d" % i, [128, D], F32) for i in range(2)]
            tmp = P.sb("tmp", [128, D], F32)
            ub = [P.sb("ub%d" % i, [128, D], BF16) for i in range(2)]
            sq = P.sb("sq", [128, D], BF16)
            st = [P.sb("st%d" % i, [128, 4], F32) for i in range(2)]
            uTs = P.sb("uTs", [128, KC, 512], BF16)
            pts = [P.ps("pt%d" % i, [128, 1024], BF16) for i in range(4)]
            uTv = uT.rearrange("(k p) l -> p k l", p=128)
            cur_var = None
            for g0, gn in splits(0, L // 128, 4):
                for ti in range(g0, g0 + gn):
                    var = 0 if ti * 128 < S else 1
                    if var != cur_var:
                        cur_var = var
                        P.dma("sp", Sh[:], modscr[var:var + 1, 0:D].partition_broadcast(128))
                        P.dma("sp", A[:], modscr[var:var + 1, D:2 * D].partition_broadcast(128))
                        P.stt(A[:], A[:], 1.0, gb[:], ALU.add, ALU.mult)
                    x, s, u = xs[ti % 2], st[ti % 2], ub[ti % 2]
                    P.dma("sp", x[:], x_l[ti * 128:(ti + 1) * 128, :])
                    rms_stats(P, s, x[:], sq[:], D)
                    P.stt(tmp[:], x[:], s[:, 3:4], A[:], ALU.mult, ALU.mult)
                    P.tt("pool", u[:], tmp[:], Sh[:], ALU.add)
                    for q4 in range(4):
                        pt = pts[q4]
                        for k8 in range(8):
                            kc = q4 * 8 + k8
                            P.tr(pt[:, k8 * 128:(k8 + 1) * 128], u[:, kc * 128:(kc + 1) * 128], idb[:])
                        dst = uTs[:, q4 * 8:(q4 + 1) * 8, (ti - g0) * 128:(ti - g0 + 1) * 128]
                        P.copy("act" if q4 % 2 else "dve", dst, pt[:].rearrange("p (k t) -> p k t", k=8))
                P.dma("sp", uTv[:, :, g0 * 128:(g0 + gn) * 128], uTs[:, :, 0:gn * 128])

    if on(2):
        with P.phase():
            w_in = IN("w_in", [D, IN_COLS])
            cos_d = IN("cosT", [128, L])
            sin_d = IN("sinT", [128, L])
            pmf = P.sb("pmf", [128, 128], F32)
            pmb = P.sb("pmb", [128, 128], BF16)
            P.dma("sp", pmf[:], IN("pm", [128, 128])[:, :])
            P.copy("dve", pmb[:], pmf[:])
            TBMAX = 1152
            uTb = P.sb("uTb", [128, KC, TBMAX], BF16)
            cosb = P.sb("cosb", [128, TBMAX], F32)
            sinb = P.sb("sinb", [128, TBMAX], F32)
            wbs = Rot([P.sb("wb%d" % i, [128, KC, 256], BF16) for i in range(2)])
            pss = Rot([P.ps("pp%d" % i, [128, 512]) for i in range(3)])
            ps2s = Rot([P.ps("pq%d" % i, [128, 512]) for i in range(2)])
            pTs = Rot([P.sb("pT%d" % i, [128, 512], BF16) for i in range(2)])
            t1s = Rot([P.sb("t1%d" % i, [128, 512], F32) for i in range(2)])
            t2s = Rot([P.sb("t2%d" % i, [128, 512], F32) for i in range(2)])
            obs = Rot([P.sb("ob%d" % i, [128, 512], BF16) for i in range(3)])
            uTv = uT.rearrange("(k p) l -> p k l", p=128)
            wv = w_in.rearrange("(k p) n -> p k n", p=128)
            blocks = [(t0, tn, True) for t0, tn in splits(0, OWN, 1024)] + \
                     [(t0, tn, False) for t0, tn in splits(OWN, L, TBMAX)]
            for (t0, tn, is_own) in blocks:
                P.dma("sp", uTb[:, :, 0:tn], uTv[:, :, t0:t0 + tn])
                P.dma("sp", cosb[:, 0:tn], cos_d[:, t0:t0 + tn])
                P.dma("sp", sinb[:, 0:tn], sin_d[:, t0:t0 + tn])
                groups = []
                if is_own:
                    groups += [("q", c, n) for c, n in splits(0, DA_W, 256)]
                groups += [("k", c, n) for c, n in splits(DA_W, 2 * DA_W, 256)]
                groups += [("v", c, n) for c, n in splits(2 * DA_W, 3 * DA_W, 256)]
                groups += [("rw", c, n) for c, n in splits(3 * DA_W, IN_COLS, 256)]
                for kind, c0, cn in groups:
                    wb = wbs()
                    P.dma("pool", wb[:, :, 0:cn], wv[:, :, c0:c0 + cn])
                    if kind == "v":
                        for tt0, _ in splits(0, tn, 128):
                            ps = pss()
                            for kc in range(KC):
                                P.mm(ps[:, 0:cn], uTb[:, kc, tt0:tt0 + 128], wb[:, kc, 0:cn], start=(kc == 0), stop=(kc == KC - 1))
                            ob = obs()
                            P.copy("act", ob[:, 0:cn], ps[:, 0:cn])
                            P.dma("sp", vS[t0 + tt0:t0 + tt0 + 128, c0 - 2 * DA_W:c0 - 2 * DA_W + cn], ob[:, 0:cn])
                        continue
                    for h0, hn in splits(0, cn, 128):
                        for s0, sn in splits(0, tn, 512):
                            ps = pss()
                            for kc in range(KC):
                                P.mm(ps[0:hn, 0:sn], wb[:, kc, h0:h0 + hn], uTb[:, kc, s0:s0 + sn], start=(kc == 0), stop=(kc == KC - 1))
                            ob = obs()
                            if kind == "rw":
                                P.copy("act", ob[0:hn, 0:sn], ps[0:hn, 0:sn])
                                r0 = c0 + h0 - 3 * DA_W
                                P.dma("sp", rwT[r0:r0 + hn, t0 + s0:t0 + s0 + sn], ob[0:hn, 0:sn])
                            else:
                                pT = pTs()
                                P.copy("act", pT[:, 0:sn], ps[:, 0:sn])
                                ps2 = ps2s()
                                P.mm(ps2[:, 0:sn], pmb[:], pT[:, 0:sn])
                                t1, t2 = t1s(), t2s()
                                P.tt("pool", t1[:, 0:sn], pT[:, 0:sn], cosb[:, s0:s0 + sn], ALU.mult)
                                P.tt("dve", t2[:, 0:sn], ps2[:, 0:sn], sinb[:, s0:s0 + sn], ALU.mult)
                                P.tt("dve", ob[:, 0:sn], t1[:, 0:sn], t2[:, 0:sn], ALU.add)
                                if kind == "q":
                                    r0 = c0 + h0
                                    P.dma("sp", qT[r0:r0 + 128, t0 + s0:t0 + s0 + sn], ob[:, 0:sn])
                                else:
                                    r0 = c0 + h0 - DA_W
                                    P.dma("sp", kT[r0:r0 + 128, t0 + s0:t0 + s0 + sn], ob[:, 0:sn])

    if on(3):
        with P.phase():
            idf, idb = load_ident(P)
            dl = P.sb("dl", [128, 512], F32)
            P.dma("sp", dl[:], IN("da_lambda", [1, 512])[0:1, :].partition_broadcast(128))
            pr = P.sb("pr", [128, 256], F32)
            lm = P.sb("lm", [128, 8], F32)
            P.tt("dve", pr[:, 0:128], dl[:, 0:128], dl[:, 128:256], ALU.mult)
            P.tt("dve", pr[:, 128:256], dl[:, 256:384], dl[:, 384:512], ALU.mult)
            P.red(lm[:, 0:1], pr[:, 0:128], ALU.add)
            P.red(lm[:, 1:2], pr[:, 128:256], ALU.add)
            P.act(lm[:, 2:4], lm[:, 0:2], AF.Exp)
            P.tt("dve", lm[:, 4:5], lm[:, 2:3], lm[:, 3:4], ALU.subtract)
            P.ts("dve", lm[:, 5:6], lm[:, 4:5], LAM_INIT, ALU.add, -1.0, ALU.mult)
            sg = P.sb("sg", [128, 256], F32)
            P.dma("sp", sg[:], IN("da_subln", [1, 256])[0:1, :].partition_broadcast(128))
            P.ts("dve", sg[:], sg[:], 1.0 - LAM_INIT, ALU.mult)
            NKT = L // 128
            SC = 128.0 ** -0.5
            kTv = kT.rearrange("(h m d) l -> h d m l", m=2, d=128)
            qTv = qT.rearrange("(h m d) l -> h d m l", m=2, d=128)
            vSv = vS.rearrange("(kt p) (h e) -> h p kt e", p=128, e=256)
            sets = []
            for i in range(2):
                kth = P.sb("kth%d" % i, [128, 2, L], BF16)
                qth = P.sb("qth%d" % i, [128, 2, OWN], BF16)
                vh = P.sb("vh%d" % i, [128, NKT, 257], BF16)
                P.memset("pool", vh[:, :, 256:257], 1.0)
                sets.append((kth, qth, vh))
            pss = Rot([P.ps("as%d" % i, [128, 512]) for i in range(2)])
            acc = [P.ps("ac%d" % i, [128, 512]) for i in range(4)]
            ptrs = Rot([P.ps("atr", [128, 1024], BF16)])
            PTs = Rot([P.sb("PT%d" % i, [128, 512], BF16) for i in range(3)])
            o0 = [P.sb("o0%d" % i, [128, 256], F32) for i in range(4)]
            sts = [P.sb("ast%d" % i, [128, 8], F32) for i in range(4)]
            junk = P.sb("ajunk", [128, 256], BF16)
            ons = Rot([P.sb("on%d" % i, [128, 256], BF16) for i in range(2)])
            oTss = Rot([P.sb("oTs%d" % i, [128, 2, 512], BF16) for i in range(2)])
            for h in range(8):
                kth, qth, vh = sets[h % 2]
                P.dma("sp", kth[:], kTv[h])
                P.dma("sp", qth[:], qTv[h])
                P.dma("sp", vh[:, :, 0:256], vSv[h])
                for q0, qn in splits(0, OWN, 512):
                    nqt = qn // 128
                    for m in range(2):
                        for kt in range(NKT):
                            ps = pss()
                            P.mm(ps[:, 0:qn], kth[:, m, kt * 128:(kt + 1) * 128], qth[:, m, q0:q0 + qn])
                            pt = PTs()
                            P.act(pt[:, 0:qn], ps[:, 0:qn], AF.Exp, scale=SC)
                            for qt in range(nqt):
                                P.mm(acc[qt][:, 0:257], pt[:, qt * 128:(qt + 1) * 128], vh[:, kt, :],
                                     start=(kt == 0), stop=(kt == NKT - 1))
                        for qt in range(nqt):
                            st = sts[qt]
                            P.recip(st[:, m:m + 1], acc[qt][:, 256:257])
                            if m == 0:
                                P.ts("dve", o0[qt][:], acc[qt][:, 0:256], st[:, 0:1], ALU.mult)
                            else:
                                P.tt("dve", st[:, 2:3], st[:, 1:2], lm[:, 5:6], ALU.mult)
                                P.stt(o0[qt][:], acc[qt][:, 0:256], st[:, 2:3], o0[qt][:], ALU.mult, ALU.add)
                    oTs = oTss()
                    for qt in range(nqt):
                        st = sts[qt]
                        P.act(junk[:], o0[qt][:], AF.Square, accum_out=st[:, 3:4])
                        P.ts("dve", st[:, 4:5], st[:, 3:4], 1.0 / 256, ALU.mult, NORM_EPS, ALU.add)
                        P.act(st[:, 5:6], st[:, 4:5], AF.Sqrt)
                        P.recip(st[:, 6:7], st[:, 5:6])
                        on_ = ons()
                        P.stt(on_[:], o0[qt][:], st[:, 6:7], sg[:], ALU.mult, ALU.mult)
                        ptr = ptrs()
                        for e2 in range(2):
                            P.tr(ptr[:, e2 * 128:(e2 + 1) * 128], on_[:, e2 * 128:(e2 + 1) * 128], idb[:])
                        P.copy("act", oTs[:, :, qt * 128:(qt + 1) * 128], ptr[:, 0:256].rearrange("p (a t) -> p a t", a=2))
                    P.dma("sp", oT[h * 256:(h + 1) * 256, q0:q0 + qn].rearrange("(a p) t -> p a t", p=128), oTs[:, :, 0:qn])

    Lp = L + 4

    def ix(l):
        return l + 1 if l < S else l + 3

    dir_ranges = [[(0, OWN), (S, L)], [(0, S), (S, L)]]
    if on(4):
        with P.phase():
            idf, idb = load_ident(P)
            shab = P.sb("shab", [128, NRT, 2], F32)
            P.dma("sp", shab[:], IN("shAB", [128, NRT, 2])[:, :, :])
            c0s = P.sb("c0s", [128, NRT], F32)
            P.tt("dve", c0s[:].unsqueeze(2), shab[:, :, 0:1], shab[:, :, 1:2], ALU.add)
            P.ts("dve", c0s[:], c0s[:], -1.0, ALU.mult, 1.0, ALU.add)
            chp = P.sb("chp", [128, 16, NPAR], F32)
            P.dma("sp", chp[:], IN("chp", [128, 16, NPAR])[:, :, :])
            P.ts("dve", chp[:, :, POMKA:POMKA + 1], chp[:, :, PK_A:PK_A + 1], -1.0, ALU.mult, 1.0, ALU.add)
            bo = P.sb("bo", [128, 128], F32)
            P.dma("sp", bo[:], IN("bo", [128, 128])[:, :])
            w2b = [P.sb("w2b%d" % d, [128, RW_W], BF16) for d in range(2)]
            a2b = [P.sb("a2b%d" % d, [128, RW_W], BF16) for d in range(2)]
            w2d = IN("rw_w2d", [2, 128, RW_W])
            a2d = IN("rw_a2d", [2, 128, RW_W])
            for d in range(2):
                P.dma("pool", w2b[d][:], w2d[d])
                P.dma("pool", a2b[d][:], a2d[d])
            raws = [P.sb("raw%d" % i, [128, Lp], BF16) for i in range(3)]
            for r in raws:
                for pidx in (0, S + 1, S + 2, L + 3):
                    P.memset("pool", r[:, pidx:pidx + 1], 0.0)

            def load_raw(raw, r0, rn):
                P.dma("sp", raw[0:rn, 1:S + 1], rwT[r0:r0 + rn, 0:S])
                P.dma("sp", raw[0:rn, S + 3:L + 3], rwT[r0:r0 + rn, S:L])

            def shift(out, raw, rt, rn=128):
                P.ts("dve", out[0:rn, 1:L + 3], raw[0:rn, 1:L + 3], c0s[0:rn, rt:rt + 1], ALU.mult)
                P.stt(out[0:rn, 1:L + 3], raw[0:rn, 0:L + 2], shab[0:rn, rt, 0:1], out[0:rn, 1:L + 3], ALU.mult, ALU.add)
                P.stt(out[0:rn, 1:L + 3], raw[0:rn, 2:L + 4], shab[0:rn, rt, 1:2], out[0:rn, 1:L + 3], ALU.mult, ALU.add)

            ks = P.sb("ks", [128, Lp], F32)
            kk = P.sb("kk", [128, Lp], F32)
            rs = P.sb("rs", [128, Lp], BF16)
            vs = P.sb("vs", [128, Lp], BF16)
            tw = P.sb("tw", [128, Lp], BF16)
            xab = P.sb("xab", [128, Lp], BF16)
            load_raw(raws[0], 3 * RW_W, 128)
            shift(ks, raws[0], 48)
            P.act(tw[:, 1:L + 3], ks[:, 1:L + 3], AF.Tanh)
            load_raw(raws[1], 3 * RW_W + 128, 128)
            shift(xab, raws[1], 49)
            sgs = P.sb("sgs", [128, OWN], BF16)
            for gi in range(4):
                rn = 128 if gi < 3 else 96
                raw = raws[(2 + gi) % 3]
                load_raw(raw, 3 * RW_W + 256 + gi * 128, rn)
                shift(kk, raw, 50 + gi, rn)
                P.act(sgs[0:rn, :], kk[0:rn, 1:OWN + 1], AF.Sigmoid)
                P.dma("sp", sgT[gi * 128:gi * 128 + rn, :], sgs[0:rn, :])
            SEG = 512
            pss = Rot([P.ps("fp%d" % i, [128, 512]) for i in range(3)])
            ptb = Rot([P.ps("ftb%d" % i, [128, 1024], BF16) for i in range(3)])
            T = {n: P.sb("f_" + n, [128, SEG], F32) for n in ("sig", "lw", "cum", "cum2", "ta", "tb", "e1", "e2", "e3", "av", "kd")}
            OB = {n: P.sb("f_" + n, [128, SEG], BF16) for n in ("ah", "rh", "bt", "kt")}
            m01 = P.sb("m01", [128, SEG], F32)
            P.memset("dve", m01[:], 1.0)
            P.memset("dve", m01[:].rearrange("p (c i) -> p c i", i=128)[:, :, 0:1], 0.0)
            kdsum = P.sb("kdsum", [128, OWN], F32)
            sqb = P.sb("sqb", [128, 512], F32)
            kkr = P.sb("kkr", [128, 512], F32)
            sd = P.sb("sd", [128, 512], F32)
            wcs = P.sb("wcs", [128, 8], F32)
            stg = Rot([P.sb("stg%d" % i, [128, 8, 128], BF16) for i in range(3)])
            bon = P.sb("bon", [128, 512], F32)
            for ct in range(16):
                for i, (rt, dst) in enumerate(((ct, rs), (16 + ct, ks), (32 + ct, vs))):
                    load_raw(raws[i], rt * 128, 128)
                    shift(dst, raws[i], rt)
                for b0, bn in splits(1, L + 3, 512):
                    P.ts("dve", kkr[:, 0:bn], ks[:, b0:b0 + bn], chp[:, ct, PK_K:PK_K + 1], ALU.mult)
                    P.tt("pool", sqb[:, 0:bn], kkr[:, 0:bn], kkr[:, 0:bn], ALU.mult)
                    ps = pss()
                    P.mm(ps[:, 0:bn], bo[:], sqb[:, 0:bn])
                    P.act(sd[:, 0:bn], ps[:, 0:bn], AF.Sqrt)
                    P.ts("dve", sd[:, 0:bn], sd[:, 0:bn], 1e-12, ALU.max)
                    P.recip(sd[:, 0:bn], sd[:, 0:bn])
                    P.tt("dve", kk[:, b0:b0 + bn], kkr[:, 0:bn], sd[:, 0:bn], ALU.mult)
                for c0, cn in splits(0, NCH, 8):
                    pt = ptb()
                    for cc in range(cn):
                        i0 = ix((c0 + cc) * 128)
                        P.tr(pt[:, cc * 128:(cc + 1) * 128], vs[:, i0:i0 + 128], idb[:])
                    sg_ = stg()
                    P.copy("act", sg_[:, 0:cn, :], pt[:, 0:cn * 128].rearrange("p (c n) -> p c n", n=128))
                    P.dma("sp", Vtok[c0 * 128:(c0 + cn) * 128, ct * 128:(ct + 1) * 128].rearrange("(c p) n -> p c n", p=128), sg_[:, 0:cn, :])
                for di in range(2):
                    for (ra, rb) in dir_ranges[di]:
                        for l0, n in splits(ra, rb, SEG):
                            i0 = ix(l0)
                            ncn = n // 128
                            for s0, sn in splits(0, n, 512):
                                ps = pss()
                                P.mm(ps[:, 0:sn], w2b[di][:, ct * 128:(ct + 1) * 128], tw[:, i0 + s0:i0 + s0 + sn])
                                P.act(T["sig"][:, s0:s0 + sn], ps[:, 0:sn], AF.Sigmoid, bias=chp[:, ct, PW0 + di:PW0 + di + 1])
                                ps = pss()
                                P.mm(ps[:, 0:sn], a2b[di][:, ct * 128:(ct + 1) * 128], xab[:, i0 + s0:i0 + s0 + sn])
                                P.act(T["av"][:, s0:s0 + sn], ps[:, 0:sn], AF.Sigmoid, bias=chp[:, ct, PA0 + di:PA0 + di + 1])
                            P.ts("dve", T["lw"][:, 0:n], T["sig"][:, 0:n], -EXPM05, ALU.mult)
                            P.scan(T["cum"][:, 0:n], m01[:, 0:n], T["lw"][:, 0:n], 0.0, ALU.mult, ALU.add)
                            cum = T["cum"]
                            if di == 1:
                                P.tt("dve", T["ta"][:, 0:n], T["lw"][:, 0:n], T["cum"][:, 0:n], ALU.subtract)
                                tot = T["cum"][:, 0:n].rearrange("p (c i) -> p c i", i=128)[:, :, 127:128].to_broadcast([128, ncn, 128])
                                P.tt("dve", T["cum2"][:, 0:n].rearrange("p (c i) -> p c i", i=128),
                                     T["ta"][:, 0:n].rearrange("p (c i) -> p c i", i=128), tot, ALU.add)
                                cum = T["cum2"]
                            P.act(T["e1"][:, 0:n], cum[:, 0:n], AF.Exp)
                            P.act(T["e2"][:, 0:n], cum[:, 0:n], AF.Exp, scale=-1.0)
                            P.tt("pool", T["tb"][:, 0:n], cum[:, 0:n], T["lw"][:, 0:n], ALU.subtract)
                            P.act(T["e3"][:, 0:n], T["tb"][:, 0:n], AF.Exp)
                            P.ts("dve", T["ta"][:, 0:n], T["av"][:, 0:n], chp[:, ct, PK_A:PK_A + 1], ALU.mult, chp[:, ct, POMKA:POMKA + 1], ALU.add)
                            P.tt("dve", T["kd"][:, 0:n], T["ta"][:, 0:n], ks[:, i0:i0 + n], ALU.mult)
                            P.stt(OB["ah"][:, 0:n], kk[:, i0:i0 + n], -1.0, T["e3"][:, 0:n], ALU.mult, ALU.mult)
                            P.tt("pool", OB["rh"][:, 0:n], rs[:, i0:i0 + n], T["e1"][:, 0:n], ALU.mult)
                            P.tt("dve", T["ta"][:, 0:n], kk[:, i0:i0 + n], T["av"][:, 0:n], ALU.mult)
                            P.tt("dve", OB["bt"][:, 0:n], T["ta"][:, 0:n], T["e2"][:, 0:n], ALU.mult)
                            P.tt("pool", OB["kt"][:, 0:n], T["kd"][:, 0:n], T["e2"][:, 0:n], ALU.mult)
                            e1v = T["e1"][:, 0:n].rearrange("p (c i) -> p c i", i=128)
                            pos = 127 if di == 0 else 0
                            P.copy("act", wcs[:, 0:ncn], e1v[:, :, pos])
                            rows = slice(ct * 128, (ct + 1) * 128)
                            P.dma("sp", Wc[di][rows, l0 // 128:l0 // 128 + ncn], wcs[:, 0:ncn], slow=True)
                            P.dma("sp", AhT[di][rows, l0:l0 + n], OB["ah"][:, 0:n])
                            P.dma("sp", RhT[di][rows, l0:l0 + n], OB["rh"][:, 0:n])
                            P.dma("sp", BtT[di][rows, l0:l0 + n], OB["bt"][:, 0:n])
                            P.dma("sp", KtT[di][rows, l0:l0 + n], OB["kt"][:, 0:n])
                            for nm, dstT in (("bt", Btok[di]), ("kt", Ktok[di])):
                                pt = ptb()
                                for cc in range(ncn):
                                    P.tr(pt[:, cc * 128:(cc + 1) * 128], OB[nm][:, cc * 128:(cc + 1) * 128], idb[:])
                                sg_ = stg()
                                P.copy("act", sg_[:, 0:ncn, :], pt[:, 0:ncn * 128].rearrange("p (c n) -> p c n", n=128))
                                P.dma("sp", dstT[l0:l0 + n, ct * 128:(ct + 1) * 128].rearrange("(c p) n -> p c n", p=128), sg_[:, 0:ncn, :])
                            if l0 < OWN:
                                no = min(n, OWN - l0)
                                if di == 0:
                                    P.copy("pool", kdsum[:, l0:l0 + no], T["kd"][:, 0:no])
                                else:
                                    P.tt("pool", kdsum[:, l0:l0 + no], kdsum[:, l0:l0 + no], T["kd"][:, 0:no], ALU.add)
                for b0, bn in splits(0, OWN, 512):
                    P.tt("dve", sqb[:, 0:bn], rs[:, b0 + 1:b0 + 1 + bn], kdsum[:, b0:b0 + bn], ALU.mult)
                    P.ts("dve", sqb[:, 0:bn], sqb[:, 0:bn], chp[:, ct, PR_K:PR_K + 1], ALU.mult)
                    ps = pss()
                    P.mm(ps[:, 0:bn], bo[:], sqb[:, 0:bn])
                    P.tt("dve", bon[:, 0:bn], ps[:, 0:bn], vs[:, b0 + 1:b0 + 1 + bn], ALU.mult)
                    P.dma("sp", bonusT[ct * 128:(ct + 1) * 128, b0:b0 + bn], bon[:, 0:bn])

    own_ch = list(range(OWN // 128))
    oth_ch = list(range(OWN // 128, S // 128))
    ctx_ch = list(range(S // 128, NCH))
    dir_chunks = [ctx_ch + own_ch, ctx_ch[::-1] + oth_ch[::-1] + own_ch[::-1]]

    if on(5):
        with P.phase():
            idf, idb = load_ident(P)
            mks = [P.sb("mk%d" % d, [128, 384], F32) for d in range(2)]
            msk_d = IN("masks", [2, 128, 384])
            for d in range(2):
                P.dma("sp", mks[d][:], msk_d[d])
            ins = []
            for i in range(2):
                ins.append(dict(
                    ARh=P.sb("cARh%d" % i, [128, 16, 2, 128], BF16), Bt=P.sb("cBt%d" % i, [128, 16, 128], BF16),
                    Kt=P.sb("cKt%d" % i, [128, 16, 128], BF16), Vt=P.sb("cVt%d" % i, [128, RW_W], BF16),
                    Gst=P.sb("cG%d" % i, [128, 32, 128], BF16), Nst=P.sb("cN%d" % i, [128, 32, 128], BF16),
                    Zst=P.sb("cZ%d" % i, [128, 32, 64], F32), Yst=P.sb("cY%d" % i, [128, 16, 128], F32)))
            banks = [dict(AB=P.ps("cAB%d" % i, [128, 512]), CZ=P.ps("cCZ%d" % i, [128, 512]),
                          NN=P.ps("cNN%d" % i, [128, 512]), LL=P.ps("cLL%d" % i, [128, 512])) for i in range(2)]
            XNs = Rot([P.sb("cXN%d" % i, [128, 256], F32) for i in range(2)])
            MNs = Rot([P.sb("cMN%d" % i, [128, 256], BF16) for i in range(2)])
            XGs = Rot([P.sb("cXG%d" % i, [128, 256], F32) for i in range(4)])
            Lbs = Rot([P.sb("cLb%d" % i, [128, 128], F32) for i in range(4)])
            step = 0
            for di in range(2):
                mk = mks[di]
                for c in dir_chunks[di]:
                    I = ins[step % 2]
                    step += 1
                    own = c * 128 < OWN
                    cols = slice(c * 128, (c + 1) * 128)
                    P.dma("sp", I["ARh"][:, :, 0, :], AhT[di].rearrange("(ct p) l -> p ct l", p=128)[:, :, cols])
                    P.dma("sp", I["ARh"][:, :, 1, :], RhT[di].rearrange("(ct p) l -> p ct l", p=128)[:, :, cols])
                    P.dma("sp", I["Bt"][:], BtT[di].rearrange("(ct p) l -> p ct l", p=128)[:, :, cols])
                    P.dma("sp", I["Kt"][:], KtT[di].rearrange("(ct p) l -> p ct l", p=128)[:, :, cols])
                    P.dma("sp", I["Vt"][:], Vtok[c * 128:(c + 1) * 128, :])
                    for h in range(32):
                        ct, half = h // 2, h % 2
                        hp = slice(half * 64, half * 64 + 64)
                        B = banks[h % 2]
                        AB, CZ, NN, LL = B["AB"], B["CZ"], B["NN"], B["LL"]
                        P.mm(AB[:, 0:256], I["Bt"][hp, ct, :], I["ARh"][hp, ct, :, :])
                        P.mm(AB[:, 256:512], I["Kt"][hp, ct, :], I["ARh"][hp, ct, :, :])
                        P.mm(CZ[:, 0:128], I["ARh"][hp, ct, 0, :], I["Bt"][hp, ct, :])
                        XN, MN, Lb = XNs(), MNs(), Lbs()
                        P.tt("dve", XN[:], AB[:, 0:256], mk[:, 0:256], ALU.mult)
                        P.tt("dve", MN[:], AB[:, 256:512], mk[:, 0:256], ALU.mult)
                        P.tt("dve", Lb[:], CZ[:, 0:128], mk[:, 256:384], ALU.mult)
                        hc = slice(h * 64, (h + 1) * 64)
                        P.mm(CZ[:, 128:192], MN[:, 0:128], I["Vt"][:, hc])
                        P.copy("act", I["Zst"][:, h, :], CZ[:, 128:192])
                        if own:
                            P.mm(CZ[hp, 256:384], I["Vt"][:, hc], MN[:, 128:256])
                            P.copy("act", I["Yst"][hp, ct, :], CZ[hp, 256:384])
                            P.copy("act", I["Nst"][:, h, :], XN[:, 128:256])
                        XG = XGs()
                        P.tt("dve", XG[:, 128:256], XN[:, 0:128], idf[:], ALU.add)
                        P.mm(NN[:, 0:128], Lb[:], XN[:, 0:128])
                        P.mm(LL[:, 0:128], XN[:, 0:128], Lb[:])
                        P.copy("act", XG[:, 0:128], NN[:, 0:128])
                        Lc = Lbs()
                        P.copy("act", Lc[:], LL[:, 0:128])
                        for k in range(1, 7):
                            last = (k == 6)
                            if not last:
                                P.mm(NN[:, 0:256], Lc[:], XG[:, 0:256])
                                P.mm(LL[:, 0:128], XG[:, 0:128], Lc[:])
                                XG2 = XGs()
                                P.copy("act", XG2[:, 0:128], NN[:, 0:128])
                                P.tt("dve", XG2[:, 128:256], NN[:, 128:256], XG[:, 128:256], ALU.add)
                                Lc2 = Lbs()
                                P.copy("act", Lc2[:], LL[:, 0:128])
                                XG, Lc = XG2, Lc2
                            else:
                                P.mm(NN[:, 128:256], Lc[:], XG[:, 128:256])
                                P.tt("dve", I["Gst"][:, h, :], NN[:, 128:256], XG[:, 128:256], ALU.add)
                    P.dma("sp", Gs[di][c], I["Gst"][:].rearrange("p h i -> p (h i)"))
                    P.dma("sp", Z0s[di][c], I["Zst"][:].rearrange("p h i -> p (h i)"))
                    if own:
                        P.dma("sp", Ns[di][c], I["Nst"][:].rearrange("p h i -> p (h i)"))
                        P.dma("sp", Y0s[di][c], I["Yst"][:].rearrange("p h i -> p (h i)"))

    if on(6):
        with P.phase():
            ST = P.sb("ST", [128, 16, 64], F32)
            STz = P.sb("STz", [128, 16, 2, 64], BF16)
            wcs = P.sb("swc", [128, 16, NCH], F32)
            ins = []
            for i in range(2):
                ins.append(dict(
                    ARh=P.sb("sARh%d" % i, [128, 16, 2, 128], BF16), G=P.sb("sG%d" % i, [128, 32, 128], BF16),
                    N=P.sb("sN%d" % i, [128, 32, 128], BF16), Z0=P.sb("sZ%d" % i, [128, 32, 64], F32),
                    Y0=P.sb("sY%d" % i, [128, 16, 128], F32), Bt=P.sb("sBt%d" % i, [128, RW_W], BF16),
                    Kt=P.sb("sKt%d" % i, [128, RW_W], BF16), Vt=P.sb("sVt%d" % i, [128, RW_W], BF16)))
            Zb = P.sb("sZb", [128, 32, 64], BF16)
            Ub = P.sb("sUb", [128, 32, 64], BF16)
            yts = Rot([P.sb("syt%d" % i, [128, 16, 128], F32) for i in range(2)])
            pz = Rot([P.ps("spz%d" % i, [128, 512]) for i in range(2)])
            pu = Rot([P.ps("spu%d" % i, [128, 512]) for i in range(2)])
            py = Rot([P.ps("spy%d" % i, [128, 512]) for i in range(2)])
            psb = Rot([P.ps("sps%d" % i, [128, 512]) for i in range(2)])
            step = 0
            for di in range(2):
                P.memset("dve", ST[:], 0.0)
                P.memset("pool", STz[:], 0.0)
                P.dma("sp", wcs[:], Wc[di].rearrange("(ct p) c -> p ct c", p=128))
                for c in dir_chunks[di]:
                    I = ins[step % 2]
                    step += 1
                    own = c * 128 < OWN
                    cols = slice(c * 128, (c + 1) * 128)
                    P.dma("sp", I["ARh"][:, :, 0, :], AhT[di].rearrange("(ct p) l -> p ct l", p=128)[:, :, cols])
                    P.dma("sp", I["G"][:].rearrange("p h i -> p (h i)"), Gs[di][c])
                    P.dma("sp", I["Z0"][:].rearrange("p h i -> p (h i)"), Z0s[di][c])
                    P.dma("sp", I["Bt"][:], Btok[di][c * 128:(c + 1) * 128, :])
                    P.dma("sp", I["Kt"][:], Ktok[di][c * 128:(c + 1) * 128, :])
                    P.dma("sp", I["Vt"][:], Vtok[c * 128:(c + 1) * 128, :])
                    if own:
                        P.dma("sp", I["ARh"][:, :, 1, :], RhT[di].rearrange("(ct p) l -> p ct l", p=128)[:, :, cols])
                        P.dma("sp", I["N"][:].rearrange("p h i -> p (h i)"), Ns[di][c])
                        P.dma("sp", I["Y0"][:].rearrange("p h i -> p (h i)"), Y0s[di][c])
                    for g in range(4):
                        p_ = pz()
                        for hh in range(8):
                            h = g * 8 + hh
                            ct, hp = h // 2, slice((h % 2) * 64, (h % 2) * 64 + 64)
                            P.mm(p_[:, hh * 64:(hh + 1) * 64], I["ARh"][:, ct, 0, :], STz[:, ct, h % 2, :])
                        P.tt("dve", Zb[:, g * 8:(g + 1) * 8, :], p_[:].rearrange("p (h v) -> p h v", v=64), I["Z0"][:, g * 8:(g + 1) * 8, :], ALU.add)
                    for g in range(4):
                        p_ = pu()
                        for hh in range(8):
                            h = g * 8 + hh
                            P.mm(p_[:, hh * 64:(hh + 1) * 64], I["G"][:, h, :], Zb[:, h, :])
                        P.copy("act", Ub[:, g * 8:(g + 1) * 8, :], p_[:].rearrange("p (h v) -> p h v", v=64))
                    if own:
                        yt = yts()
                        for g in range(4):
                            p_ = py()
                            for c4 in range(4):
                                ct = g * 4 + c4
                                for half in range(2):
                                    h = ct * 2 + half
                                    hp = slice(half * 64, half * 64 + 64)
                                    P.mm(p_[hp, c4 * 128:(c4 + 1) * 128], STz[:, ct, half, :], I["ARh"][:, ct, 1, :], start=True, stop=False)
                                    P.mm(p_[hp, c4 * 128:(c4 + 1) * 128], Ub[:, h, :], I["N"][:, h, :], start=False, stop=True)
                            P.tt("dve", yt[:, g * 4:(g + 1) * 4, :], p_[:].rearrange("p (c i) -> p c i", i=128), I["Y0"][:, g * 4:(g + 1) * 4, :], ALU.add)
                        P.dma("sp", ysc[di].rearrange("(ct p) l -> p ct l", p=128)[:, :, cols], yt[:])
                    for g in range(2):
                        p_ = psb()
                        for c8 in range(8):
                            ct = g * 8 + c8
                            for half in range(2):
                                h = ct * 2 + half
                                hp = slice(half * 64, half * 64 + 64)
                                hc = slice(h * 64, (h + 1) * 64)
                                P.mm(p_[hp, c8 * 64:(c8 + 1) * 64], I["Bt"][:, hc], Ub[:, h, :], start=True, stop=False)
                                P.mm(p_[hp, c8 * 64:(c8 + 1) * 64], I["Kt"][:, hc], I["Vt"][:, hc], start=False, stop=True)
                        P.tt("dve", ST[:, g * 8:(g + 1) * 8, :], p_[:].rearrange("p (c v) -> p c v", v=64), ST[:, g * 8:(g + 1) * 8, :], ALU.add)
                    P.tt("dve", ST[:], ST[:], wcs[:, :, c:c + 1].to_broadcast([128, 16, 64]), ALU.mult)
                    P.copy("act", STz[0:64, :, 0, :], ST[0:64, :, :])
                    P.copy("act", STz[64:128, :, 1, :], ST[64:128, :, :])

    if on(7):
        with P.phase():
            chp = P.sb("chp", [128, 16, NPAR], F32)
            P.dma("sp", chp[:], IN("chp", [128, 16, NPAR])[:, :, :])
            bo = P.sb("bo", [128, 128], F32)
            P.dma("sp", bo[:], IN("bo", [128, 128])[:, :])
            bo64 = P.sb("bo64", [128, 128], F32)
            P.ts("dve", bo64[:], bo[:], 1.0 / 64, ALU.mult)
            g2b = P.sb("g2b", [128, 4, RW_W], BF16)
            g2d = IN("rw_g2", [480, RW_W])
            for gi in range(4):
                rn = 128 if gi < 3 else 96
                P.dma("pool", g2b[0:rn, gi, :], g2d[gi * 128:gi * 128 + rn, :])
            sgb = P.sb("sgb", [128, 4, OWN], BF16)
            P.dma("sp", sgb[:], sgT.rearrange("(g p) t -> p g t", p=128))
            pss = Rot([P.ps("op%d" % i, [128, 512]) for i in range(4)])
            ya = Rot([P.sb("ya%d" % i, [128, 512], F32) for i in range(2)])
            yb = Rot([P.sb("yb%d" % i, [128, 512], F32) for i in range(2)])
            bn_ = Rot([P.sb("obn%d" % i, [128, 512], F32) for i in range(2)])
            yc = P.sb("yc", [128, 512], F32)
            sq = P.sb("osq", [128, 512], F32)
            rsd = P.sb("rsd", [128, 512], F32)
            ob = Rot([P.sb("oob%d" % i, [128, 512], BF16) for i in range(2)])
            for ct in range(16):
                rows = slice(ct * 128, (ct + 1) * 128)
                for b0, bn in splits(0, OWN, 512):
                    y0, y1, bt = ya(), yb(), bn_()
                    P.dma("sp", y0[:, 0:bn], ysc[0][rows, b0:b0 + bn])
                    P.dma("sp", y1[:, 0:bn], ysc[1][rows, b0:b0 + bn])
                    P.dma("sp", bt[:, 0:bn], bonusT[rows, b0:b0 + bn])
                    P.tt("dve", y0[:, 0:bn], y0[:, 0:bn], y1[:, 0:bn], ALU.add)
                    pm_ = pss()
                    P.mm(pm_[:, 0:bn], bo64[:], y0[:, 0:bn])
                    P.tt("dve", yc[:, 0:bn], y0[:, 0:bn], pm_[:, 0:bn], ALU.subtract)
                    P.tt("pool", sq[:, 0:bn], yc[:, 0:bn], yc[:, 0:bn], ALU.mult)
                    pv = pss()
                    P.mm(pv[:, 0:bn], bo64[:], sq[:, 0:bn])
                    P.ts("dve", rsd[:, 0:bn], pv[:, 0:bn], GN_EPS, ALU.add)
                    P.act(rsd[:, 0:bn], rsd[:, 0:bn], AF.Sqrt)
                    P.recip(rsd[:, 0:bn], rsd[:, 0:bn])
                    P.tt("dve", yc[:, 0:bn], yc[:, 0:bn], rsd[:, 0:bn], ALU.mult)
                    P.ts("dve", yc[:, 0:bn], yc[:, 0:bn], chp[:, ct, PLNW:PLNW + 1], ALU.mult, chp[:, ct, PLNB:PLNB + 1], ALU.add)
                    P.tt("dve", yc[:, 0:bn], yc[:, 0:bn], bt[:, 0:bn], ALU.add)
                    pg = pss()
                    for gi in range(4):
                        rn = 128 if gi < 3 else 96
                        P.mm(pg[:, 0:bn], g2b[0:rn, gi, rows], sgb[0:rn, gi, b0:b0 + bn], start=(gi == 0), stop=(gi == 3))
                    o_ = ob()
                    P.tt("dve", o_[:, 0:bn], yc[:, 0:bn], pg[:, 0:bn], ALU.mult)
                    P.dma("sp", oT[DA_W + ct * 128:DA_W + (ct + 1) * 128, b0:b0 + bn], o_[:, 0:bn])

    if on(8):
        with P.phase():
            w_out = IN("w_out", [D, D])
            wv = w_out.rearrange("(k p) n -> p k n", p=128)
            oTv = oT.rearrange("(k p) t -> p k t", p=128)
            oTb = P.sb("oTb", [128, KC, 1024], BF16)
            wbs = Rot([P.sb("wo%d" % i, [128, KC, 512], BF16) for i in range(2)])
            pss = Rot([P.ps("wp%d" % i, [128, 512]) for i in range(4)])
            obs = Rot([P.sb("wob%d" % i, [128, 512], F32) for i in range(3)])
            for t0, tn in splits(0, OWN, 1024):
                P.dma("sp", oTb[:, :, 0:tn], oTv[:, :, t0:t0 + tn])
                for c0, cn in splits(0, D, 512):
                    wb = wbs()
                    P.dma("pool", wb[:], wv[:, :, c0:c0 + cn])
                    for tt0, _ in splits(0, tn, 128):
                        ps = pss()
                        for kc in range(KC):
                            P.mm(ps[:], oTb[:, kc, tt0:tt0 + 128], wb[:, kc, :], start=(kc == 0), stop=(kc == KC - 1))
                        o_ = obs()
                        P.copy("act", o_[:], ps[:])
                        P.dma("sp", olat[t0 + tt0:t0 + tt0 + 128, c0:c0 + cn], o_[:])

    if on(9):
        with P.phase():
            x_l = IN("x_l", [L, D])
            idf, idb = load_ident(P)
            G2 = P.sb("G2", [128, D], F32)
            A2 = P.sb("A2", [128, D], F32)
            Sh2 = P.sb("Sh2", [128, D], F32)
            tmp = P.sb("tmp", [128, D], F32)
            P.dma("sp", G2[:], modscr[0:1, 2 * D:3 * D].partition_broadcast(128))
            P.dma("sp", tmp[:], IN("g_post_attn", [1, D])[0:1, :].partition_broadcast(128))
            P.tt("dve", G2[:], G2[:], tmp[:], ALU.mult)
            P.dma("sp", Sh2[:], modscr[0:1, 3 * D:4 * D].partition_broadcast(128))
            P.dma("sp", A2[:], modscr[0:1, 4 * D:5 * D].partition_broadcast(128))
            P.dma("sp", tmp[:], IN("g_pre_ffn", [1, D])[0:1, :].partition_broadcast(128))
            P.stt(A2[:], A2[:], 1.0, tmp[:], ALU.add, ALU.mult)
            rwf = P.sb("rwf", [128, KC, NE], F32)
            P.dma("sp", rwf[:], IN("router_w", [D, NE]).rearrange("(k p) e -> p k e", p=128))
            rbias = P.sb("rbias", [128, NE], F32)
            P.dma("sp", rbias[:], IN("router_bias", [1, NE])[0:1, :].partition_broadcast(128))
            xt = P.sb("xt", [128, D], F32)
            ol = P.sb("ol", [128, D], F32)
            x1 = P.sb("x1", [128, D], F32)
            hf = ol
            hb = P.sb("hb", [128, D], BF16)
            junk = P.sb("junk", [128, D], BF16)
            hTf = P.sb("hTf", [128, KC, 128], F32)
            hTb = P.sb("hTb", [128, KC, 128], BF16)
            st = P.sb("st", [128, 8], F32)
            ptb = Rot([P.ps("rtb%d" % i, [128, 1024], BF16) for i in range(2)])
            ptf = Rot([P.ps("rtf%d" % i, [128, 512]) for i in range(3)])
            prr = P.ps("prr", [128, 512])
            R = {n: P.sb("r_" + n, [128, NE], F32) for n in ("sc", "bi", "eq", "mk", "mb", "sel", "ga")}
            r8 = {n: P.sb("r8_" + n, [128, 8], F32) for n in ("m1", "m2", "gs", "srt", "gm", "pen", "srt2", "den")}
            gT = P.sb("gT", [128, 128], F32)
            for ti in range(OWN // 128):
                rowsl = slice(ti * 128, (ti + 1) * 128)
                P.dma("sp", xt[:], x_l[rowsl, :])
                P.dma("sp", ol[:], olat[rowsl, :])
                rms_stats(P, st, ol[:], junk[:], D)
                P.stt(tmp[:], ol[:], st[:, 3:4], G2[:], ALU.mult, ALU.mult)
                P.tt("pool", x1[:], tmp[:], xt[:], ALU.add)
                P.dma("sp", x1s[rowsl, :], x1[:])
                rms_stats(P, st[:, 4:8], x1[:], junk[:], D)
                P.stt(tmp[:], x1[:], st[:, 7:8], A2[:], ALU.mult, ALU.mult)
                P.tt("dve", hf[:], tmp[:], Sh2[:], ALU.add)
                P.copy("pool", hb[:], hf[:])
                for q4 in range(4):
                    pt = ptb()
                    for k8 in range(8):
                        kc = q4 * 8 + k8
                        P.tr(pt[:, k8 * 128:(k8 + 1) * 128], hb[:, kc * 128:(kc + 1) * 128], idb[:])
                    P.copy("act", hTb[:, q4 * 8:(q4 + 1) * 8, :], pt[:].rearrange("p (k t) -> p k t", k=8))
                P.dma("sp", hTs.rearrange("(k p) t -> p k t", p=128)[:, :, rowsl], hTb[:])
                for q8 in range(8):
                    pt = ptf()
                    for k4 in range(4):
                        kc = q8 * 4 + k4
                        P.tr(pt[:, k4 * 128:(k4 + 1) * 128], hf[:, kc * 128:(kc + 1) * 128], idf[:])
                    P.copy("act" if q8 % 2 else "dve", hTf[:, q8 * 4:(q8 + 1) * 4, :], pt[:].rearrange("p (k t) -> p k t", k=4))
                for kc in range(KC):
                    P.mm(prr[:, 0:NE], hTf[:, kc, :], rwf[:, kc, :], start=(kc == 0), stop=(kc == KC - 1))
                P.act(R["sc"][:], prr[:, 0:NE], AF.Sigmoid)
                P.tt("dve", R["bi"][:], R["sc"][:], rbias[:], ALU.add)
                bv = R["bi"][:].rearrange("p (g s) -> p g s", s=GS)
                P.red(r8["m1"][:], bv, ALU.max)
                P.tt("dve", R["eq"][:].rearrange("p (g s) -> p g s", s=GS), bv, r8["m1"][:].unsqueeze(2).to_broadcast([128, NG, GS]), ALU.is_equal)
                P.stt(R["mk"][:], R["eq"][:], -1e9, R["bi"][:], ALU.mult, ALU.add)
                P.red(r8["m2"][:], R["mk"][:].rearrange("p (g s) -> p g s", s=GS), ALU.max)
                P.tt("dve", r8["gs"][:], r8["m1"][:], r8["m2"][:], ALU.add)
                P.max8(r8["srt"][:], r8["gs"][:])
                P.ts("dve", r8["gm"][:], r8["gs"][:], r8["srt"][:, 3:4], ALU.is_ge)
                P.ts("dve", r8["pen"][:], r8["gm"][:], -1.0, ALU.add, 1e9, ALU.mult)
                P.tt("dve", R["mb"][:].rearrange("p (g s) -> p g s", s=GS), bv, r8["pen"][:].unsqueeze(2).to_broadcast([128, NG, GS]), ALU.add)
                P.max8(r8["srt2"][:], R["mb"][:])
                P.ts("dve", R["sel"][:], R["mb"][:], r8["srt2"][:, 5:6], ALU.is_ge)
                P.tt("dve", R["ga"][:], R["sc"][:], R["sel"][:], ALU.mult)
                P.red(r8["den"][:, 0:1], R["ga"][:], ALU.add)
                P.recip(r8["den"][:, 1:2], r8["den"][:, 0:1])
                P.ts("dve", R["ga"][:], R["ga"][:], r8["den"][:, 1:2], ALU.mult, 2.5, ALU.mult)
                pt = ptf()
                P.tr(pt[0:NE, 0:128], R["ga"][:], idf[:])
                P.copy("act", gT[0:NE, :], pt[0:NE, 0:128])
                P.dma("sp", gscT[:, rowsl], gT[0:NE, :])

    if on(10):
        with P.phase():
            w1a = IN("w1all", [NE1, D, FF])
            w3a = IN("w3all", [NE1, D, FF])
            hTv = hTs.rearrange("(k p) t -> p k t", p=128)
            hTb = P.sb("ehT", [128, KC, 1024], BF16)
            w1s = Rot([P.sb("ew1%d" % i, [128, KC, 256], BF16) for i in range(2)])
            w3s = Rot([P.sb("ew3%d" % i, [128, KC, 256], BF16) for i in range(2)])
            gbs = Rot([P.sb("egb%d" % i, [128, 1024], F32) for i in range(2)])
            p1s = Rot([P.ps("ep1%d" % i, [128, 512]) for i in range(3)])
            p3s = Rot([P.ps("ep3%d" % i, [128, 512]) for i in range(3)])
            sls = Rot([P.sb("esl%d" % i, [128, 512], F32) for i in range(2)])
            hms = Rot([P.sb("ehm%d" % i, [128, 512], F32) for i in range(2)])
            hos = Rot([P.sb("eho%d" % i, [128, 512], BF16) for i in range(3)])
            for t0, tn in splits(0, OWN, 1024):
                P.dma("sp", hTb[:, :, 0:tn], hTv[:, :, t0:t0 + tn])
                for e in range(NE1):
                    gb = None
                    if e < NE:
                        gb = gbs()
                        P.dma("sp", gb[:, 0:tn], gscT[e:e + 1, t0:t0 + tn].partition_broadcast(128))
                    for f0 in (0, 256):
                        w1, w3 = w1s(), w3s()
                        P.dma("pool", w1[:], w1a[e].rearrange("(k p) f -> p k f", p=128)[:, :, f0:f0 + 256])
                        P.dma("pool", w3[:], w3a[e].rearrange("(k p) f -> p k f", p=128)[:, :, f0:f0 + 256])
                        for fh in range(2):
                            fs = slice(fh * 128, (fh + 1) * 128)
                            for s0, sn in splits(0, tn, 512):
                                p1, p3 = p1s(), p3s()
                                for kc in range(KC):
                                    P.mm(p1[:, 0:sn], w1[:, kc, fs], hTb[:, kc, s0:s0 + sn], start=(kc == 0), stop=(kc == KC - 1))
                                for kc in range(KC):
                                    P.mm(p3[:, 0:sn], w3[:, kc, fs], hTb[:, kc, s0:s0 + sn], start=(kc == 0), stop=(kc == KC - 1))
                                sl, ho = sls(), hos()
                                P.act(sl[:, 0:sn], p1[:, 0:sn], AF.Silu)
                                if gb is None:
                                    P.tt("dve", ho[:, 0:sn], sl[:, 0:sn], p3[:, 0:sn], ALU.mult)
                                else:
                                    hm = hms()
                                    P.tt("dve", hm[:, 0:sn], sl[:, 0:sn], p3[:, 0:sn], ALU.mult)
                                    P.tt("pool", ho[:, 0:sn], hm[:, 0:sn], gb[:, s0:s0 + sn], ALU.mult)
                                r0 = e * FF + f0 + fh * 128
                                P.dma("sp", hbs[r0:r0 + 128, t0 + s0:t0 + s0 + sn], ho[:, 0:sn])

    if on(11):
        with P.phase():
            w2a = IN("w2all", [NE1 * FF, D])
            NCK = NE1 * FF // 128
            GK = 20
            hbv = hbs.rearrange("(c p) t -> p c t", p=128)
            w2v = w2a.rearrange("(c p) n -> p c n", p=128)
            hbg = Rot([P.sb("dhb%d" % i, [128, GK, 1024], BF16) for i in range(2)])
            wbs = Rot([P.sb("dw%d" % i, [128, GK, 512], BF16) for i in range(2)])
            yacc = P.sb("yacc", [128, 8, 512], F32)
            pss = Rot([P.ps("dp%d" % i, [128, 512]) for i in range(4)])
            for t0, tn in splits(0, OWN, 1024):
                ntt = tn // 128
                for c0, cn in splits(0, D, 512):
                    for gi, (k0, kn) in enumerate(splits(0, NCK, GK)):
                        hg, wb = hbg(), wbs()
                        P.dma("sp", hg[:, 0:kn, 0:tn], hbv[:, k0:k0 + kn, t0:t0 + tn])
                        P.dma("pool", wb[:, 0:kn, :], w2v[:, k0:k0 + kn, c0:c0 + cn])
                        for tt in range(ntt):
                            ps = pss()
                            for k in range(kn):
                                P.mm(ps[:], hg[:, k, tt * 128:(tt + 1) * 128], wb[:, k, :], start=(k == 0), stop=(k == kn - 1))
                            if gi == 0:
                                P.copy("act", yacc[:, tt, :], ps[:])
                            else:
                                P.tt("dve", yacc[:, tt, :], ps[:], yacc[:, tt, :], ALU.add)
                    P.dma("sp", ymoe[t0:t0 + tn, c0:c0 + cn].rearrange("(t p) n -> p t n", p=128), yacc[:, 0:ntt, :])

    if on(12):
        with P.phase():
            G5 = P.sb("G5", [128, D], F32)
            tmp = P.sb("tmp", [128, D], F32)
            P.dma("sp", G5[:], modscr[0:1, 5 * D:6 * D].partition_broadcast(128))
            P.dma("sp", tmp[:], IN("g_post_ffn", [1, D])[0:1, :].partition_broadcast(128))
            P.tt("dve", G5[:], G5[:], tmp[:], ALU.mult)
            yms = Rot([P.sb("ym%d" % i, [128, D], F32) for i in range(2)])
            x1b = Rot([P.sb("x1b%d" % i, [128, D], F32) for i in range(2)])
            ots = Rot([P.sb("ot%d" % i, [128, D], F32) for i in range(2)])
            junk = P.sb("junk", [128, D], BF16)
            sts = Rot([P.sb("fst%d" % i, [128, 4], F32) for i in range(2)])
            for ti in range(OWN // 128):
                rowsl = slice(ti * 128, (ti + 1) * 128)
                ym, x1, ot, st = yms(), x1b(), ots(), sts()
                P.dma("sp", ym[:], ymoe[rowsl, :])
                P.dma("sp", x1[:], x1s[rowsl, :])
                rms_stats(P, st, ym[:], junk[:], D)
                P.stt(tmp[:], ym[:], st[:, 3:4], G5[:], ALU.mult, ALU.mult)
                P.tt("pool", ot[:], tmp[:], x1[:], ALU.add)
                P.dma("sp", out_d[rowsl, :], ot[:])
    elif cfg.get("dummy_out", True):
        with P.phase():
            z = P.sb("z", [128, 64], F32)
            P.memset("dve", z[:], 0.0)
            P.dma("sp", out_d[0:128, 0:64], z[:])
    P.close()
    P.in_names = in_names
    return nc, P


def qk_perm():
    idx = np.arange(IN_COLS)
    blk = np.concatenate([np.arange(0, 128, 2), np.arange(1, 128, 2)])
    for base in range(0, 2 * DA_W, 128):
        idx[base:base + 128] = base + blk
    return idx


def rope_tables(cfg, j):
    S, CTX, GW = cfg["S"], cfg["CTX"], cfg["GW"]
    L = S + CTX
    l = np.arange(S)
    t = l if j == 0 else (S - 1 - l)
    row = (t // GW).astype(np.float32)
    col = (t % GW).astype(np.float32)
    inv = np.power(np.float32(10000.0), -np.arange(32, dtype=np.float32) / np.float32(32)).astype(np.float32)
    ang = np.concatenate([row[:, None] * inv, col[:, None] * inv], axis=-1).astype(np.float32)
    cos, sin = np.cos(ang).astype(np.float32), np.sin(ang).astype(np.float32)
    cosT = np.ones((128, L), np.float32)
    sinT = np.zeros((128, L), np.float32)
    cosT[0:64, :S] = cos.T
    cosT[64:128, :S] = cos.T
    sinT[0:64, :S] = -sin.T
    sinT[64:128, :S] = sin.T
    return cosT, sinT


def chunk_masks():
    i = np.arange(128)
    m = np.zeros((2, 128, 384), np.float32)
    r, c = i[:, None], i[None, :]
    m[0, :, 0:128] = (c > r)
    m[0, :, 128:256] = (c >= r)
    m[0, :, 256:384] = (r > c)
    m[1, :, 0:128] = (c < r)
    m[1, :, 128:256] = (c <= r)
    m[1, :, 256:384] = (r < c)
    return m


def host_inputs(inputs, cfg, names=None):
    S, CTX = cfg["S"], cfg["CTX"]
    g = lambda k: np.asarray(inputs[k]) if k in inputs else None
    want = (lambda n: True) if names is None else (lambda n: n in names)
    shared = {}
    if want("w_in"):
        shared["w_in"] = np.ascontiguousarray(g("w_in")[0][:, qk_perm()])
    for k in ("w_mod", "w_out", "rw_g2", "router_w"):
        if want(k):
            shared[k] = np.ascontiguousarray(g(k)[0])
    for k in ("g_pre_attn", "g_post_attn", "g_pre_ffn", "g_post_ffn", "da_subln", "router_bias"):
        if want(k):
            shared[k] = np.ascontiguousarray(g(k).reshape(1, -1))
    if want("b_mod"):
        shared["b_mod"] = np.ascontiguousarray(g("b_mod")[0][None, :])
    if want("da_lambda"):
        shared["da_lambda"] = np.ascontiguousarray(g("da_lambda")[0].reshape(1, 512))
    if want("w1all"):
        shared["w1all"] = np.concatenate([g("exp_w1")[0], g("sh_w1")], axis=0)
    if want("w3all"):
        shared["w3all"] = np.concatenate([g("exp_w3")[0], g("sh_w3")], axis=0)
    if want("w2all"):
        e2 = g("exp_w2")[0]
        shared["w2all"] = np.concatenate([e2.reshape(-1, D), g("sh_w2")[0]], axis=0)
    shared["ident"] = np.eye(128, dtype=np.float32)
    pm = np.zeros((128, 128), np.float32)
    for m in range(128):
        pm[(m + 64) % 128, m] = 1.0
    shared["pm"] = pm
    bo = np.zeros((128, 128), np.float32)
    bo[:64, :64] = 1.0
    bo[64:, 64:] = 1.0
    shared["bo"] = bo
    shared["masks"] = chunk_masks()
    maps = []
    for c in range(cfg.get("NCORES", 8)):
        b, j = c // 2, c % 2
        m = dict(shared)
        if want("x_l"):
            xb, cb = g("x")[b], g("ctx")[b]
            if j == 1:
                xb, cb = xb[::-1], cb[::-1]
            m["x_l"] = np.ascontiguousarray(np.concatenate([xb, cb], axis=0))
        if want("cvec"):
            cv = np.stack([g("c")[b], g("c_ctx")], axis=-1)
            m["cvec"] = np.ascontiguousarray(cv.reshape(KC, 128, 2).transpose(1, 0, 2))
        if want("cosT") or want("sinT"):
            m["cosT"], m["sinT"] = rope_tables(cfg, j)
        dsel = [j, 1 - j]
        if want("shAB"):
            sh = g("rw_shift")[0]
            ab = np.zeros((NRT * 128, 2), np.float32)
            ab[:RW_COLS, 0] = sh[dsel[0]]
            ab[:RW_COLS, 1] = sh[dsel[1]]
            m["shAB"] = np.ascontiguousarray(ab.reshape(NRT, 128, 2).transpose(1, 0, 2))
        if want("chp"):
            cp = np.zeros((RW_W, NPAR), np.float32)
            cp[:, PK_K] = g("rw_k_k")[0]
            cp[:, PK_A] = g("rw_k_a")[0]
            cp[:, PR_K] = g("rw_r_k")[0].reshape(-1)
            cp[:, PLNW] = g("rw_ln_w")[0]
            cp[:, PLNB] = g("rw_ln_b")[0]
            for i in range(2):
                cp[:, PW0 + i] = g("rw_w0")[0][dsel[i]]
                cp[:, PA0 + i] = g("rw_a0")[0][dsel[i]]
            m["chp"] = np.ascontiguousarray(cp.reshape(16, 128, NPAR).transpose(1, 0, 2))
        if want("rw_w2d"):
            m["rw_w2d"] = np.ascontiguousarray(g("rw_w2")[0][dsel])
        if want("rw_a2d"):
            m["rw_a2d"] = np.ascontiguousarray(g("rw_a2")[0][dsel])
        if names is not None:
            m = {k: v for k, v in m.items() if k in names}
        maps.append(m)
    return maps


def assemble(results, cfg):
    S = cfg["S"]
    OWN = S // 2
    out = np.zeros((cfg["B"], S, D), np.float32)
    for c, r in enumerate(results):
        b, j = c // 2, c % 2
        o = r["out"]
        if j == 0:
            out[b, :OWN] = o
        else:
            out[b, S - 1 - np.arange(OWN)] = o
    return out


def kernel(**inputs):
    cfg = FULL_CFG
    nc, P = build_program(cfg)
    maps = host_inputs(inputs, cfg, names=set(P.in_names))
    res = run_bass_kernel_spmd(nc, maps, core_ids=list(range(8)))
    return assemble(res.results, cfg)
```

```python
import numpy as np
from contextlib import ExitStack
import concourse.bass as bass
import concourse.mybir as mybir
from concourse.bass_utils import run_bass_kernel_spmd

F32 = mybir.dt.float32
BF16 = mybir.dt.bfloat16
F32R = mybir.dt.float32r
AF = mybir.ActivationFunctionType
ALU = mybir.AluOpType
AX = mybir.AxisListType

D = 4096
KC = D // 128
N_MOD = 6
DA_W = 2048
RW_W = 2048
RW_COLS = 3 * RW_W + 128 + 128 + 480
IN_COLS = 3 * DA_W + RW_COLS
NORM_EPS = 1e-6
GN_EPS = 64e-5
LAM_INIT = 0.2
N_EXP = 64
FF = 512

FULL_CFG = dict(B=4, S=4096, CTX=256, GW=64)


class Buf:
    __slots__ = ("name", "lw", "rd")

    def __init__(self, name):
        self.name = name
        self.lw = None
        self.rd = []


class Op:
    __slots__ = ("eng", "fn", "r", "w", "dma", "need", "sig", "eidx", "fn_deps")

    def __init__(self, eng, fn, r, w, dma):
        self.eng, self.fn, self.r, self.w, self.dma = eng, fn, r, w, dma
        self.need = False
        self.sig = None
        self.eidx = 0


class Prog:
    ENGS = ("pe", "act", "dve", "pool", "sp")
    RING = {"sp": 8, "pool": 6, "act": 4}

    def __init__(self, nc):
        self.nc = nc
        self.e = {"pe": nc.tensor, "act": nc.scalar, "dve": nc.vector, "pool": nc.gpsimd, "sp": nc.sync}
        self.es = ExitStack()
        self.sems = []

        def mk(name):
            h = self.es.enter_context(nc.semaphore(name))
            self.sems.append(h)
            return len(self.sems) - 1

        self.csem = {e: mk("c_" + e) for e in ("pe", "act", "dve", "pool")}
        self.ccnt = {e: 0 for e in self.csem}
        self.ring = {q: [mk("d_%s%d" % (q, i)) for i in range(n)] for q, n in self.RING.items()}
        self.ringval = {q: [0] * n for q, n in self.RING.items()}
        self.dcnt = {q: 0 for q in self.RING}
        self.waited = {e: {} for e in self.ENGS}
        self.ops = []
        self.reg = {}
        self.pstack = None
        self.ecount = {e: 0 for e in self.ENGS}
        self.n_inst = 0
        self.pid = 0

    def sb(self, name, shape, dtype):
        name = "p%d_%s" % (self.pid, name)
        t = self.pstack.enter_context(self.nc.sbuf_tensor(name, list(shape), dtype))
        self.reg[name] = Buf(name)
        return t

    def ps(self, name, shape, dtype=F32):
        name = "p%d_%s" % (self.pid, name)
        t = self.pstack.enter_context(self.nc.psum_tensor(name, list(shape), dtype))
        self.reg[name] = Buf(name)
        return t

    def _bufs(self, aps):
        out = []
        for a in aps:
            if a is None or isinstance(a, (int, float)):
                continue
            b = self.reg.get(a.tensor.name)
            if b is not None and b not in out:
                out.append(b)
        return out

    def add(self, eng, fn, outs, ins, dma=False):
        self.ops.append(Op(eng, fn, self._bufs(ins), self._bufs(outs), dma))

    def dma(self, q, out, in_, slow=False):
        e = self.e[q]
        if slow:
            self.add(q, lambda: e.dma_start(out=out, in_=in_, allow_slow_non_contiguous=True), [out], [in_], dma=True)
        else:
            self.add(q, lambda: e.dma_start(out=out, in_=in_), [out], [in_], dma=True)

    def mm(self, out, lhsT, rhs, start=True, stop=True):
        self.add("pe", lambda: self.nc.tensor.matmul(out, lhsT=lhsT, rhs=rhs, start=start, stop=stop), [out], [lhsT, rhs])

    def tr(self, out, in_, ident):
        self.add("pe", lambda: self.nc.tensor.transpose(out, in_, ident), [out], [in_, ident])

    def act(self, out, in_, func, bias=None, scale=None, accum_out=None):
        kw = {}
        if bias is not None:
            kw["bias"] = bias
        if scale is not None:
            kw["scale"] = scale
        if accum_out is not None:
            kw["accum_out"] = accum_out
        self.add("act", lambda: self.nc.scalar.activation(out=out, in_=in_, func=func, **kw),
                 [out, accum_out], [in_, bias, scale])

    def tt(self, eng, out, in0, in1, op):
        e = self.e[eng]
        self.add(eng, lambda: e.tensor_tensor(out=out, in0=in0, in1=in1, op=op), [out], [in0, in1])

    def ts(self, eng, out, in0, s1, op0, s2=None, op1=None, accum_out=None):
        e = self.e[eng]
        kw = {}
        if op1 is not None:
            kw["op1"] = op1
        if accum_out is not None:
            kw["accum_out"] = accum_out
        self.add(eng, lambda: e.tensor_scalar(out=out, in0=in0, scalar1=s1, scalar2=s2, op0=op0, **kw),
                 [out, accum_out], [in0, s1, s2])

    def stt(self, out, in0, scalar, in1, op0, op1):
        self.add("dve", lambda: self.nc.vector.scalar_tensor_tensor(out=out, in0=in0, scalar=scalar, in1=in1, op0=op0, op1=op1),
                 [out], [in0, scalar, in1])

    def copy(self, eng, out, in_):
        if eng == "act":
            self.add("act", lambda: self.nc.scalar.copy(out=out, in_=in_), [out], [in_])
        else:
            e = self.e[eng]
            self.add(eng, lambda: e.tensor_copy(out=out, in_=in_), [out], [in_])

    def red(self, out, in_, op, axis=None, negate=None):
        ax = AX.X if axis is None else axis
        kw = {}
        if negate is not None:
            kw["negate"] = negate
        self.add("dve", lambda: self.nc.vector.tensor_reduce(out=out, in_=in_, axis=ax, op=op, **kw), [out], [in_])

    def recip(self, out, in_):
        self.add("dve", lambda: self.nc.vector.reciprocal(out=out, in_=in_), [out], [in_])

    def memset(self, eng, ap, val):
        e = self.e[eng]
        self.add(eng, lambda: e.memset(ap, val), [ap], [])

    def scan(self, out, d0, d1, initial, op0, op1):
        self.add("dve", lambda: self.nc.vector.tensor_tensor_scan(out=out, data0=d0, data1=d1, initial=initial, op0=op0, op1=op1),
                 [out], [d0, d1, initial])

    def max8(self, out, in_):
        self.add("dve", lambda: self.nc.vector.max(out=out, in_=in_), [out], [in_])

    def phase(self):
        prog = self

        class _Ph:
            def __enter__(s):
                prog.pid += 1
                prog.pstack = ExitStack()
                prog.pstack.__enter__()
                return prog

            def __exit__(s, et, ev, tb):
                if et is None:
                    prog.flush()
                prog.pstack.__exit__(et, ev, tb)
                prog.pstack = None
                prog.reg = {}
                return False

        return _Ph()

    def _wait(self, eng, sem, val):
        if val <= 0:
            return
        if self.waited[eng].get(sem, 0) >= val:
            return
        self.e[eng].wait_ge(self.sems[sem], val)
        self.waited[eng][sem] = val
        self.n_inst += 1

    def flush(self):
        ops = self.ops
        self.ops = []
        ecnt = dict(self.ecount)
        last_compute = {}
        for op in ops:
            ecnt[op.eng] += 1
            op.eidx = ecnt[op.eng]
            deps = []
            for b in op.r:
                if b.lw is not None:
                    deps.append((b.lw, "raw"))
            for b in op.w:
                if b.lw is not None:
                    deps.append((b.lw, "waw"))
                for r in b.rd:
                    deps.append((r, "war"))
            keep = []
            for d, kind in deps:
                if d is op:
                    continue
                if d.eng == op.eng and not d.dma:
                    if op.dma:
                        pass
                    elif op.eng == "pe":
                        continue
                    elif kind != "raw":
                        continue
                    elif op.eidx - d.eidx > 6:
                        continue
                if d not in keep:
                    keep.append(d)
            best = {}
            final = []
            for d in keep:
                if d.dma:
                    final.append(d)
                elif d.eng not in best or d.eidx > best[d.eng].eidx:
                    best[d.eng] = d
            keep = final + list(best.values())
            for d in keep:
                d.need = True
            op.fn_deps = keep
            for b in op.r:
                b.rd.append(op)
            for b in op.w:
                b.lw = op
                b.rd = []
            if not op.dma:
                last_compute[op.eng] = op
        for op in last_compute.values():
            op.need = True
        for op in ops:
            eng = op.eng
            for d in op.fn_deps:
                sem, val = d.sig
                self._wait(eng, sem, val)
            if op.dma:
                n = len(self.ring[eng])
                i = self.dcnt[eng] % n
                self.dcnt[eng] += 1
                sem = self.ring[eng][i]
                self._wait(eng, sem, self.ringval[eng][i])
                inst = op.fn()
                self.ringval[eng][i] += 16
                inst.then_inc(self.sems[sem], 16)
                op.sig = (sem, self.ringval[eng][i])
            else:
                inst = op.fn()
                if op.need:
                    self.ccnt[eng] += 1
                    inst.then_inc(self.sems[self.csem[eng]], 1)
                    op.sig = (self.csem[eng], self.ccnt[eng])
            self.n_inst += 1
            self.ecount[eng] += 1
        self.barrier()

    def barrier(self):
        for eng in self.ENGS:
            for c in self.csem:
                if c != eng:
                    self._wait(eng, self.csem[c], self.ccnt[c])
            for q in self.ring:
                for i, sem in enumerate(self.ring[q]):
                    self._wait(eng, sem, self.ringval[q][i])

    def close(self):
        self.es.close()


def splits(n0, n1, w):
    out = []
    c = n0
    while c < n1:
        n = min(w, n1 - c)
        out.append((c, n))
        c += n
    return out


class Rot:
    def __init__(self, lst):
        self.lst = lst
        self.i = 0

    def __call__(self):
        r = self.lst[self.i % len(self.lst)]
        self.i += 1
        return r


EXPM05 = 0.6065306597126334
NPAR = 10
PK_K, PK_A, POMKA, PR_K, PLNW, PLNB, PW0, PA0 = 0, 1, 2, 3, 4, 5, 6, 8
NRT = 54


def build_program(cfg, debug=()):
    S, CTX = cfg["S"], cfg["CTX"]
    OWN = S // 2
    L = S + CTX
    NCH = L // 128
    NE = cfg.get("NE", N_EXP)
    NG = 8
    GS = NE // NG
    NE1 = NE + 1
    phases = cfg.get("phases", None)
    feed = cfg.get("feed", ())
    nc = bass.Bass("TRN2", target_bir_lowering=False)
    in_names = []
    in_cache = {}

    def on(ph):
        return phases is None or ph in phases

    def IN(name, shape, dt=F32):
        if name not in in_cache:
            in_names.append(name)
            in_cache[name] = nc.dram_tensor(name, list(shape), dt, kind="ExternalInput").ap()
        return in_cache[name]

    def scratch(name, shape, dt):
        if name in feed:
            return IN(name, shape, dt)
        kind = "ExternalOutput" if name in debug else "Internal"
        return nc.dram_tensor(name, list(shape), dt, kind=kind).ap()

    out_d = nc.dram_tensor("out", [OWN, D], F32, kind="ExternalOutput").ap()

    modscr = scratch("modscr", [2, N_MOD * D], F32)
    uT = scratch("uT", [D, L], BF16)
    qT = scratch("qT", [DA_W, OWN], BF16)
    kT = scratch("kT", [DA_W, L], BF16)
    vS = scratch("vS", [L, DA_W], BF16)
    rwT = scratch("rwT", [RW_COLS, L], BF16)
    oT = scratch("oT", [D, OWN], BF16)
    sgT = scratch("sgT", [512, OWN], BF16)
    AhT = [scratch("AhT%d" % d, [RW_W, L], BF16) for d in range(2)]
    RhT = [scratch("RhT%d" % d, [RW_W, L], BF16) for d in range(2)]
    BtT = [scratch("BtT%d" % d, [RW_W, L], BF16) for d in range(2)]
    KtT = [scratch("KtT%d" % d, [RW_W, L], BF16) for d in range(2)]
    Btok = [scratch("Btok%d" % d, [L, RW_W], BF16) for d in range(2)]
    Ktok = [scratch("Ktok%d" % d, [L, RW_W], BF16) for d in range(2)]
    Vtok = scratch("Vtok", [L, RW_W], BF16)
    Wc = [scratch("Wc%d" % d, [RW_W, NCH], F32) for d in range(2)]
    bonusT = scratch("bonusT", [RW_W, OWN], F32)
    Gs = [scratch("Gs%d" % d, [NCH, 128, 32 * 128], BF16) for d in range(2)]
    Ns = [scratch("Ns%d" % d, [NCH, 128, 32 * 128], BF16) for d in range(2)]
    Z0s = [scratch("Z0s%d" % d, [NCH, 128, 32 * 64], F32) for d in range(2)]
    Y0s = [scratch("Y0s%d" % d, [NCH, 128, 16 * 128], F32) for d in range(2)]
    ysc = [scratch("ysc%d" % d, [RW_W, OWN], F32) for d in range(2)]
    olat = scratch("olat", [OWN, D], F32)
    x1s = scratch("x1s", [OWN, D], F32)
    hTs = scratch("hTs", [D, OWN], BF16)
    gscT = scratch("gscT", [NE, OWN], F32)
    hbs = scratch("hbs", [NE1 * FF, OWN], BF16)
    ymoe = scratch("ymoe", [OWN, D], F32)

    P = Prog(nc)

    def load_ident(P):
        idf = P.sb("idf", [128, 128], F32)
        idb = P.sb("idb", [128, 128], BF16)
        P.dma("sp", idf[:], IN("ident", [128, 128])[:, :])
        P.copy("dve", idb[:], idf[:])
        return idf, idb

    def rms_stats(P, s, x, junk, n):
        P.act(junk, x, AF.Square, accum_out=s[:, 0:1])
        P.ts("dve", s[:, 1:2], s[:, 0:1], 1.0 / n, ALU.mult, NORM_EPS, ALU.add)
        P.act(s[:, 2:3], s[:, 1:2], AF.Sqrt)
        P.recip(s[:, 3:4], s[:, 2:3])

    if on(0):
        with P.phase():
            cv = P.sb("cv", [128, KC, 2], F32)
            cs = P.sb("cs", [128, KC, 2], F32)
            P.dma("sp", cv[:], IN("cvec", [128, KC, 2])[:, :, :])
            P.act(cs[:], cv[:], AF.Silu)
            w_mod = IN("w_mod", [D, N_MOD * D])
            b_mod = IN("b_mod", [1, N_MOD * D])
            wv = w_mod.rearrange("(k p) n -> p k n", p=128)
            NB = 256
            wts = [P.sb("wm%d" % i, [128, KC, NB], F32) for i in range(2)]
            pss = [P.ps("pm%d" % i, [128, 512]) for i in range(2)]
            bms = [P.sb("bm%d" % i, [2, NB], F32) for i in range(2)]
            mos = [P.sb("mo%d" % i, [2, NB], F32) for i in range(2)]
            for blk in range(N_MOD * D // NB):
                wt, ps, bm, mo = wts[blk % 2], pss[blk % 2], bms[blk % 2], mos[blk % 2]
                c0 = blk * NB
                P.dma("sp", wt[:], wv[:, :, c0:c0 + NB])
                P.dma("sp", bm[:], b_mod[0:1, c0:c0 + NB].partition_broadcast(2))
                for kc in range(KC):
                    P.mm(ps[0:2, 0:NB], cs[:, kc, :], wt[:, kc, :], start=(kc == 0), stop=(kc == KC - 1))
                P.tt("dve", mo[:], ps[0:2, 0:NB], bm[:], ALU.add)
                P.dma("sp", modscr[:, c0:c0 + NB], mo[:])

    if on(1):
        with P.phase():
            x_l = IN("x_l", [L, D])
            idf, idb = load_ident(P)
            A = P.sb("A", [128, D], F32)
            Sh = P.sb("Sh", [128, D], F32)
            gb = P.sb("gb", [128, D], F32)
            P.dma("sp", gb[:], IN("g_pre_attn", [1, D])[0:1, :].partition_broadcast(128))
            xs = [P.sb("x%d" % i, [128, D], F32) for i in range(2)]
            tmp = P.sb("tmp", [128, D], F32)
            ub = [P.sb("ub%d" % i, [128, D], BF16) for i in range(2)]
            sq = P.sb("sq", [128, D], BF16)
            st = [P.sb("st%d" % i, [128, 4], F32) for i in range(2)]
            uTs = P.sb("uTs", [128, KC, 512], BF16)
            pts = [P.ps("pt%d" % i, [128, 1024], BF16) for i in range(4)]
            uTv = uT.rearrange("(k p) l -> p k l", p=128)
            cur_var = None
            for g0, gn in splits(0, L // 128, 4):
                for ti in range(g0, g0 + gn):
                    var = 0 if ti * 128 < S else 1
                    if var != cur_var:
                        cur_var = var
                        P.dma("sp", Sh[:], modscr[var:var + 1, 0:D].partition_broadcast(128))
                        P.dma("sp", A[:], modscr[var:var + 1, D:2 * D].partition_broadcast(128))
                        P.stt(A[:], A[:], 1.0, gb[:], ALU.add, ALU.mult)
                    x, s, u = xs[ti % 2], st[ti % 2], ub[ti % 2]
                    P.dma("sp", x[:], x_l[ti * 128:(ti + 1) * 128, :])
                    rms_stats(P, s, x[:], sq[:], D)
                    P.stt(tmp[:], x[:], s[:, 3:4], A[:], ALU.mult, ALU.mult)
                    P.tt("pool", u[:], tmp[:], Sh[:], ALU.add)
                    for q4 in range(4):
                        pt = pts[q4]
                        for k8 in range(8):
                            kc = q4 * 8 + k8
                            P.tr(pt[:, k8 * 128:(k8 + 1) * 128], u[:, kc * 128:(kc + 1) * 128], idb[:])
                        dst = uTs[:, q4 * 8:(q4 + 1) * 8, (ti - g0) * 128:(ti - g0 + 1) * 128]
                        P.copy("act" if q4 % 2 else "dve", dst, pt[:].rearrange("p (k t) -> p k t", k=8))
                P.dma("sp", uTv[:, :, g0 * 128:(g0 + gn) * 128], uTs[:, :, 0:gn * 128])

    if on(2):
        with P.phase():
            w_in = IN("w_in", [D, IN_COLS])
            cos_d = IN("cosT", [128, L])
            sin_d = IN("sinT", [128, L])
            pmf = P.sb("pmf", [128, 128], F32)
            pmb = P.sb("pmb", [128, 128], BF16)
            P.dma("sp", pmf[:], IN("pm", [128, 128])[:, :])
            P.copy("dve", pmb[:], pmf[:])
            TBMAX = 1152
            uTb = P.sb("uTb", [128, KC, TBMAX], BF16)
            cosb = P.sb("cosb", [128, TBMAX], F32)
            sinb = P.sb("sinb", [128, TBMAX], F32)
            wbs = Rot([P.sb("wb%d" % i, [128, KC, 256], BF16) for i in range(2)])
            pss = Rot([P.ps("pp%d" % i, [128, 512]) for i in range(3)])
            ps2s = Rot([P.ps("pq%d" % i, [128, 512]) for i in range(2)])
            pTs = Rot([P.sb("pT%d" % i, [128, 512], BF16) for i in range(2)])
            t1s = Rot([P.sb("t1%d" % i, [128, 512], F32) for i in range(2)])
            t2s = Rot([P.sb("t2%d" % i, [128, 512], F32) for i in range(2)])
            obs = Rot([P.sb("ob%d" % i, [128, 512], BF16) for i in range(3)])
            uTv = uT.rearrange("(k p) l -> p k l", p=128)
            wv = w_in.rearrange("(k p) n -> p k n", p=128)
            blocks = [(t0, tn, True) for t0, tn in splits(0, OWN, 1024)] + \
                     [(t0, tn, False) for t0, tn in splits(OWN, L, TBMAX)]
            for (t0, tn, is_own) in blocks:
                P.dma("sp", uTb[:, :, 0:tn], uTv[:, :, t0:t0 + tn])
                P.dma("sp", cosb[:, 0:tn], cos_d[:, t0:t0 + tn])
                P.dma("sp", sinb[:, 0:tn], sin_d[:, t0:t0 + tn])
                groups = []
                if is_own:
                    groups += [("q", c, n) for c, n in splits(0, DA_W, 256)]
                groups += [("k", c, n) for c, n in splits(DA_W, 2 * DA_W, 256)]
                groups += [("v", c, n) for c, n in splits(2 * DA_W, 3 * DA_W, 256)]
                groups += [("rw", c, n) for c, n in splits(3 * DA_W, IN_COLS, 256)]
                for kind, c0, cn in groups:
                    wb = wbs()
                    P.dma("pool", wb[:, :, 0:cn], wv[:, :, c0:c0 + cn])
                    if kind == "v":
                        for tt0, _ in splits(0, tn, 128):
                            ps = pss()
                            for kc in range(KC):
                                P.mm(ps[:, 0:cn], uTb[:, kc, tt0:tt0 + 128], wb[:, kc, 0:cn], start=(kc == 0), stop=(kc == KC - 1))
                            ob = obs()
                            P.copy("act", ob[:, 0:cn], ps[:, 0:cn])
                            P.dma("sp", vS[t0 + tt0:t0 + tt0 + 128, c0 - 2 * DA_W:c0 - 2 * DA_W + cn], ob[:, 0:cn])
                        continue
                    for h0, hn in splits(0, cn, 128):
                        for s0, sn in splits(0, tn, 512):
                            ps = pss()
                            for kc in range(KC):
                                P.mm(ps[0:hn, 0:sn], wb[:, kc, h0:h0 + hn], uTb[:, kc, s0:s0 + sn], start=(kc == 0), stop=(kc == KC - 1))
                            ob = obs()
                            if kind == "rw":
                                P.copy("act", ob[0:hn, 0:sn], ps[0:hn, 0:sn])
                                r0 = c0 + h0 - 3 * DA_W
                                P.dma("sp", rwT[r0:r0 + hn, t0 + s0:t0 + s0 + sn], ob[0:hn, 0:sn])
                            else:
                                pT = pTs()
                                P.copy("act", pT[:, 0:sn], ps[:, 0:sn])
                                ps2 = ps2s()
                                P.mm(ps2[:, 0:sn], pmb[:], pT[:, 0:sn])
                                t1, t2 = t1s(), t2s()
                                P.tt("pool", t1[:, 0:sn], pT[:, 0:sn], cosb[:, s0:s0 + sn], ALU.mult)
                                P.tt("dve", t2[:, 0:sn], ps2[:, 0:sn], sinb[:, s0:s0 + sn], ALU.mult)
                                P.tt("dve", ob[:, 0:sn], t1[:, 0:sn], t2[:, 0:sn], ALU.add)
                                if kind == "q":
                                    r0 = c0 + h0
                                    P.dma("sp", qT[r0:r0 + 128, t0 + s0:t0 + s0 + sn], ob[:, 0:sn])
                                else:
                                    r0 = c0 + h0 - DA_W
                                    P.dma("sp", kT[r0:r0 + 128, t0 + s0:t0 + s0 + sn], ob[:, 0:sn])

    if on(3):
        with P.phase():
            idf, idb = load_ident(P)
            dl = P.sb("dl", [128, 512], F32)
            P.dma("sp", dl[:], IN("da_lambda", [1, 512])[0:1, :].partition_broadcast(128))
            pr = P.sb("pr", [128, 256], F32)
            lm = P.sb("lm", [128, 8], F32)
            P.tt("dve", pr[:, 0:128], dl[:, 0:128], dl[:, 128:256], ALU.mult)
            P.tt("dve", pr[:, 128:256], dl[:, 256:384], dl[:, 384:512], ALU.mult)
            P.red(lm[:, 0:1], pr[:, 0:128], ALU.add)
            P.red(lm[:, 1:2], pr[:, 128:256], ALU.add)
            P.act(lm[:, 2:4], lm[:, 0:2], AF.Exp)
            P.tt("dve", lm[:, 4:5], lm[:, 2:3], lm[:, 3:4], ALU.subtract)
            P.ts("dve", lm[:, 5:6], lm[:, 4:5], LAM_INIT, ALU.add, -1.0, ALU.mult)
            sg = P.sb("sg", [128, 256], F32)
            P.dma("sp", sg[:], IN("da_subln", [1, 256])[0:1, :].partition_broadcast(128))
            P.ts("dve", sg[:], sg[:], 1.0 - LAM_INIT, ALU.mult)
            NKT = L // 128
            SC = 128.0 ** -0.5
            kTv = kT.rearrange("(h m d) l -> h d m l", m=2, d=128)
            qTv = qT.rearrange("(h m d) l -> h d m l", m=2, d=128)
            vSv = vS.rearrange("(kt p) (h e) -> h p kt e", p=128, e=256)
            sets = []
            for i in range(2):
                kth = P.sb("kth%d" % i, [128, 2, L], BF16)
                qth = P.sb("qth%d" % i, [128, 2, OWN], BF16)
                vh = P.sb("vh%d" % i, [128, NKT, 257], BF16)
                P.memset("pool", vh[:, :, 256:257], 1.0)
                sets.append((kth, qth, vh))
            pss = Rot([P.ps("as%d" % i, [128, 512]) for i in range(2)])
            acc = [P.ps("ac%d" % i, [128, 512]) for i in range(4)]
            ptrs = Rot([P.ps("atr", [128, 1024], BF16)])
            PTs = Rot([P.sb("PT%d" % i, [128, 512], BF16) for i in range(3)])
            o0 = [P.sb("o0%d" % i, [128, 256], F32) for i in range(4)]
            sts = [P.sb("ast%d" % i, [128, 8], F32) for i in range(4)]
            junk = P.sb("ajunk", [128, 256], BF16)
            ons = Rot([P.sb("on%d" % i, [128, 256], BF16) for i in range(2)])
            oTss = Rot([P.sb("oTs%d" % i, [128, 2, 512], BF16) for i in range(2)])
            for h in range(8):
                kth, qth, vh = sets[h % 2]
                P.dma("sp", kth[:], kTv[h])
                P.dma("sp", qth[:], qTv[h])
                P.dma("sp", vh[:, :, 0:256], vSv[h])
                for q0, qn in splits(0, OWN, 512):
                    nqt = qn // 128
                    for m in range(2):
                        for kt in range(NKT):
                            ps = pss()
                            P.mm(ps[:, 0:qn], kth[:, m, kt * 128:(kt + 1) * 128], qth[:, m, q0:q0 + qn])
                            pt = PTs()
                            P.act(pt[:, 0:qn], ps[:, 0:qn], AF.Exp, scale=SC)
                            for qt in range(nqt):
                                P.mm(acc[qt][:, 0:257], pt[:, qt * 128:(qt + 1) * 128], vh[:, kt, :],
                                     start=(kt == 0), stop=(kt == NKT - 1))
                        for qt in range(nqt):
                            st = sts[qt]
                            P.recip(st[:, m:m + 1], acc[qt][:, 256:257])
                            if m == 0:
                                P.ts("dve", o0[qt][:], acc[qt][:, 0:256], st[:, 0:1], ALU.mult)
                            else:
                                P.tt("dve", st[:, 2:3], st[:, 1:2], lm[:, 5:6], ALU.mult)
                                P.stt(o0[qt][:], acc[qt][:, 0:256], st[:, 2:3], o0[qt][:], ALU.mult, ALU.add)
                    oTs = oTss()
                    for qt in range(nqt):
                        st = sts[qt]
                        P.act(junk[:], o0[qt][:], AF.Square, accum_out=st[:, 3:4])
                        P.ts("dve", st[:, 4:5], st[:, 3:4], 1.0 / 256, ALU.mult, NORM_EPS, ALU.add)
                        P.act(st[:, 5:6], st[:, 4:5], AF.Sqrt)
                        P.recip(st[:, 6:7], st[:, 5:6])
                        on_ = ons()
                        P.stt(on_[:], o0[qt][:], st[:, 6:7], sg[:], ALU.mult, ALU.mult)
                        ptr = ptrs()
                        for e2 in range(2):
                            P.tr(ptr[:, e2 * 128:(e2 + 1) * 128], on_[:, e2 * 128:(e2 + 1) * 128], idb[:])
                        P.copy("act", oTs[:, :, qt * 128:(qt + 1) * 128], ptr[:, 0:256].rearrange("p (a t) -> p a t", a=2))
                    P.dma("sp", oT[h * 256:(h + 1) * 256, q0:q0 + qn].rearrange("(a p) t -> p a t", p=128), oTs[:, :, 0:qn])

    Lp = L + 4

    def ix(l):
        return l + 1 if l < S else l + 3

    dir_ranges = [[(0, OWN), (S, L)], [(0, S), (S, L)]]
    if on(4):
        with P.phase():
            idf, idb = load_ident(P)
            shab = P.sb("shab", [128, NRT, 2], F32)
            P.dma("sp", shab[:], IN("shAB", [128, NRT, 2])[:, :, :])
            c0s = P.sb("c0s", [128, NRT], F32)
            P.tt("dve", c0s[:].unsqueeze(2), shab[:, :, 0:1], shab[:, :, 1:2], ALU.add)
            P.ts("dve", c0s[:], c0s[:], -1.0, ALU.mult, 1.0, ALU.add)
            chp = P.sb("chp", [128, 16, NPAR], F32)
            P.dma("sp", chp[:], IN("chp", [128, 16, NPAR])[:, :, :])
            P.ts("dve", chp[:, :, POMKA:POMKA + 1], chp[:, :, PK_A:PK_A + 1], -1.0, ALU.mult, 1.0, ALU.add)
            bo = P.sb("bo", [128, 128], F32)
            P.dma("sp", bo[:], IN("bo", [128, 128])[:, :])
            w2b = [P.sb("w2b%d" % d, [128, RW_W], BF16) for d in range(2)]
            a2b = [P.sb("a2b%d" % d, [128, RW_W], BF16) for d in range(2)]
            w2d = IN("rw_w2d", [2, 128, RW_W])
            a2d = IN("rw_a2d", [2, 128, RW_W])
            for d in range(2):
                P.dma("pool", w2b[d][:], w2d[d])
                P.dma("pool", a2b[d][:], a2d[d])
            raws = [P.sb("raw%d" % i, [128, Lp], BF16) for i in range(3)]
            for r in raws:
                for pidx in (0, S + 1, S + 2, L + 3):
                    P.memset("pool", r[:, pidx:pidx + 1], 0.0)

            def load_raw(raw, r0, rn):
                P.dma("sp", raw[0:rn, 1:S + 1], rwT[r0:r0 + rn, 0:S])
                P.dma("sp", raw[0:rn, S + 3:L + 3], rwT[r0:r0 + rn, S:L])

            def shift(out, raw, rt, rn=128):
                P.ts("dve", out[0:rn, 1:L + 3], raw[0:rn, 1:L + 3], c0s[0:rn, rt:rt + 1], ALU.mult)
                P.stt(out[0:rn, 1:L + 3], raw[0:rn, 0:L + 2], shab[0:rn, rt, 0:1], out[0:rn, 1:L + 3], ALU.mult, ALU.add)
                P.stt(out[0:rn, 1:L + 3], raw[0:rn, 2:L + 4], shab[0:rn, rt, 1:2], out[0:rn, 1:L + 3], ALU.mult, ALU.add)

            ks = P.sb("ks", [128, Lp], F32)
            kk = P.sb("kk", [128, Lp], F32)
            rs = P.sb("rs", [128, Lp], BF16)
            vs = P.sb("vs", [128, Lp], BF16)
            tw = P.sb("tw", [128, Lp], BF16)
            xab = P.sb("xab", [128, Lp], BF16)
            load_raw(raws[0], 3 * RW_W, 128)
            shift(ks, raws[0], 48)
            P.act(tw[:, 1:L + 3], ks[:, 1:L + 3], AF.Tanh)
            load_raw(raws[1], 3 * RW_W + 128, 128)
            shift(xab, raws[1], 49)
            sgs = P.sb("sgs", [128, OWN], BF16)
            for gi in range(4):
                rn = 128 if gi < 3 else 96
                raw = raws[(2 + gi) % 3]
                load_raw(raw, 3 * RW_W + 256 + gi * 128, rn)
                shift(kk, raw, 50 + gi, rn)
                P.act(sgs[0:rn, :], kk[0:rn, 1:OWN + 1], AF.Sigmoid)
                P.dma("sp", sgT[gi * 128:gi * 128 + rn, :], sgs[0:rn, :])
            SEG = 512
            pss = Rot([P.ps("fp%d" % i, [128, 512]) for i in range(3)])
            ptb = Rot([P.ps("ftb%d" % i, [128, 1024], BF16) for i in range(3)])
            T = {n: P.sb("f_" + n, [128, SEG], F32) for n in ("sig", "lw", "cum", "cum2", "ta", "tb", "e1", "e2", "e3", "av", "kd")}
            OB = {n: P.sb("f_" + n, [128, SEG], BF16) for n in ("ah", "rh", "bt", "kt")}
            m01 = P.sb("m01", [128, SEG], F32)
            P.memset("dve", m01[:], 1.0)
            P.memset("dve", m01[:].rearrange("p (c i) -> p c i", i=128)[:, :, 0:1], 0.0)
            kdsum = P.sb("kdsum", [128, OWN], F32)
            sqb = P.sb("sqb", [128, 512], F32)
            kkr = P.sb("kkr", [128, 512], F32)
            sd = P.sb("sd", [128, 512], F32)
            wcs = P.sb("wcs", [128, 8], F32)
            stg = Rot([P.sb("stg%d" % i, [128, 8, 128], BF16) for i in range(3)])
            bon = P.sb("bon", [128, 512], F32)
            for ct in range(16):
                for i, (rt, dst) in enumerate(((ct, rs), (16 + ct, ks), (32 + ct, vs))):
                    load_raw(raws[i], rt * 128, 128)
                    shift(dst, raws[i], rt)
                for b0, bn in splits(1, L + 3, 512):
                    P.ts("dve", kkr[:, 0:bn], ks[:, b0:b0 + bn], chp[:, ct, PK_K:PK_K + 1], ALU.mult)
                    P.tt("pool", sqb[:, 0:bn], kkr[:, 0:bn], kkr[:, 0:bn], ALU.mult)
                    ps = pss()
                    P.mm(ps[:, 0:bn], bo[:], sqb[:, 0:bn])
                    P.act(sd[:, 0:bn], ps[:, 0:bn], AF.Sqrt)
                    P.ts("dve", sd[:, 0:bn], sd[:, 0:bn], 1e-12, ALU.max)
                    P.recip(sd[:, 0:bn], sd[:, 0:bn])
                    P.tt("dve", kk[:, b0:b0 + bn], kkr[:, 0:bn], sd[:, 0:bn], ALU.mult)
                for c0, cn in splits(0, NCH, 8):
                    pt = ptb()
                    for cc in range(cn):
                        i0 = ix((c0 + cc) * 128)
                        P.tr(pt[:, cc * 128:(cc + 1) * 128], vs[:, i0:i0 + 128], idb[:])
                    sg_ = stg()
                    P.copy("act", sg_[:, 0:cn, :], pt[:, 0:cn * 128].rearrange("p (c n) -> p c n", n=128))
                    P.dma("sp", Vtok[c0 * 128:(c0 + cn) * 128, ct * 128:(ct + 1) * 128].rearrange("(c p) n -> p c n", p=128), sg_[:, 0:cn, :])
                for di in range(2):
                    for (ra, rb) in dir_ranges[di]:
                        for l0, n in splits(ra, rb, SEG):
                            i0 = ix(l0)
                            ncn = n // 128
                            for s0, sn in splits(0, n, 512):
                                ps = pss()
                                P.mm(ps[:, 0:sn], w2b[di][:, ct * 128:(ct + 1) * 128], tw[:, i0 + s0:i0 + s0 + sn])
                                P.act(T["sig"][:, s0:s0 + sn], ps[:, 0:sn], AF.Sigmoid, bias=chp[:, ct, PW0 + di:PW0 + di + 1])
                                ps = pss()
                                P.mm(ps[:, 0:sn], a2b[di][:, ct * 128:(ct + 1) * 128], xab[:, i0 + s0:i0 + s0 + sn])
                                P.act(T["av"][:, s0:s0 + sn], ps[:, 0:sn], AF.Sigmoid, bias=chp[:, ct, PA0 + di:PA0 + di + 1])
                            P.ts("dve", T["lw"][:, 0:n], T["sig"][:, 0:n], -EXPM05, ALU.mult)
                            P.scan(T["cum"][:, 0:n], m01[:, 0:n], T["lw"][:, 0:n], 0.0, ALU.mult, ALU.add)
                            cum = T["cum"]
                            if di == 1:
                                P.tt("dve", T["ta"][:, 0:n], T["lw"][:, 0:n], T["cum"][:, 0:n], ALU.subtract)
                                tot = T["cum"][:, 0:n].rearrange("p (c i) -> p c i", i=128)[:, :, 127:128].to_broadcast([128, ncn, 128])
                                P.tt("dve", T["cum2"][:, 0:n].rearrange("p (c i) -> p c i", i=128),
                                     T["ta"][:, 0:n].rearrange("p (c i) -> p c i", i=128), tot, ALU.add)
                                cum = T["cum2"]
                            P.act(T["e1"][:, 0:n], cum[:, 0:n], AF.Exp)
                            P.act(T["e2"][:, 0:n], cum[:, 0:n], AF.Exp, scale=-1.0)
                            P.tt("pool", T["tb"][:, 0:n], cum[:, 0:n], T["lw"][:, 0:n], ALU.subtract)
                            P.act(T["e3"][:, 0:n], T["tb"][:, 0:n], AF.Exp)
                            P.ts("dve", T["ta"][:, 0:n], T["av"][:, 0:n], chp[:, ct, PK_A:PK_A + 1], ALU.mult, chp[:, ct, POMKA:POMKA + 1], ALU.add)
                            P.tt("dve", T["kd"][:, 0:n], T["ta"][:, 0:n], ks[:, i0:i0 + n], ALU.mult)
                            P.stt(OB["ah"][:, 0:n], kk[:, i0:i0 + n], -1.0, T["e3"][:, 0:n], ALU.mult, ALU.mult)
                            P.tt("pool", OB["rh"][:, 0:n], rs[:, i0:i0 + n], T["e1"][:, 0:n], ALU.mult)
                            P.tt("dve", T["ta"][:, 0:n], kk[:, i0:i0 + n], T["av"][:, 0:n], ALU.mult)
                            P.tt("dve", OB["bt"][:, 0:n], T["ta"][:, 0:n], T["e2"][:, 0:n], ALU.mult)
                            P.tt("pool", OB["kt"][:, 0:n], T["kd"][:, 0:n], T["e2"][:, 0:n], ALU.mult)
                            e1v = T["e1"][:, 0:n].rearrange("p (c i) -> p c i", i=128)
                            pos = 127 if di == 0 else 0
                            P.copy("act", wcs[:, 0:ncn], e1v[:, :, pos])
                            rows = slice(ct * 128, (ct + 1) * 128)
                            P.dma("sp", Wc[di][rows, l0 // 128:l0 // 128 + ncn], wcs[:, 0:ncn], slow=True)
                            P.dma("sp", AhT[di][rows, l0:l0 + n], OB["ah"][:, 0:n])
                            P.dma("sp", RhT[di][rows, l0:l0 + n], OB["rh"][:, 0:n])
                            P.dma("sp", BtT[di][rows, l0:l0 + n], OB["bt"][:, 0:n])
                            P.dma("sp", KtT[di][rows, l0:l0 + n], OB["kt"][:, 0:n])
                            for nm, dstT in (("bt", Btok[di]), ("kt", Ktok[di])):
                                pt = ptb()
                                for cc in range(ncn):
                                    P.tr(pt[:, cc * 128:(cc + 1) * 128], OB[nm][:, cc * 128:(cc + 1) * 128], idb[:])
                                sg_ = stg()
                                P.copy("act", sg_[:, 0:ncn, :], pt[:, 0:ncn * 128].rearrange("p (c n) -> p c n", n=128))
                                P.dma("sp", dstT[l0:l0 + n, ct * 128:(ct + 1) * 128].rearrange("(c p) n -> p c n", p=128), sg_[:, 0:ncn, :])
                            if l0 < OWN:
                                no = min(n, OWN - l0)
                                if di == 0:
                                    P.copy("pool", kdsum[:, l0:l0 + no], T["kd"][:, 0:no])
                                else:
                                    P.tt("pool", kdsum[:, l0:l0 + no], kdsum[:, l0:l0 + no], T["kd"][:, 0:no], ALU.add)
                for b0, bn in splits(0, OWN, 512):
                    P.tt("dve", sqb[:, 0:bn], rs[:, b0 + 1:b0 + 1 + bn], kdsum[:, b0:b0 + bn], ALU.mult)
                    P.ts("dve", sqb[:, 0:bn], sqb[:, 0:bn], chp[:, ct, PR_K:PR_K + 1], ALU.mult)
                    ps = pss()
                    P.mm(ps[:, 0:bn], bo[:], sqb[:, 0:bn])
                    P.tt("dve", bon[:, 0:bn], ps[:, 0:bn], vs[:, b0 + 1:b0 + 1 + bn], ALU.mult)
                    P.dma("sp", bonusT[ct * 128:(ct + 1) * 128, b0:b0 + bn], bon[:, 0:bn])

    own_ch = list(range(OWN // 128))
    oth_ch = list(range(OWN // 128, S // 128))
    ctx_ch = list(range(S // 128, NCH))
    dir_chunks = [ctx_ch + own_ch, ctx_ch[::-1] + oth_ch[::-1] + own_ch[::-1]]

    if on(5):
        with P.phase():
            idf, idb = load_ident(P)
            mks = [P.sb("mk%d" % d, [128, 384], F32) for d in range(2)]
            msk_d = IN("masks", [2, 128, 384])
            for d in range(2):
                P.dma("sp", mks[d][:], msk_d[d])
            ins = []
            for i in range(2):
                ins.append(dict(
                    ARh=P.sb("cARh%d" % i, [128, 16, 2, 128], BF16), Bt=P.sb("cBt%d" % i, [128, 16, 128], BF16),
                    Kt=P.sb("cKt%d" % i, [128, 16, 128], BF16), Vt=P.sb("cVt%d" % i, [128, RW_W], BF16),
                    Gst=P.sb("cG%d" % i, [128, 32, 128], BF16), Nst=P.sb("cN%d" % i, [128, 32, 128], BF16),
                    Zst=P.sb("cZ%d" % i, [128, 32, 64], F32), Yst=P.sb("cY%d" % i, [128, 16, 128], F32)))
            NW = 4
            bk = [P.ps("cbk%d" % i, [128, 512]) for i in range(8)]
            Xs = P.sb("cXs", [128, 32, 128], F32)
            Ls = P.sb("cLs", [128, 32, 128], F32)
            MNs = Rot([P.sb("cMN%d" % i, [128, 256], BF16) for i in range(4)])
            XGt = [[P.sb("cXG%d_%d" % (i, pp), [128, 256], F32) for pp in range(2)] for i in range(NW)]
            Lct = [[P.sb("cLc%d_%d" % (i, pp), [128, 128], F32) for pp in range(2)] for i in range(NW)]
            step = 0
            for di in range(2):
                mk = mks[di]
                for c in dir_chunks[di]:
                    I = ins[step % 2]
                    step += 1
                    own = c * 128 < OWN
                    cols = slice(c * 128, (c + 1) * 128)
                    P.dma("sp", I["ARh"][:, :, 0, :], AhT[di].rearrange("(ct p) l -> p ct l", p=128)[:, :, cols])
                    P.dma("sp", I["ARh"][:, :, 1, :], RhT[di].rearrange("(ct p) l -> p ct l", p=128)[:, :, cols])
                    P.dma("sp", I["Bt"][:], BtT[di].rearrange("(ct p) l -> p ct l", p=128)[:, :, cols])
                    P.dma("sp", I["Kt"][:], KtT[di].rearrange("(ct p) l -> p ct l", p=128)[:, :, cols])
                    P.dma("sp", I["Vt"][:], Vtok[c * 128:(c + 1) * 128, :])
                    for h in range(32):
                        ct, half = h // 2, h % 2
                        hp = slice(half * 64, half * 64 + 64)
                        AB, CZ = bk[h % 4], bk[4 + h % 4]
                        P.mm(AB[:, 0:256], I["Bt"][hp, ct, :], I["ARh"][hp, ct, :, :])
                        P.mm(AB[:, 256:512], I["Kt"][hp, ct, :], I["ARh"][hp, ct, :, :])
                        P.mm(CZ[:, 0:128], I["ARh"][hp, ct, 0, :], I["Bt"][hp, ct, :])
                        MN = MNs()
                        P.tt("dve", Xs[:, h, :], AB[:, 0:128], mk[:, 0:128], ALU.mult)
                        if own:
                            P.tt("dve", I["Nst"][:, h, :], AB[:, 128:256], mk[:, 128:256], ALU.mult)
                        P.tt("dve", MN[:], AB[:, 256:512], mk[:, 0:256], ALU.mult)
                        P.tt("dve", Ls[:, h, :], CZ[:, 0:128], mk[:, 256:384], ALU.mult)
                        hc = slice(h * 64, (h + 1) * 64)
                        P.mm(CZ[:, 128:192], MN[:, 0:128], I["Vt"][:, hc])
                        P.copy("act", I["Zst"][:, h, :], CZ[:, 128:192])
                        if own:
                            P.mm(CZ[hp, 256:384], I["Vt"][:, hc], MN[:, 128:256])
                            P.copy("act", I["Yst"][hp, ct, :], CZ[hp, 256:384])
                    for g0 in range(0, 32, NW):
                        hs = list(range(g0, g0 + NW))
                        for i, h in enumerate(hs):
                            P.mm(bk[i][:, 0:128], Ls[:, h, :], Xs[:, h, :])
                            P.mm(bk[NW + i][:, 0:128], Xs[:, h, :], Ls[:, h, :])
                        for i, h in enumerate(hs):
                            XG, Lc = XGt[i][0], Lct[i][0]
                            P.tt("dve", XG[:, 128:256], Xs[:, h, :], idf[:], ALU.add)
                            P.copy("act", XG[:, 0:128], bk[i][:, 0:128])
                            P.copy("act", Lc[:], bk[NW + i][:, 0:128])
                        for k in range(1, 7):
                            pc, pn = (k - 1) % 2, k % 2
                            if k < 6:
                                for i, h in enumerate(hs):
                                    XG, Lc = XGt[i][pc], Lct[i][pc]
                                    P.mm(bk[i][:, 0:256], Lc[:], XG[:, 0:256])
                                    P.mm(bk[NW + i][:, 0:128], XG[:, 0:128], Lc[:])
                                for i, h in enumerate(hs):
                                    XG, XG2, Lc2 = XGt[i][pc], XGt[i][pn], Lct[i][pn]
                                    P.copy("act", XG2[:, 0:128], bk[i][:, 0:128])
                                    P.tt("dve", XG2[:, 128:256], bk[i][:, 128:256], XG[:, 128:256], ALU.add)
                                    P.copy("act" if i % 2 else "dve", Lc2[:], bk[NW + i][:, 0:128])
                            else:
                                for i, h in enumerate(hs):
                                    P.mm(bk[i][:, 128:256], Lct[i][pc][:], XGt[i][pc][:, 128:256])
                                for i, h in enumerate(hs):
                                    P.tt("dve", I["Gst"][:, h, :], bk[i][:, 128:256], XGt[i][pc][:, 128:256], ALU.add)
                    P.dma("sp", Gs[di][c], I["Gst"][:].rearrange("p h i -> p (h i)"))
                    P.dma("sp", Z0s[di][c], I["Zst"][:].rearrange("p h i -> p (h i)"))
                    if own:
                        P.dma("sp", Ns[di][c], I["Nst"][:].rearrange("p h i -> p (h i)"))
                        P.dma("sp", Y0s[di][c], I["Yst"][:].rearrange("p h i -> p (h i)"))

    if on(6):
        with P.phase():
            ST = P.sb("ST", [128, 16, 64], F32)
            STz = P.sb("STz", [128, 16, 2, 64], BF16)
            wcs = P.sb("swc", [128, 16, NCH], F32)
            ins = []
            for i in range(2):
                ins.append(dict(
                    ARh=P.sb("sARh%d" % i, [128, 16, 2, 128], BF16), G=P.sb("sG%d" % i, [128, 32, 128], BF16),
                    N=P.sb("sN%d" % i, [128, 32, 128], BF16), Z0=P.sb("sZ%d" % i, [128, 32, 64], F32),
                    Y0=P.sb("sY%d" % i, [128, 16, 128], F32), Bt=P.sb("sBt%d" % i, [128, RW_W], BF16),
                    Kt=P.sb("sKt%d" % i, [128, RW_W], BF16), Vt=P.sb("sVt%d" % i, [128, RW_W], BF16)))
            Zb = P.sb("sZb", [128, 32, 64], BF16)
            Ub = P.sb("sUb", [128, 32, 64], BF16)
            yts = Rot([P.sb("syt%d" % i, [128, 16, 128], F32) for i in range(2)])
            pz = Rot([P.ps("spz%d" % i, [128, 512]) for i in range(2)])
            pu = Rot([P.ps("spu%d" % i, [128, 512]) for i in range(2)])
            py = Rot([P.ps("spy%d" % i, [128, 512]) for i in range(2)])
            psb = Rot([P.ps("sps%d" % i, [128, 512]) for i in range(2)])
            step = 0
            for di in range(2):
                P.memset("dve", ST[:], 0.0)
                P.memset("pool", STz[:], 0.0)
                P.dma("sp", wcs[:], Wc[di].rearrange("(ct p) c -> p ct c", p=128))
                for c in dir_chunks[di]:
                    I = ins[step % 2]
                    step += 1
                    own = c * 128 < OWN
                    cols = slice(c * 128, (c + 1) * 128)
                    P.dma("sp", I["ARh"][:, :, 0, :], AhT[di].rearrange("(ct p) l -> p ct l", p=128)[:, :, cols])
                    P.dma("sp", I["G"][:].rearrange("p h i -> p (h i)"), Gs[di][c])
                    P.dma("sp", I["Z0"][:].rearrange("p h i -> p (h i)"), Z0s[di][c])
                    P.dma("sp", I["Bt"][:], Btok[di][c * 128:(c + 1) * 128, :])
                    P.dma("sp", I["Kt"][:], Ktok[di][c * 128:(c + 1) * 128, :])
                    P.dma("sp", I["Vt"][:], Vtok[c * 128:(c + 1) * 128, :])
                    if own:
                        P.dma("sp", I["ARh"][:, :, 1, :], RhT[di].rearrange("(ct p) l -> p ct l", p=128)[:, :, cols])
                        P.dma("sp", I["N"][:].rearrange("p h i -> p (h i)"), Ns[di][c])
                        P.dma("sp", I["Y0"][:].rearrange("p h i -> p (h i)"), Y0s[di][c])
                    for g in range(4):
                        p_ = pz()
                        for hh in range(8):
                            h = g * 8 + hh
                            ct, hp = h // 2, slice((h % 2) * 64, (h % 2) * 64 + 64)
                            P.mm(p_[:, hh * 64:(hh + 1) * 64], I["ARh"][:, ct, 0, :], STz[:, ct, h % 2, :])
                        P.tt("dve", Zb[:, g * 8:(g + 1) * 8, :], p_[:].rearrange("p (h v) -> p h v", v=64), I["Z0"][:, g * 8:(g + 1) * 8, :], ALU.add)
                    for g in range(4):
                        p_ = pu()
                        for hh in range(8):
                            h = g * 8 + hh
                            P.mm(p_[:, hh * 64:(hh + 1) * 64], I["G"][:, h, :], Zb[:, h, :])
                        P.copy("act", Ub[:, g * 8:(g + 1) * 8, :], p_[:].rearrange("p (h v) -> p h v", v=64))
                    if own:
                        yt = yts()
                        for g in range(4):
                            p_ = py()
                            for c4 in range(4):
                                ct = g * 4 + c4
                                for half in range(2):
                                    h = ct * 2 + half
                                    hp = slice(half * 64, half * 64 + 64)
                                    P.mm(p_[hp, c4 * 128:(c4 + 1) * 128], STz[:, ct, half, :], I["ARh"][:, ct, 1, :], start=True, stop=False)
                                    P.mm(p_[hp, c4 * 128:(c4 + 1) * 128], Ub[:, h, :], I["N"][:, h, :], start=False, stop=True)
                            P.tt("dve", yt[:, g * 4:(g + 1) * 4, :], p_[:].rearrange("p (c i) -> p c i", i=128), I["Y0"][:, g * 4:(g + 1) * 4, :], ALU.add)
                        P.dma("sp", ysc[di].rearrange("(ct p) l -> p ct l", p=128)[:, :, cols], yt[:])
                    for g in range(2):
                        p_ = psb()
                        for c8 in range(8):
                            ct = g * 8 + c8
                            for half in range(2):
                                h = ct * 2 + half
                                hp = slice(half * 64, half * 64 + 64)
                                hc = slice(h * 64, (h + 1) * 64)
                                P.mm(p_[hp, c8 * 64:(c8 + 1) * 64], I["Bt"][:, hc], Ub[:, h, :], start=True, stop=False)
                                P.mm(p_[hp, c8 * 64:(c8 + 1) * 64], I["Kt"][:, hc], I["Vt"][:, hc], start=False, stop=True)
                        P.tt("dve", ST[:, g * 8:(g + 1) * 8, :], p_[:].rearrange("p (c v) -> p c v", v=64), ST[:, g * 8:(g + 1) * 8, :], ALU.add)
                    P.tt("dve", ST[:], ST[:], wcs[:, :, c:c + 1].to_broadcast([128, 16, 64]), ALU.mult)
                    P.copy("act", STz[0:64, :, 0, :], ST[0:64, :, :])
                    P.copy("act", STz[64:128, :, 1, :], ST[64:128, :, :])

    if on(7):
        with P.phase():
            chp = P.sb("chp", [128, 16, NPAR], F32)
            P.dma("sp", chp[:], IN("chp", [128, 16, NPAR])[:, :, :])
            bo = P.sb("bo", [128, 128], F32)
            P.dma("sp", bo[:], IN("bo", [128, 128])[:, :])
            bo64 = P.sb("bo64", [128, 128], F32)
            P.ts("dve", bo64[:], bo[:], 1.0 / 64, ALU.mult)
            g2b = P.sb("g2b", [128, 4, RW_W], BF16)
            g2d = IN("rw_g2", [480, RW_W])
            for gi in range(4):
                rn = 128 if gi < 3 else 96
                P.dma("pool", g2b[0:rn, gi, :], g2d[gi * 128:gi * 128 + rn, :])
            sgb = P.sb("sgb", [128, 4, OWN], BF16)
            P.dma("sp", sgb[:], sgT.rearrange("(g p) t -> p g t", p=128))
            pss = Rot([P.ps("op%d" % i, [128, 512]) for i in range(4)])
            ya = Rot([P.sb("ya%d" % i, [128, 512], F32) for i in range(2)])
            yb = Rot([P.sb("yb%d" % i, [128, 512], F32) for i in range(2)])
            bn_ = Rot([P.sb("obn%d" % i, [128, 512], F32) for i in range(2)])
            yc = P.sb("yc", [128, 512], F32)
            sq = P.sb("osq", [128, 512], F32)
            rsd = P.sb("rsd", [128, 512], F32)
            ob = Rot([P.sb("oob%d" % i, [128, 512], BF16) for i in range(2)])
            for ct in range(16):
                rows = slice(ct * 128, (ct + 1) * 128)
                for b0, bn in splits(0, OWN, 512):
                    y0, y1, bt = ya(), yb(), bn_()
                    P.dma("sp", y0[:, 0:bn], ysc[0][rows, b0:b0 + bn])
                    P.dma("sp", y1[:, 0:bn], ysc[1][rows, b0:b0 + bn])
                    P.dma("sp", bt[:, 0:bn], bonusT[rows, b0:b0 + bn])
                    P.tt("dve", y0[:, 0:bn], y0[:, 0:bn], y1[:, 0:bn], ALU.add)
                    pm_ = pss()
                    P.mm(pm_[:, 0:bn], bo64[:], y0[:, 0:bn])
                    P.tt("dve", yc[:, 0:bn], y0[:, 0:bn], pm_[:, 0:bn], ALU.subtract)
                    P.tt("pool", sq[:, 0:bn], yc[:, 0:bn], yc[:, 0:bn], ALU.mult)
                    pv = pss()
                    P.mm(pv[:, 0:bn], bo64[:], sq[:, 0:bn])
                    P.ts("dve", rsd[:, 0:bn], pv[:, 0:bn], GN_EPS, ALU.add)
                    P.act(rsd[:, 0:bn], rsd[:, 0:bn], AF.Sqrt)
                    P.recip(rsd[:, 0:bn], rsd[:, 0:bn])
                    P.tt("dve", yc[:, 0:bn], yc[:, 0:bn], rsd[:, 0:bn], ALU.mult)
                    P.ts("dve", yc[:, 0:bn], yc[:, 0:bn], chp[:, ct, PLNW:PLNW + 1], ALU.mult, chp[:, ct, PLNB:PLNB + 1], ALU.add)
                    P.tt("dve", yc[:, 0:bn], yc[:, 0:bn], bt[:, 0:bn], ALU.add)
                    pg = pss()
                    for gi in range(4):
                        rn = 128 if gi < 3 else 96
                        P.mm(pg[:, 0:bn], g2b[0:rn, gi, rows], sgb[0:rn, gi, b0:b0 + bn], start=(gi == 0), stop=(gi == 3))
                    o_ = ob()
                    P.tt("dve", o_[:, 0:bn], yc[:, 0:bn], pg[:, 0:bn], ALU.mult)
                    P.dma("sp", oT[DA_W + ct * 128:DA_W + (ct + 1) * 128, b0:b0 + bn], o_[:, 0:bn])

    if on(8):
        with P.phase():
            w_out = IN("w_out", [D, D])
            wv = w_out.rearrange("(k p) n -> p k n", p=128)
            oTv = oT.rearrange("(k p) t -> p k t", p=128)
            oTb = P.sb("oTb", [128, KC, 1024], BF16)
            wbs = Rot([P.sb("wo%d" % i, [128, KC, 512], BF16) for i in range(2)])
            pss = Rot([P.ps("wp%d" % i, [128, 512]) for i in range(4)])
            obs = Rot([P.sb("wob%d" % i, [128, 512], F32) for i in range(3)])
            for t0, tn in splits(0, OWN, 1024):
                P.dma("sp", oTb[:, :, 0:tn], oTv[:, :, t0:t0 + tn])
                for c0, cn in splits(0, D, 512):
                    wb = wbs()
                    P.dma("pool", wb[:], wv[:, :, c0:c0 + cn])
                    for tt0, _ in splits(0, tn, 128):
                        ps = pss()
                        for kc in range(KC):
                            P.mm(ps[:], oTb[:, kc, tt0:tt0 + 128], wb[:, kc, :], start=(kc == 0), stop=(kc == KC - 1))
                        o_ = obs()
                        P.copy("act", o_[:], ps[:])
                        P.dma("sp", olat[t0 + tt0:t0 + tt0 + 128, c0:c0 + cn], o_[:])

    if on(9):
        with P.phase():
            x_l = IN("x_l", [L, D])
            idf, idb = load_ident(P)
            G2 = P.sb("G2", [128, D], F32)
            A2 = P.sb("A2", [128, D], F32)
            Sh2 = P.sb("Sh2", [128, D], F32)
            tmp = P.sb("tmp", [128, D], F32)
            P.dma("sp", G2[:], modscr[0:1, 2 * D:3 * D].partition_broadcast(128))
            P.dma("sp", tmp[:], IN("g_post_attn", [1, D])[0:1, :].partition_broadcast(128))
            P.tt("dve", G2[:], G2[:], tmp[:], ALU.mult)
            P.dma("sp", Sh2[:], modscr[0:1, 3 * D:4 * D].partition_broadcast(128))
            P.dma("sp", A2[:], modscr[0:1, 4 * D:5 * D].partition_broadcast(128))
            P.dma("sp", tmp[:], IN("g_pre_ffn", [1, D])[0:1, :].partition_broadcast(128))
            P.stt(A2[:], A2[:], 1.0, tmp[:], ALU.add, ALU.mult)
            rwf = P.sb("rwf", [128, KC, NE], F32)
            P.dma("sp", rwf[:], IN("router_w", [D, NE]).rearrange("(k p) e -> p k e", p=128))
            rbias = P.sb("rbias", [128, NE], F32)
            P.dma("sp", rbias[:], IN("router_bias", [1, NE])[0:1, :].partition_broadcast(128))
            xt = P.sb("xt", [128, D], F32)
            ol = P.sb("ol", [128, D], F32)
            x1 = P.sb("x1", [128, D], F32)
            hf = ol
            hb = P.sb("hb", [128, D], BF16)
            junk = P.sb("junk", [128, D], BF16)
            hTf = P.sb("hTf", [128, KC, 128], F32)
            hTb = P.sb("hTb", [128, KC, 128], BF16)
            st = P.sb("st", [128, 8], F32)
            ptb = Rot([P.ps("rtb%d" % i, [128, 1024], BF16) for i in range(2)])
            ptf = Rot([P.ps("rtf%d" % i, [128, 512]) for i in range(3)])
            prr = P.ps("prr", [128, 512])
            R = {n: P.sb("r_" + n, [128, NE], F32) for n in ("sc", "bi", "eq", "mk", "mb", "sel", "ga")}
            r8 = {n: P.sb("r8_" + n, [128, 8], F32) for n in ("m1", "m2", "gs", "srt", "gm", "pen", "srt2", "den")}
            gT = P.sb("gT", [128, 128], F32)
            for ti in range(OWN // 128):
                rowsl = slice(ti * 128, (ti + 1) * 128)
                P.dma("sp", xt[:], x_l[rowsl, :])
                P.dma("sp", ol[:], olat[rowsl, :])
                rms_stats(P, st, ol[:], junk[:], D)
                P.stt(tmp[:], ol[:], st[:, 3:4], G2[:], ALU.mult, ALU.mult)
                P.tt("pool", x1[:], tmp[:], xt[:], ALU.add)
                P.dma("sp", x1s[rowsl, :], x1[:])
                rms_stats(P, st[:, 4:8], x1[:], junk[:], D)
                P.stt(tmp[:], x1[:], st[:, 7:8], A2[:], ALU.mult, ALU.mult)
                P.tt("dve", hf[:], tmp[:], Sh2[:], ALU.add)
                P.copy("pool", hb[:], hf[:])
                for q4 in range(4):
                    pt = ptb()
                    for k8 in range(8):
                        kc = q4 * 8 + k8
                        P.tr(pt[:, k8 * 128:(k8 + 1) * 128], hb[:, kc * 128:(kc + 1) * 128], idb[:])
                    P.copy("act", hTb[:, q4 * 8:(q4 + 1) * 8, :], pt[:].rearrange("p (k t) -> p k t", k=8))
                P.dma("sp", hTs.rearrange("(k p) t -> p k t", p=128)[:, :, rowsl], hTb[:])
                for q8 in range(8):
                    pt = ptf()
                    for k4 in range(4):
                        kc = q8 * 4 + k4
                        P.tr(pt[:, k4 * 128:(k4 + 1) * 128], hf[:, kc * 128:(kc + 1) * 128], idf[:])
                    P.copy("act" if q8 % 2 else "dve", hTf[:, q8 * 4:(q8 + 1) * 4, :], pt[:].rearrange("p (k t) -> p k t", k=4))
                for kc in range(KC):
                    P.mm(prr[:, 0:NE], hTf[:, kc, :], rwf[:, kc, :], start=(kc == 0), stop=(kc == KC - 1))
                P.act(R["sc"][:], prr[:, 0:NE], AF.Sigmoid)
                P.tt("dve", R["bi"][:], R["sc"][:], rbias[:], ALU.add)
                bv = R["bi"][:].rearrange("p (g s) -> p g s", s=GS)
                P.red(r8["m1"][:], bv, ALU.max)
                P.tt("dve", R["eq"][:].rearrange("p (g s) -> p g s", s=GS), bv, r8["m1"][:].unsqueeze(2).to_broadcast([128, NG, GS]), ALU.is_equal)
                P.stt(R["mk"][:], R["eq"][:], -1e9, R["bi"][:], ALU.mult, ALU.add)
                P.red(r8["m2"][:], R["mk"][:].rearrange("p (g s) -> p g s", s=GS), ALU.max)
                P.tt("dve", r8["gs"][:], r8["m1"][:], r8["m2"][:], ALU.add)
                P.max8(r8["srt"][:], r8["gs"][:])
                P.ts("dve", r8["gm"][:], r8["gs"][:], r8["srt"][:, 3:4], ALU.is_ge)
                P.ts("dve", r8["pen"][:], r8["gm"][:], -1.0, ALU.add, 1e9, ALU.mult)
                P.tt("dve", R["mb"][:].rearrange("p (g s) -> p g s", s=GS), bv, r8["pen"][:].unsqueeze(2).to_broadcast([128, NG, GS]), ALU.add)
                P.max8(r8["srt2"][:], R["mb"][:])
                P.ts("dve", R["sel"][:], R["mb"][:], r8["srt2"][:, 5:6], ALU.is_ge)
                P.tt("dve", R["ga"][:], R["sc"][:], R["sel"][:], ALU.mult)
                P.red(r8["den"][:, 0:1], R["ga"][:], ALU.add)
                P.recip(r8["den"][:, 1:2], r8["den"][:, 0:1])
                P.ts("dve", R["ga"][:], R["ga"][:], r8["den"][:, 1:2], ALU.mult, 2.5, ALU.mult)
                pt = ptf()
                P.tr(pt[0:NE, 0:128], R["ga"][:], idf[:])
                P.copy("act", gT[0:NE, :], pt[0:NE, 0:128])
                P.dma("sp", gscT[:, rowsl], gT[0:NE, :])

    if on(10):
        with P.phase():
            w1a = IN("w1all", [NE1, D, FF])
            w3a = IN("w3all", [NE1, D, FF])
            hTv = hTs.rearrange("(k p) t -> p k t", p=128)
            hTb = P.sb("ehT", [128, KC, 1024], BF16)
            w1s = Rot([P.sb("ew1%d" % i, [128, KC, 256], BF16) for i in range(2)])
            w3s = Rot([P.sb("ew3%d" % i, [128, KC, 256], BF16) for i in range(2)])
            gbs = Rot([P.sb("egb%d" % i, [128, 1024], F32) for i in range(2)])
            p1s = Rot([P.ps("ep1%d" % i, [128, 512]) for i in range(3)])
            p3s = Rot([P.ps("ep3%d" % i, [128, 512]) for i in range(3)])
            sls = Rot([P.sb("esl%d" % i, [128, 512], F32) for i in range(2)])
            hms = Rot([P.sb("ehm%d" % i, [128, 512], F32) for i in range(2)])
            hos = Rot([P.sb("eho%d" % i, [128, 512], BF16) for i in range(3)])
            for t0, tn in splits(0, OWN, 1024):
                P.dma("sp", hTb[:, :, 0:tn], hTv[:, :, t0:t0 + tn])
                for e in range(NE1):
                    gb = None
                    if e < NE:
                        gb = gbs()
                        P.dma("sp", gb[:, 0:tn], gscT[e:e + 1, t0:t0 + tn].partition_broadcast(128))
                    for f0 in (0, 256):
                        w1, w3 = w1s(), w3s()
                        P.dma("pool", w1[:], w1a[e].rearrange("(k p) f -> p k f", p=128)[:, :, f0:f0 + 256])
                        P.dma("pool", w3[:], w3a[e].rearrange("(k p) f -> p k f", p=128)[:, :, f0:f0 + 256])
                        for fh in range(2):
                            fs = slice(fh * 128, (fh + 1) * 128)
                            for s0, sn in splits(0, tn, 512):
                                p1, p3 = p1s(), p3s()
                                for kc in range(KC):
                                    P.mm(p1[:, 0:sn], w1[:, kc, fs], hTb[:, kc, s0:s0 + sn], start=(kc == 0), stop=(kc == KC - 1))
                                for kc in range(KC):
                                    P.mm(p3[:, 0:sn], w3[:, kc, fs], hTb[:, kc, s0:s0 + sn], start=(kc == 0), stop=(kc == KC - 1))
                                sl, ho = sls(), hos()
                                P.act(sl[:, 0:sn], p1[:, 0:sn], AF.Silu)
                                if gb is None:
                                    P.tt("dve", ho[:, 0:sn], sl[:, 0:sn], p3[:, 0:sn], ALU.mult)
                                else:
                                    hm = hms()
                                    P.tt("dve", hm[:, 0:sn], sl[:, 0:sn], p3[:, 0:sn], ALU.mult)
                                    P.tt("pool", ho[:, 0:sn], hm[:, 0:sn], gb[:, s0:s0 + sn], ALU.mult)
                                r0 = e * FF + f0 + fh * 128
                                P.dma("sp", hbs[r0:r0 + 128, t0 + s0:t0 + s0 + sn], ho[:, 0:sn])

    if on(11):
        with P.phase():
            w2a = IN("w2all", [NE1 * FF, D])
            NCK = NE1 * FF // 128
            GK = 20
            hbv = hbs.rearrange("(c p) t -> p c t", p=128)
            w2v = w2a.rearrange("(c p) n -> p c n", p=128)
            hbg = Rot([P.sb("dhb%d" % i, [128, GK, 1024], BF16) for i in range(2)])
            wbs = Rot([P.sb("dw%d" % i, [128, GK, 512], BF16) for i in range(2)])
            yacc = P.sb("yacc", [128, 8, 512], F32)
            pss = Rot([P.ps("dp%d" % i, [128, 512]) for i in range(4)])
            for t0, tn in splits(0, OWN, 1024):
                ntt = tn // 128
                for c0, cn in splits(0, D, 512):
                    for gi, (k0, kn) in enumerate(splits(0, NCK, GK)):
                        hg, wb = hbg(), wbs()
                        P.dma("sp", hg[:, 0:kn, 0:tn], hbv[:, k0:k0 + kn, t0:t0 + tn])
                        P.dma("pool", wb[:, 0:kn, :], w2v[:, k0:k0 + kn, c0:c0 + cn])
                        for tt in range(ntt):
                            ps = pss()
                            for k in range(kn):
                                P.mm(ps[:], hg[:, k, tt * 128:(tt + 1) * 128], wb[:, k, :], start=(k == 0), stop=(k == kn - 1))
                            if gi == 0:
                                P.copy("act", yacc[:, tt, :], ps[:])
                            else:
                                P.tt("dve", yacc[:, tt, :], ps[:], yacc[:, tt, :], ALU.add)
                    P.dma("sp", ymoe[t0:t0 + tn, c0:c0 + cn].rearrange("(t p) n -> p t n", p=128), yacc[:, 0:ntt, :])

    if on(12):
        with P.phase():
            G5 = P.sb("G5", [128, D], F32)
            tmp = P.sb("tmp", [128, D], F32)
            P.dma("sp", G5[:], modscr[0:1, 5 * D:6 * D].partition_broadcast(128))
            P.dma("sp", tmp[:], IN("g_post_ffn", [1, D])[0:1, :].partition_broadcast(128))
            P.tt("dve", G5[:], G5[:], tmp[:], ALU.mult)
            yms = Rot([P.sb("ym%d" % i, [128, D], F32) for i in range(2)])
            x1b = Rot([P.sb("x1b%d" % i, [128, D], F32) for i in range(2)])
            ots = Rot([P.sb("ot%d" % i, [128, D], F32) for i in range(2)])
            junk = P.sb("junk", [128, D], BF16)
            sts = Rot([P.sb("fst%d" % i, [128, 4], F32) for i in range(2)])
            for ti in range(OWN // 128):
                rowsl = slice(ti * 128, (ti + 1) * 128)
                ym, x1, ot, st = yms(), x1b(), ots(), sts()
                P.dma("sp", ym[:], ymoe[rowsl, :])
                P.dma("sp", x1[:], x1s[rowsl, :])
                rms_stats(P, st, ym[:], junk[:], D)
                P.stt(tmp[:], ym[:], st[:, 3:4], G5[:], ALU.mult, ALU.mult)
                P.tt("pool", ot[:], tmp[:], x1[:], ALU.add)
                P.dma("sp", out_d[rowsl, :], ot[:])
    elif cfg.get("dummy_out", True):
        with P.phase():
            z = P.sb("z", [128, 64], F32)
            P.memset("dve", z[:], 0.0)
            P.dma("sp", out_d[0:128, 0:64], z[:])
    P.close()
    P.in_names = in_names
    return nc, P


def qk_perm():
    idx = np.arange(IN_COLS)
    blk = np.concatenate([np.arange(0, 128, 2), np.arange(1, 128, 2)])
    for base in range(0, 2 * DA_W, 128):
        idx[base:base + 128] = base + blk
    return idx


def rope_tables(cfg, j):
    S, CTX, GW = cfg["S"], cfg["CTX"], cfg["GW"]
    L = S + CTX
    l = np.arange(S)
    t = l if j == 0 else (S - 1 - l)
    row = (t // GW).astype(np.float32)
    col = (t % GW).astype(np.float32)
    inv = np.power(np.float32(10000.0), -np.arange(32, dtype=np.float32) / np.float32(32)).astype(np.float32)
    ang = np.concatenate([row[:, None] * inv, col[:, None] * inv], axis=-1).astype(np.float32)
    cos, sin = np.cos(ang).astype(np.float32), np.sin(ang).astype(np.float32)
    cosT = np.ones((128, L), np.float32)
    sinT = np.zeros((128, L), np.float32)
    cosT[0:64, :S] = cos.T
    cosT[64:128, :S] = cos.T
    sinT[0:64, :S] = -sin.T
    sinT[64:128, :S] = sin.T
    return cosT, sinT


def chunk_masks():
    i = np.arange(128)
    m = np.zeros((2, 128, 384), np.float32)
    r, c = i[:, None], i[None, :]
    m[0, :, 0:128] = (c > r)
    m[0, :, 128:256] = (c >= r)
    m[0, :, 256:384] = (r > c)
    m[1, :, 0:128] = (c < r)
    m[1, :, 128:256] = (c <= r)
    m[1, :, 256:384] = (r < c)
    return m


def host_inputs(inputs, cfg, names=None):
    S, CTX = cfg["S"], cfg["CTX"]
    g = lambda k: np.asarray(inputs[k]) if k in inputs else None
    want = (lambda n: True) if names is None else (lambda n: n in names)
    shared = {}
    if want("w_in"):
        shared["w_in"] = np.ascontiguousarray(g("w_in")[0][:, qk_perm()])
    for k in ("w_mod", "w_out", "rw_g2", "router_w"):
        if want(k):
            shared[k] = np.ascontiguousarray(g(k)[0])
    for k in ("g_pre_attn", "g_post_attn", "g_pre_ffn", "g_post_ffn", "da_subln", "router_bias"):
        if want(k):
            shared[k] = np.ascontiguousarray(g(k).reshape(1, -1))
    if want("b_mod"):
        shared["b_mod"] = np.ascontiguousarray(g("b_mod")[0][None, :])
    if want("da_lambda"):
        shared["da_lambda"] = np.ascontiguousarray(g("da_lambda")[0].reshape(1, 512))
    if want("w1all"):
        shared["w1all"] = np.concatenate([g("exp_w1")[0], g("sh_w1")], axis=0)
    if want("w3all"):
        shared["w3all"] = np.concatenate([g("exp_w3")[0], g("sh_w3")], axis=0)
    if want("w2all"):
        e2 = g("exp_w2")[0]
        shared["w2all"] = np.concatenate([e2.reshape(-1, D), g("sh_w2")[0]], axis=0)
    shared["ident"] = np.eye(128, dtype=np.float32)
    pm = np.zeros((128, 128), np.float32)
    for m in range(128):
        pm[(m + 64) % 128, m] = 1.0
    shared["pm"] = pm
    bo = np.zeros((128, 128), np.float32)
    bo[:64, :64] = 1.0
    bo[64:, 64:] = 1.0
    shared["bo"] = bo
    shared["masks"] = chunk_masks()
    maps = []
    for c in range(cfg.get("NCORES", 8)):
        b, j = c // 2, c % 2
        m = dict(shared)
        if want("x_l"):
            xb, cb = g("x")[b], g("ctx")[b]
            if j == 1:
                xb, cb = xb[::-1], cb[::-1]
            m["x_l"] = np.ascontiguousarray(np.concatenate([xb, cb], axis=0))
        if want("cvec"):
            cv = np.stack([g("c")[b], g("c_ctx")], axis=-1)
            m["cvec"] = np.ascontiguousarray(cv.reshape(KC, 128, 2).transpose(1, 0, 2))
        if want("cosT") or want("sinT"):
            m["cosT"], m["sinT"] = rope_tables(cfg, j)
        dsel = [j, 1 - j]
        if want("shAB"):
            sh = g("rw_shift")[0]
            ab = np.zeros((NRT * 128, 2), np.float32)
            ab[:RW_COLS, 0] = sh[dsel[0]]
            ab[:RW_COLS, 1] = sh[dsel[1]]
            m["shAB"] = np.ascontiguousarray(ab.reshape(NRT, 128, 2).transpose(1, 0, 2))
        if want("chp"):
            cp = np.zeros((RW_W, NPAR), np.float32)
            cp[:, PK_K] = g("rw_k_k")[0]
            cp[:, PK_A] = g("rw_k_a")[0]
            cp[:, PR_K] = g("rw_r_k")[0].reshape(-1)
            cp[:, PLNW] = g("rw_ln_w")[0]
            cp[:, PLNB] = g("rw_ln_b")[0]
            for i in range(2):
                cp[:, PW0 + i] = g("rw_w0")[0][dsel[i]]
                cp[:, PA0 + i] = g("rw_a0")[0][dsel[i]]
            m["chp"] = np.ascontiguousarray(cp.reshape(16, 128, NPAR).transpose(1, 0, 2))
        if want("rw_w2d"):
            m["rw_w2d"] = np.ascontiguousarray(g("rw_w2")[0][dsel])
        if want("rw_a2d"):
            m["rw_a2d"] = np.ascontiguousarray(g("rw_a2")[0][dsel])
        if names is not None:
            m = {k: v for k, v in m.items() if k in names}
        maps.append(m)
    return maps


def assemble(results, cfg):
    S = cfg["S"]
    OWN = S // 2
    out = np.zeros((cfg["B"], S, D), np.float32)
    for c, r in enumerate(results):
        b, j = c // 2, c % 2
        o = r["out"]
        if j == 0:
            out[b, :OWN] = o
        else:
            out[b, S - 1 - np.arange(OWN)] = o
    return out


def kernel(**inputs):
    cfg = FULL_CFG
    nc, P = build_program(cfg)
    maps = host_inputs(inputs, cfg, names=set(P.in_names))
    res = run_bass_kernel_spmd(nc, maps, core_ids=list(range(8)))
    return assemble(res.results, cfg)
```

```python
import numpy as np
from contextlib import ExitStack
import concourse.bass as bass
import concourse.mybir as mybir
from concourse.bass_utils import run_bass_kernel_spmd

F32 = mybir.dt.float32
BF16 = mybir.dt.bfloat16
F32R = mybir.dt.float32r
AF = mybir.ActivationFunctionType
ALU = mybir.AluOpType
AX = mybir.AxisListType

D = 4096
KC = D // 128
N_MOD = 6
DA_W = 2048
RW_W = 2048
RW_COLS = 3 * RW_W + 128 + 128 + 480
IN_COLS = 3 * DA_W + RW_COLS
NORM_EPS = 1e-6
GN_EPS = 64e-5
LAM_INIT = 0.2
N_EXP = 64
FF = 512

FULL_CFG = dict(B=4, S=4096, CTX=256, GW=64)


class Buf:
    __slots__ = ("name", "lw", "rd")

    def __init__(self, name):
        self.name = name
        self.lw = None
        self.rd = []


class Op:
    __slots__ = ("eng", "fn", "r", "w", "dma", "need", "sig", "eidx", "fn_deps")

    def __init__(self, eng, fn, r, w, dma):
        self.eng, self.fn, self.r, self.w, self.dma = eng, fn, r, w, dma
        self.need = False
        self.sig = None
        self.eidx = 0


class Prog:
    ENGS = ("pe", "act", "dve", "pool", "sp")
    RING = {"sp": 8, "pool": 6, "act": 4}

    def __init__(self, nc):
        self.nc = nc
        self.e = {"pe": nc.tensor, "act": nc.scalar, "dve": nc.vector, "pool": nc.gpsimd, "sp": nc.sync}
        self.es = ExitStack()
        self.sems = []

        def mk(name):
            h = self.es.enter_context(nc.semaphore(name))
            self.sems.append(h)
            return len(self.sems) - 1

        self.csem = {e: mk("c_" + e) for e in ("pe", "act", "dve", "pool")}
        self.ccnt = {e: 0 for e in self.csem}
        self.ring = {q: [mk("d_%s%d" % (q, i)) for i in range(n)] for q, n in self.RING.items()}
        self.ringval = {q: [0] * n for q, n in self.RING.items()}
        self.dcnt = {q: 0 for q in self.RING}
        self.waited = {e: {} for e in self.ENGS}
        self.ops = []
        self.reg = {}
        self.pstack = None
        self.ecount = {e: 0 for e in self.ENGS}
        self.n_inst = 0
        self.pid = 0

    def sb(self, name, shape, dtype):
        name = "p%d_%s" % (self.pid, name)
        t = self.pstack.enter_context(self.nc.sbuf_tensor(name, list(shape), dtype))
        self.reg[name] = Buf(name)
        return t

    def ps(self, name, shape, dtype=F32):
        name = "p%d_%s" % (self.pid, name)
        t = self.pstack.enter_context(self.nc.psum_tensor(name, list(shape), dtype))
        self.reg[name] = Buf(name)
        return t

    def _bufs(self, aps):
        out = []
        for a in aps:
            if a is None or isinstance(a, (int, float)):
                continue
            b = self.reg.get(a.tensor.name)
            if b is not None and b not in out:
                out.append(b)
        return out

    def add(self, eng, fn, outs, ins, dma=False):
        self.ops.append(Op(eng, fn, self._bufs(ins), self._bufs(outs), dma))

    def dma(self, q, out, in_, slow=False):
        e = self.e[q]
        if slow:
            self.add(q, lambda: e.dma_start(out=out, in_=in_, allow_slow_non_contiguous=True), [out], [in_], dma=True)
        else:
            self.add(q, lambda: e.dma_start(out=out, in_=in_), [out], [in_], dma=True)

    def mm(self, out, lhsT, rhs, start=True, stop=True):
        self.add("pe", lambda: self.nc.tensor.matmul(out, lhsT=lhsT, rhs=rhs, start=start, stop=stop), [out], [lhsT, rhs])

    def tr(self, out, in_, ident):
        self.add("pe", lambda: self.nc.tensor.transpose(out, in_, ident), [out], [in_, ident])

    def act(self, out, in_, func, bias=None, scale=None, accum_out=None):
        kw = {}
        if bias is not None:
            kw["bias"] = bias
        if scale is not None:
            kw["scale"] = scale
        if accum_out is not None:
            kw["accum_out"] = accum_out
        self.add("act", lambda: self.nc.scalar.activation(out=out, in_=in_, func=func, **kw),
                 [out, accum_out], [in_, bias, scale])

    def tt(self, eng, out, in0, in1, op):
        e = self.e[eng]
        self.add(eng, lambda: e.tensor_tensor(out=out, in0=in0, in1=in1, op=op), [out], [in0, in1])

    def ts(self, eng, out, in0, s1, op0, s2=None, op1=None, accum_out=None):
        e = self.e[eng]
        kw = {}
        if op1 is not None:
            kw["op1"] = op1
        if accum_out is not None:
            kw["accum_out"] = accum_out
        self.add(eng, lambda: e.tensor_scalar(out=out, in0=in0, scalar1=s1, scalar2=s2, op0=op0, **kw),
                 [out, accum_out], [in0, s1, s2])

    def stt(self, out, in0, scalar, in1, op0, op1):
        self.add("dve", lambda: self.nc.vector.scalar_tensor_tensor(out=out, in0=in0, scalar=scalar, in1=in1, op0=op0, op1=op1),
                 [out], [in0, scalar, in1])

    def copy(self, eng, out, in_):
        if eng == "act":
            self.add("act", lambda: self.nc.scalar.copy(out=out, in_=in_), [out], [in_])
        else:
            e = self.e[eng]
            self.add(eng, lambda: e.tensor_copy(out=out, in_=in_), [out], [in_])

    def red(self, out, in_, op, axis=None, negate=None):
        ax = AX.X if axis is None else axis
        kw = {}
        if negate is not None:
            kw["negate"] = negate
        self.add("dve", lambda: self.nc.vector.tensor_reduce(out=out, in_=in_, axis=ax, op=op, **kw), [out], [in_])

    def recip(self, out, in_):
        self.add("dve", lambda: self.nc.vector.reciprocal(out=out, in_=in_), [out], [in_])

    def memset(self, eng, ap, val):
        e = self.e[eng]
        self.add(eng, lambda: e.memset(ap, val), [ap], [])

    def scan(self, out, d0, d1, initial, op0, op1):
        self.add("dve", lambda: self.nc.vector.tensor_tensor_scan(out=out, data0=d0, data1=d1, initial=initial, op0=op0, op1=op1),
                 [out], [d0, d1, initial])

    def max8(self, out, in_):
        self.add("dve", lambda: self.nc.vector.max(out=out, in_=in_), [out], [in_])

    def phase(self):
        prog = self

        class _Ph:
            def __enter__(s):
                prog.pid += 1
                prog.pstack = ExitStack()
                prog.pstack.__enter__()
                return prog

            def __exit__(s, et, ev, tb):
                if et is None:
                    prog.flush()
                prog.pstack.__exit__(et, ev, tb)
                prog.pstack = None
                prog.reg = {}
                return False

        return _Ph()

    def _wait(self, eng, sem, val):
        if val <= 0:
            return
        if self.waited[eng].get(sem, 0) >= val:
            return
        self.e[eng].wait_ge(self.sems[sem], val)
        self.waited[eng][sem] = val
        self.n_inst += 1

    def flush(self):
        ops = self.ops
        self.ops = []
        ecnt = dict(self.ecount)
        last_compute = {}
        for op in ops:
            ecnt[op.eng] += 1
            op.eidx = ecnt[op.eng]
            deps = []
            for b in op.r:
                if b.lw is not None:
                    deps.append((b.lw, "raw"))
            for b in op.w:
                if b.lw is not None:
                    deps.append((b.lw, "waw"))
                for r in b.rd:
                    deps.append((r, "war"))
            keep = []
            for d, kind in deps:
                if d is op:
                    continue
                if d.eng == op.eng and not d.dma:
                    if op.dma:
                        pass
                    elif op.eng == "pe":
                        continue
                    elif kind != "raw":
                        continue
                    elif op.eidx - d.eidx > 6:
                        continue
                if d not in keep:
                    keep.append(d)
            best = {}
            final = []
            for d in keep:
                if d.dma:
                    final.append(d)
                elif d.eng not in best or d.eidx > best[d.eng].eidx:
                    best[d.eng] = d
            keep = final + list(best.values())
            for d in keep:
                d.need = True
            op.fn_deps = keep
            for b in op.r:
                b.rd.append(op)
            for b in op.w:
                b.lw = op
                b.rd = []
            if not op.dma:
                last_compute[op.eng] = op
        for op in last_compute.values():
            op.need = True
        for op in ops:
            eng = op.eng
            for d in op.fn_deps:
                sem, val = d.sig
                self._wait(eng, sem, val)
            if op.dma:
                n = len(self.ring[eng])
                i = self.dcnt[eng] % n
                self.dcnt[eng] += 1
                sem = self.ring[eng][i]
                self._wait(eng, sem, self.ringval[eng][i])
                inst = op.fn()
                self.ringval[eng][i] += 16
                inst.then_inc(self.sems[sem], 16)
                op.sig = (sem, self.ringval[eng][i])
            else:
                inst = op.fn()
                if op.need:
                    self.ccnt[eng] += 1
                    inst.then_inc(self.sems[self.csem[eng]], 1)
                    op.sig = (self.csem[eng], self.ccnt[eng])
            self.n_inst += 1
            self.ecount[eng] += 1
        self.barrier()

    def barrier(self):
        for eng in self.ENGS:
            for c in self.csem:
                if c != eng:
                    self._wait(eng, self.csem[c], self.ccnt[c])
            for q in self.ring:
                for i, sem in enumerate(self.ring[q]):
                    self._wait(eng, sem, self.ringval[q][i])

    def close(self):
        self.es.close()


def splits(n0, n1, w):
    out = []
    c = n0
    while c < n1:
        n = min(w, n1 - c)
        out.append((c, n))
        c += n
    return out


class Rot:
    def __init__(self, lst):
        self.lst = lst
        self.i = 0

    def __call__(self):
        r = self.lst[self.i % len(self.lst)]
        self.i += 1
        return r


EXPM05 = 0.6065306597126334
NPAR = 10
PK_K, PK_A, POMKA, PR_K, PLNW, PLNB, PW0, PA0 = 0, 1, 2, 3, 4, 5, 6, 8
NRT = 54


def build_program(cfg, debug=()):
    S, CTX = cfg["S"], cfg["CTX"]
    OWN = S // 2
    L = S + CTX
    NCH = L // 128
    NE = cfg.get("NE", N_EXP)
    NG = 8
    GS = NE // NG
    NE1 = NE + 1
    phases = cfg.get("phases", None)
    feed = cfg.get("feed", ())
    nc = bass.Bass("TRN2", target_bir_lowering=False)
    in_names = []
    in_cache = {}

    def on(ph):
        return phases is None or ph in phases

    def IN(name, shape, dt=F32):
        if name not in in_cache:
            in_names.append(name)
            in_cache[name] = nc.dram_tensor(name, list(shape), dt, kind="ExternalInput").ap()
        return in_cache[name]

    def scratch(name, shape, dt):
        if name in feed:
            return IN(name, shape, dt)
        kind = "ExternalOutput" if name in debug else "Internal"
        return nc.dram_tensor(name, list(shape), dt, kind=kind).ap()

    out_d = nc.dram_tensor("out", [OWN, D], F32, kind="ExternalOutput").ap()

    modscr = scratch("modscr", [2, N_MOD * D], F32)
    uT = scratch("uT", [D, L], BF16)
    qT = scratch("qT", [DA_W, OWN], BF16)
    kT = scratch("kT", [DA_W, L], BF16)
    vS = scratch("vS", [L, DA_W], BF16)
    rwT = scratch("rwT", [RW_COLS, L], BF16)
    oT = scratch("oT", [D, OWN], BF16)
    sgT = scratch("sgT", [512, OWN], BF16)
    AhT = [scratch("AhT%d" % d, [RW_W, L], BF16) for d in range(2)]
    RhT = [scratch("RhT%d" % d, [RW_W, L], BF16) for d in range(2)]
    BtT = [scratch("BtT%d" % d, [RW_W, L], BF16) for d in range(2)]
    KtT = [scratch("KtT%d" % d, [RW_W, L], BF16) for d in range(2)]
    Btok = [scratch("Btok%d" % d, [L, RW_W], BF16) for d in range(2)]
    Ktok = [scratch("Ktok%d" % d, [L, RW_W], BF16) for d in range(2)]
    Vtok = scratch("Vtok", [L, RW_W], BF16)
    Wc = [scratch("Wc%d" % d, [RW_W, NCH], F32) for d in range(2)]
    bonusT = scratch("bonusT", [RW_W, OWN], F32)
    Gs = [scratch("Gs%d" % d, [NCH, 128, 32 * 128], BF16) for d in range(2)]
    Ns = [scratch("Ns%d" % d, [NCH, 128, 32 * 128], BF16) for d in range(2)]
    Z0s = [scratch("Z0s%d" % d, [NCH, 128, 32 * 64], F32) for d in range(2)]
    Y0s = [scratch("Y0s%d" % d, [NCH, 128, 16 * 128], F32) for d in range(2)]
    ysc = [scratch("ysc%d" % d, [RW_W, OWN], F32) for d in range(2)]
    olat = scratch("olat", [OWN, D], F32)
    x1s = scratch("x1s", [OWN, D], F32)
    hTs = scratch("hTs", [D, OWN], BF16)
    gscT = scratch("gscT", [NE, OWN], F32)
    hbs = scratch("hbs", [NE1 * FF, OWN], BF16)
    ymoe = scratch("ymoe", [OWN, D], F32)

    P = Prog(nc)

    def load_ident(P):
        idf = P.sb("idf", [128, 128], F32)
        idb = P.sb("idb", [128, 128], BF16)
        P.dma("sp", idf[:], IN("ident", [128, 128])[:, :])
        P.copy("dve", idb[:], idf[:])
        return idf, idb

    def rms_stats(P, s, x, junk, n):
        P.act(junk, x, AF.Square, accum_out=s[:, 0:1])
        P.ts("dve", s[:, 1:2], s[:, 0:1], 1.0 / n, ALU.mult, NORM_EPS, ALU.add)
        P.act(s[:, 2:3], s[:, 1:2], AF.Sqrt)
        P.recip(s[:, 3:4], s[:, 2:3])

    if on(0):
        with P.phase():
            cv = P.sb("cv", [128, KC, 2], F32)
            cs = P.sb("cs", [128, KC, 2], F32)
            P.dma("sp", cv[:], IN("cvec", [128, KC, 2])[:, :, :])
            P.act(cs[:], cv[:], AF.Silu)
            w_mod = IN("w_mod", [D, N_MOD * D])
            b_mod = IN("b_mod", [1, N_MOD * D])
            wv = w_mod.rearrange("(k p) n -> p k n", p=128)
            NB = 256
            wts = [P.sb("wm%d" % i, [128, KC, NB], F32) for i in range(2)]
            pss = [P.ps("pm%d" % i, [128, 512]) for i in range(2)]
            bms = [P.sb("bm%d" % i, [2, NB], F32) for i in range(2)]
            mos = [P.sb("mo%d" % i, [2, NB], F32) for i in range(2)]
            for blk in range(N_MOD * D // NB):
                wt, ps, bm, mo = wts[blk % 2], pss[blk % 2], bms[blk % 2], mos[blk % 2]
                c0 = blk * NB
                P.dma("sp", wt[:], wv[:, :, c0:c0 + NB])
                P.dma("sp", bm[:], b_mod[0:1, c0:c0 + NB].partition_broadcast(2))
                for kc in range(KC):
                    P.mm(ps[0:2, 0:NB], cs[:, kc, :], wt[:, kc, :], start=(kc == 0), stop=(kc == KC - 1))
                P.tt("dve", mo[:], ps[0:2, 0:NB], bm[:], ALU.add)
                P.dma("sp", modscr[:, c0:c0 + NB], mo[:])

    if on(1):
        with P.phase():
            x_l = IN("x_l", [L, D])
            idf, idb = load_ident(P)
            A = P.sb("A", [128, D], F32)
            Sh = P.sb("Sh", [128, D], F32)
            gb = P.sb("gb", [128, D], F32)
            P.dma("sp", gb[:], IN("g_pre_attn", [1, D])[0:1, :].partition_broadcast(128))
            xs = [P.sb("x%d" % i, [128, D], F32) for i in range(2)]
            tmp = P.sb("tmp", [128, D], F32)
            ub = [P.sb("ub%d" % i, [128, D], BF16) for i in range(2)]
            sq = P.sb("sq", [128, D], BF16)
            st = [P.sb("st%d" % i, [128, 4], F32) for i in range(2)]
            uTs = P.sb("uTs", [128, KC, 512], BF16)
            pts = [P.ps("pt%d" % i, [128, 1024], BF16) for i in range(4)]
            uTv = uT.rearrange("(k p) l -> p k l", p=128)
            cur_var = None
            for g0, gn in splits(0, L // 128, 4):
                for ti in range(g0, g0 + gn):
                    var = 0 if ti * 128 < S else 1
                    if var != cur_var:
                        cur_var = var
                        P.dma("sp", Sh[:], modscr[var:var + 1, 0:D].partition_broadcast(128))
                        P.dma("sp", A[:], modscr[var:var + 1, D:2 * D].partition_broadcast(128))
                        P.stt(A[:], A[:], 1.0, gb[:], ALU.add, ALU.mult)
                    x, s, u = xs[ti % 2], st[ti % 2], ub[ti % 2]
                    P.dma("sp", x[:], x_l[ti * 128:(ti + 1) * 128, :])
                    rms_stats(P, s, x[:], sq[:], D)
                    P.stt(tmp[:], x[:], s[:, 3:4], A[:], ALU.mult, ALU.mult)
                    P.tt("pool", u[:], tmp[:], Sh[:], ALU.add)
                    for q4 in range(4):
                        pt = pts[q4]
                        for k8 in range(8):
                            kc = q4 * 8 + k8
                            P.tr(pt[:, k8 * 128:(k8 + 1) * 128], u[:, kc * 128:(kc + 1) * 128], idb[:])
                        dst = uTs[:, q4 * 8:(q4 + 1) * 8, (ti - g0) * 128:(ti - g0 + 1) * 128]
                        P.copy("act" if q4 % 2 else "dve", dst, pt[:].rearrange("p (k t) -> p k t", k=8))
                P.dma("sp", uTv[:, :, g0 * 128:(g0 + gn) * 128], uTs[:, :, 0:gn * 128])

    if on(2):
        with P.phase():
            w_in = IN("w_in", [D, IN_COLS])
            cos_d = IN("cosT", [128, L])
            sin_d = IN("sinT", [128, L])
            pmf = P.sb("pmf", [128, 128], F32)
            pmb = P.sb("pmb", [128, 128], BF16)
            P.dma("sp", pmf[:], IN("pm", [128, 128])[:, :])
            P.copy("dve", pmb[:], pmf[:])
            TBMAX = 1152
            uTb = P.sb("uTb", [128, KC, TBMAX], BF16)
            cosb = P.sb("cosb", [128, TBMAX], F32)
            sinb = P.sb("sinb", [128, TBMAX], F32)
            wbs = Rot([P.sb("wb%d" % i, [128, KC, 256], BF16) for i in range(2)])
            pss = Rot([P.ps("pp%d" % i, [128, 512]) for i in range(3)])
            ps2s = Rot([P.ps("pq%d" % i, [128, 512]) for i in range(2)])
            pTs = Rot([P.sb("pT%d" % i, [128, 512], BF16) for i in range(2)])
            t1s = Rot([P.sb("t1%d" % i, [128, 512], F32) for i in range(2)])
            t2s = Rot([P.sb("t2%d" % i, [128, 512], F32) for i in range(2)])
            obs = Rot([P.sb("ob%d" % i, [128, 512], BF16) for i in range(3)])
            uTv = uT.rearrange("(k p) l -> p k l", p=128)
            wv = w_in.rearrange("(k p) n -> p k n", p=128)
            blocks = [(t0, tn, True) for t0, tn in splits(0, OWN, 1024)] + \
                     [(t0, tn, False) for t0, tn in splits(OWN, L, TBMAX)]
            for (t0, tn, is_own) in blocks:
                P.dma("sp", uTb[:, :, 0:tn], uTv[:, :, t0:t0 + tn])
                P.dma("sp", cosb[:, 0:tn], cos_d[:, t0:t0 + tn])
                P.dma("sp", sinb[:, 0:tn], sin_d[:, t0:t0 + tn])
                groups = []
                if is_own:
                    groups += [("q", c, n) for c, n in splits(0, DA_W, 256)]
                groups += [("k", c, n) for c, n in splits(DA_W, 2 * DA_W, 256)]
                groups += [("v", c, n) for c, n in splits(2 * DA_W, 3 * DA_W, 256)]
                groups += [("rw", c, n) for c, n in splits(3 * DA_W, IN_COLS, 256)]
                for kind, c0, cn in groups:
                    wb = wbs()
                    P.dma("pool", wb[:, :, 0:cn], wv[:, :, c0:c0 + cn])
                    if kind == "v":
                        for tt0, _ in splits(0, tn, 128):
                            ps = pss()
                            for kc in range(KC):
                                P.mm(ps[:, 0:cn], uTb[:, kc, tt0:tt0 + 128], wb[:, kc, 0:cn], start=(kc == 0), stop=(kc == KC - 1))
                            ob = obs()
                            P.copy("act", ob[:, 0:cn], ps[:, 0:cn])
                            P.dma("sp", vS[t0 + tt0:t0 + tt0 + 128, c0 - 2 * DA_W:c0 - 2 * DA_W + cn], ob[:, 0:cn])
                        continue
                    for h0, hn in splits(0, cn, 128):
                        for s0, sn in splits(0, tn, 512):
                            ps = pss()
                            for kc in range(KC):
                                P.mm(ps[0:hn, 0:sn], wb[:, kc, h0:h0 + hn], uTb[:, kc, s0:s0 + sn], start=(kc == 0), stop=(kc == KC - 1))
                            ob = obs()
                            if kind == "rw":
                                P.copy("act", ob[0:hn, 0:sn], ps[0:hn, 0:sn])
                                r0 = c0 + h0 - 3 * DA_W
                                P.dma("sp", rwT[r0:r0 + hn, t0 + s0:t0 + s0 + sn], ob[0:hn, 0:sn])
                            else:
                                pT = pTs()
                                P.copy("act", pT[:, 0:sn], ps[:, 0:sn])
                                ps2 = ps2s()
                                P.mm(ps2[:, 0:sn], pmb[:], pT[:, 0:sn])
                                t1, t2 = t1s(), t2s()
                                P.tt("dve", t1[:, 0:sn], pT[:, 0:sn], cosb[:, s0:s0 + sn], ALU.mult)
                                P.tt("dve", t2[:, 0:sn], ps2[:, 0:sn], sinb[:, s0:s0 + sn], ALU.mult)
                                P.tt("dve", ob[:, 0:sn], t1[:, 0:sn], t2[:, 0:sn], ALU.add)
                                if kind == "q":
                                    r0 = c0 + h0
                                    P.dma("sp", qT[r0:r0 + 128, t0 + s0:t0 + s0 + sn], ob[:, 0:sn])
                                else:
                                    r0 = c0 + h0 - DA_W
                                    P.dma("sp", kT[r0:r0 + 128, t0 + s0:t0 + s0 + sn], ob[:, 0:sn])

    if on(3):
        with P.phase():
            idf, idb = load_ident(P)
            dl = P.sb("dl", [128, 512], F32)
            P.dma("sp", dl[:], IN("da_lambda", [1, 512])[0:1, :].partition_broadcast(128))
            pr = P.sb("pr", [128, 256], F32)
            lm = P.sb("lm", [128, 8], F32)
            P.tt("dve", pr[:, 0:128], dl[:, 0:128], dl[:, 128:256], ALU.mult)
            P.tt("dve", pr[:, 128:256], dl[:, 256:384], dl[:, 384:512], ALU.mult)
            P.red(lm[:, 0:1], pr[:, 0:128], ALU.add)
            P.red(lm[:, 1:2], pr[:, 128:256], ALU.add)
            P.act(lm[:, 2:4], lm[:, 0:2], AF.Exp)
            P.tt("dve", lm[:, 4:5], lm[:, 2:3], lm[:, 3:4], ALU.subtract)
            P.ts("dve", lm[:, 5:6], lm[:, 4:5], LAM_INIT, ALU.add, -1.0, ALU.mult)
            sg = P.sb("sg", [128, 256], F32)
            P.dma("sp", sg[:], IN("da_subln", [1, 256])[0:1, :].partition_broadcast(128))
            P.ts("dve", sg[:], sg[:], 1.0 - LAM_INIT, ALU.mult)
            NKT = L // 128
            SC = 128.0 ** -0.5
            kTv = kT.rearrange("(h m d) l -> h d m l", m=2, d=128)
            qTv = qT.rearrange("(h m d) l -> h d m l", m=2, d=128)
            vSv = vS.rearrange("(kt p) (h e) -> h p kt e", p=128, e=256)
            sets = []
            for i in range(2):
                kth = P.sb("kth%d" % i, [128, 2, L], BF16)
                qth = P.sb("qth%d" % i, [128, 2, OWN], BF16)
                vh = P.sb("vh%d" % i, [128, NKT, 257], BF16)
                P.memset("pool", vh[:, :, 256:257], 1.0)
                sets.append((kth, qth, vh))
            pss = Rot([P.ps("as%d" % i, [128, 512]) for i in range(2)])
            acc = [P.ps("ac%d" % i, [128, 512]) for i in range(4)]
            ptrs = Rot([P.ps("atr", [128, 1024], BF16)])
            PTs = Rot([P.sb("PT%d" % i, [128, 512], BF16) for i in range(3)])
            o0 = [P.sb("o0%d" % i, [128, 256], F32) for i in range(4)]
            sts = [P.sb("ast%d" % i, [128, 8], F32) for i in range(4)]
            junk = P.sb("ajunk", [128, 256], BF16)
            ons = Rot([P.sb("on%d" % i, [128, 256], BF16) for i in range(2)])
            oTss = Rot([P.sb("oTs%d" % i, [128, 2, 512], BF16) for i in range(2)])
            for h in range(8):
                kth, qth, vh = sets[h % 2]
                P.dma("sp", kth[:], kTv[h])
                P.dma("sp", qth[:], qTv[h])
                P.dma("sp", vh[:, :, 0:256], vSv[h])
                for q0, qn in splits(0, OWN, 512):
                    nqt = qn // 128
                    for m in range(2):
                        for kt in range(NKT):
                            ps = pss()
                            P.mm(ps[:, 0:qn], kth[:, m, kt * 128:(kt + 1) * 128], qth[:, m, q0:q0 + qn])
                            pt = PTs()
                            P.act(pt[:, 0:qn], ps[:, 0:qn], AF.Exp, scale=SC)
                            for qt in range(nqt):
                                P.mm(acc[qt][:, 0:257], pt[:, qt * 128:(qt + 1) * 128], vh[:, kt, :],
                                     start=(kt == 0), stop=(kt == NKT - 1))
                        for qt in range(nqt):
                            st = sts[qt]
                            P.recip(st[:, m:m + 1], acc[qt][:, 256:257])
                            if m == 0:
                                P.ts("dve", o0[qt][:], acc[qt][:, 0:256], st[:, 0:1], ALU.mult)
                            else:
                                P.tt("dve", st[:, 2:3], st[:, 1:2], lm[:, 5:6], ALU.mult)
                                P.stt(o0[qt][:], acc[qt][:, 0:256], st[:, 2:3], o0[qt][:], ALU.mult, ALU.add)
                    oTs = oTss()
                    for qt in range(nqt):
                        st = sts[qt]
                        P.act(junk[:], o0[qt][:], AF.Square, accum_out=st[:, 3:4])
                        P.ts("dve", st[:, 4:5], st[:, 3:4], 1.0 / 256, ALU.mult, NORM_EPS, ALU.add)
                        P.act(st[:, 5:6], st[:, 4:5], AF.Sqrt)
                        P.recip(st[:, 6:7], st[:, 5:6])
                        on_ = ons()
                        P.stt(on_[:], o0[qt][:], st[:, 6:7], sg[:], ALU.mult, ALU.mult)
                        ptr = ptrs()
                        for e2 in range(2):
                            P.tr(ptr[:, e2 * 128:(e2 + 1) * 128], on_[:, e2 * 128:(e2 + 1) * 128], idb[:])
                        P.copy("act", oTs[:, :, qt * 128:(qt + 1) * 128], ptr[:, 0:256].rearrange("p (a t) -> p a t", a=2))
                    P.dma("sp", oT[h * 256:(h + 1) * 256, q0:q0 + qn].rearrange("(a p) t -> p a t", p=128), oTs[:, :, 0:qn])

    Lp = L + 4

    def ix(l):
        return l + 1 if l < S else l + 3

    dir_ranges = [[(0, OWN), (S, L)], [(0, S), (S, L)]]
    if on(4):
        with P.phase():
            idf, idb = load_ident(P)
            shab = P.sb("shab", [128, NRT, 2], F32)
            P.dma("sp", shab[:], IN("shAB", [128, NRT, 2])[:, :, :])
            c0s = P.sb("c0s", [128, NRT], F32)
            P.tt("dve", c0s[:].unsqueeze(2), shab[:, :, 0:1], shab[:, :, 1:2], ALU.add)
            P.ts("dve", c0s[:], c0s[:], -1.0, ALU.mult, 1.0, ALU.add)
            chp = P.sb("chp", [128, 16, NPAR], F32)
            P.dma("sp", chp[:], IN("chp", [128, 16, NPAR])[:, :, :])
            P.ts("dve", chp[:, :, POMKA:POMKA + 1], chp[:, :, PK_A:PK_A + 1], -1.0, ALU.mult, 1.0, ALU.add)
            bo = P.sb("bo", [128, 128], F32)
            P.dma("sp", bo[:], IN("bo", [128, 128])[:, :])
            w2b = [P.sb("w2b%d" % d, [128, RW_W], BF16) for d in range(2)]
            a2b = [P.sb("a2b%d" % d, [128, RW_W], BF16) for d in range(2)]
            w2d = IN("rw_w2d", [2, 128, RW_W])
            a2d = IN("rw_a2d", [2, 128, RW_W])
            for d in range(2):
                P.dma("pool", w2b[d][:], w2d[d])
                P.dma("pool", a2b[d][:], a2d[d])
            raws = [P.sb("raw%d" % i, [128, Lp], BF16) for i in range(3)]
            for r in raws:
                for pidx in (0, S + 1, S + 2, L + 3):
                    P.memset("pool", r[:, pidx:pidx + 1], 0.0)

            def load_raw(raw, r0, rn):
                P.dma("sp", raw[0:rn, 1:S + 1], rwT[r0:r0 + rn, 0:S])
                P.dma("sp", raw[0:rn, S + 3:L + 3], rwT[r0:r0 + rn, S:L])

            def shift(out, raw, rt, rn=128):
                P.ts("dve", out[0:rn, 1:L + 3], raw[0:rn, 1:L + 3], c0s[0:rn, rt:rt + 1], ALU.mult)
                P.stt(out[0:rn, 1:L + 3], raw[0:rn, 0:L + 2], shab[0:rn, rt, 0:1], out[0:rn, 1:L + 3], ALU.mult, ALU.add)
                P.stt(out[0:rn, 1:L + 3], raw[0:rn, 2:L + 4], shab[0:rn, rt, 1:2], out[0:rn, 1:L + 3], ALU.mult, ALU.add)

            ks = P.sb("ks", [128, Lp], F32)
            kk = P.sb("kk", [128, Lp], F32)
            rs = P.sb("rs", [128, Lp], BF16)
            vs = P.sb("vs", [128, Lp], BF16)
            tw = P.sb("tw", [128, Lp], BF16)
            xab = P.sb("xab", [128, Lp], BF16)
            load_raw(raws[0], 3 * RW_W, 128)
            shift(ks, raws[0], 48)
            P.act(tw[:, 1:L + 3], ks[:, 1:L + 3], AF.Tanh)
            load_raw(raws[1], 3 * RW_W + 128, 128)
            shift(xab, raws[1], 49)
            sgs = P.sb("sgs", [128, OWN], BF16)
            for gi in range(4):
                rn = 128 if gi < 3 else 96
                raw = raws[(2 + gi) % 3]
                load_raw(raw, 3 * RW_W + 256 + gi * 128, rn)
                shift(kk, raw, 50 + gi, rn)
                P.act(sgs[0:rn, :], kk[0:rn, 1:OWN + 1], AF.Sigmoid)
                P.dma("sp", sgT[gi * 128:gi * 128 + rn, :], sgs[0:rn, :])
            SEG = 512
            pss = Rot([P.ps("fp%d" % i, [128, 512]) for i in range(3)])
            ptb = Rot([P.ps("ftb%d" % i, [128, 1024], BF16) for i in range(3)])
            T = {n: P.sb("f_" + n, [128, SEG], F32) for n in ("sig", "lw", "cum", "cum2", "ta", "tb", "e1", "e2", "e3", "av", "kd")}
            OB = {n: P.sb("f_" + n, [128, SEG], BF16) for n in ("ah", "rh", "bt", "kt")}
            m01 = P.sb("m01", [128, SEG], F32)
            P.memset("dve", m01[:], 1.0)
            P.memset("dve", m01[:].rearrange("p (c i) -> p c i", i=128)[:, :, 0:1], 0.0)
            kdsum = P.sb("kdsum", [128, OWN], F32)
            sqb = P.sb("sqb", [128, 512], F32)
            kkr = P.sb("kkr", [128, 512], F32)
            sd = P.sb("sd", [128, 512], F32)
            wcs = P.sb("wcs", [128, 8], F32)
            stg = Rot([P.sb("stg%d" % i, [128, 8, 128], BF16) for i in range(3)])
            bon = P.sb("bon", [128, 512], F32)
            for ct in range(16):
                for i, (rt, dst) in enumerate(((ct, rs), (16 + ct, ks), (32 + ct, vs))):
                    load_raw(raws[i], rt * 128, 128)
                    shift(dst, raws[i], rt)
                for b0, bn in splits(1, L + 3, 512):
                    P.ts("dve", kkr[:, 0:bn], ks[:, b0:b0 + bn], chp[:, ct, PK_K:PK_K + 1], ALU.mult)
                    P.tt("pool", sqb[:, 0:bn], kkr[:, 0:bn], kkr[:, 0:bn], ALU.mult)
                    ps = pss()
                    P.mm(ps[:, 0:bn], bo[:], sqb[:, 0:bn])
                    P.act(sd[:, 0:bn], ps[:, 0:bn], AF.Sqrt)
                    P.ts("dve", sd[:, 0:bn], sd[:, 0:bn], 1e-12, ALU.max)
                    P.recip(sd[:, 0:bn], sd[:, 0:bn])
                    P.tt("dve", kk[:, b0:b0 + bn], kkr[:, 0:bn], sd[:, 0:bn], ALU.mult)
                for c0, cn in splits(0, NCH, 8):
                    pt = ptb()
                    for cc in range(cn):
                        i0 = ix((c0 + cc) * 128)
                        P.tr(pt[:, cc * 128:(cc + 1) * 128], vs[:, i0:i0 + 128], idb[:])
                    sg_ = stg()
                    P.copy("act", sg_[:, 0:cn, :], pt[:, 0:cn * 128].rearrange("p (c n) -> p c n", n=128))
                    P.dma("sp", Vtok[c0 * 128:(c0 + cn) * 128, ct * 128:(ct + 1) * 128].rearrange("(c p) n -> p c n", p=128), sg_[:, 0:cn, :])
                for di in range(2):
                    for (ra, rb) in dir_ranges[di]:
                        for l0, n in splits(ra, rb, SEG):
                            i0 = ix(l0)
                            ncn = n // 128
                            for s0, sn in splits(0, n, 512):
                                ps = pss()
                                P.mm(ps[:, 0:sn], w2b[di][:, ct * 128:(ct + 1) * 128], tw[:, i0 + s0:i0 + s0 + sn])
                                P.act(T["sig"][:, s0:s0 + sn], ps[:, 0:sn], AF.Sigmoid, bias=chp[:, ct, PW0 + di:PW0 + di + 1])
                                ps = pss()
                                P.mm(ps[:, 0:sn], a2b[di][:, ct * 128:(ct + 1) * 128], xab[:, i0 + s0:i0 + s0 + sn])
                                P.act(T["av"][:, s0:s0 + sn], ps[:, 0:sn], AF.Sigmoid, bias=chp[:, ct, PA0 + di:PA0 + di + 1])
                            P.ts("dve", T["lw"][:, 0:n], T["sig"][:, 0:n], -EXPM05, ALU.mult)
                            P.scan(T["cum"][:, 0:n], m01[:, 0:n], T["lw"][:, 0:n], 0.0, ALU.mult, ALU.add)
                            cum = T["cum"]
                            if di == 1:
                                P.tt("dve", T["ta"][:, 0:n], T["lw"][:, 0:n], T["cum"][:, 0:n], ALU.subtract)
                                tot = T["cum"][:, 0:n].rearrange("p (c i) -> p c i", i=128)[:, :, 127:128].to_broadcast([128, ncn, 128])
                                P.tt("dve", T["cum2"][:, 0:n].rearrange("p (c i) -> p c i", i=128),
                                     T["ta"][:, 0:n].rearrange("p (c i) -> p c i", i=128), tot, ALU.add)
                                cum = T["cum2"]
                            P.act(T["e1"][:, 0:n], cum[:, 0:n], AF.Exp)
                            P.act(T["e2"][:, 0:n], cum[:, 0:n], AF.Exp, scale=-1.0)
                            P.tt("pool", T["tb"][:, 0:n], cum[:, 0:n], T["lw"][:, 0:n], ALU.subtract)
                            P.act(T["e3"][:, 0:n], T["tb"][:, 0:n], AF.Exp)
                            P.ts("dve", T["ta"][:, 0:n], T["av"][:, 0:n], chp[:, ct, PK_A:PK_A + 1], ALU.mult, chp[:, ct, POMKA:POMKA + 1], ALU.add)
                            P.tt("dve", T["kd"][:, 0:n], T["ta"][:, 0:n], ks[:, i0:i0 + n], ALU.mult)
                            P.stt(OB["ah"][:, 0:n], kk[:, i0:i0 + n], -1.0, T["e3"][:, 0:n], ALU.mult, ALU.mult)
                            P.tt("pool", OB["rh"][:, 0:n], rs[:, i0:i0 + n], T["e1"][:, 0:n], ALU.mult)
                            P.tt("dve", T["ta"][:, 0:n], kk[:, i0:i0 + n], T["av"][:, 0:n], ALU.mult)
                            P.tt("dve", OB["bt"][:, 0:n], T["ta"][:, 0:n], T["e2"][:, 0:n], ALU.mult)
                            P.tt("pool", OB["kt"][:, 0:n], T["kd"][:, 0:n], T["e2"][:, 0:n], ALU.mult)
                            e1v = T["e1"][:, 0:n].rearrange("p (c i) -> p c i", i=128)
                            pos = 127 if di == 0 else 0
                            P.copy("act", wcs[:, 0:ncn], e1v[:, :, pos])
                            rows = slice(ct * 128, (ct + 1) * 128)
                            P.dma("sp", Wc[di][rows, l0 // 128:l0 // 128 + ncn], wcs[:, 0:ncn], slow=True)
                            P.dma("sp", AhT[di][rows, l0:l0 + n], OB["ah"][:, 0:n])
                            P.dma("sp", RhT[di][rows, l0:l0 + n], OB["rh"][:, 0:n])
                            P.dma("sp", BtT[di][rows, l0:l0 + n], OB["bt"][:, 0:n])
                            P.dma("sp", KtT[di][rows, l0:l0 + n], OB["kt"][:, 0:n])
                            for nm, dstT in (("bt", Btok[di]), ("kt", Ktok[di])):
                                pt = ptb()
                                for cc in range(ncn):
                                    P.tr(pt[:, cc * 128:(cc + 1) * 128], OB[nm][:, cc * 128:(cc + 1) * 128], idb[:])
                                sg_ = stg()
                                P.copy("act", sg_[:, 0:ncn, :], pt[:, 0:ncn * 128].rearrange("p (c n) -> p c n", n=128))
                                P.dma("sp", dstT[l0:l0 + n, ct * 128:(ct + 1) * 128].rearrange("(c p) n -> p c n", p=128), sg_[:, 0:ncn, :])
                            if l0 < OWN:
                                no = min(n, OWN - l0)
                                if di == 0:
                                    P.copy("pool", kdsum[:, l0:l0 + no], T["kd"][:, 0:no])
                                else:
                                    P.tt("pool", kdsum[:, l0:l0 + no], kdsum[:, l0:l0 + no], T["kd"][:, 0:no], ALU.add)
                for b0, bn in splits(0, OWN, 512):
                    P.tt("dve", sqb[:, 0:bn], rs[:, b0 + 1:b0 + 1 + bn], kdsum[:, b0:b0 + bn], ALU.mult)
                    P.ts("dve", sqb[:, 0:bn], sqb[:, 0:bn], chp[:, ct, PR_K:PR_K + 1], ALU.mult)
                    ps = pss()
                    P.mm(ps[:, 0:bn], bo[:], sqb[:, 0:bn])
                    P.tt("dve", bon[:, 0:bn], ps[:, 0:bn], vs[:, b0 + 1:b0 + 1 + bn], ALU.mult)
                    P.dma("sp", bonusT[ct * 128:(ct + 1) * 128, b0:b0 + bn], bon[:, 0:bn])

    own_ch = list(range(OWN // 128))
    oth_ch = list(range(OWN // 128, S // 128))
    ctx_ch = list(range(S // 128, NCH))
    dir_chunks = [ctx_ch + own_ch, ctx_ch[::-1] + oth_ch[::-1] + own_ch[::-1]]

    if on(5):
        with P.phase():
            idf, idb = load_ident(P)
            mks = [P.sb("mk%d" % d, [128, 384], F32) for d in range(2)]
            msk_d = IN("masks", [2, 128, 384])
            for d in range(2):
                P.dma("sp", mks[d][:], msk_d[d])
            ins = []
            for i in range(2):
                ins.append(dict(
                    ARh=P.sb("cARh%d" % i, [128, 16, 2, 128], BF16), Bt=P.sb("cBt%d" % i, [128, 16, 128], BF16),
                    Kt=P.sb("cKt%d" % i, [128, 16, 128], BF16), Vt=P.sb("cVt%d" % i, [128, RW_W], BF16),
                    Gst=P.sb("cG%d" % i, [128, 32, 128], BF16), Nst=P.sb("cN%d" % i, [128, 32, 128], BF16),
                    Zst=P.sb("cZ%d" % i, [128, 32, 64], F32), Yst=P.sb("cY%d" % i, [128, 16, 128], F32)))
            NW = 4
            bk = [P.ps("cbk%d" % i, [128, 512]) for i in range(8)]
            Xs = P.sb("cXs", [128, 32, 128], F32)
            Ls = P.sb("cLs", [128, 32, 128], F32)
            MNs = Rot([P.sb("cMN%d" % i, [128, 256], BF16) for i in range(4)])
            XGt = [[P.sb("cXG%d_%d" % (i, pp), [128, 256], F32) for pp in range(2)] for i in range(NW)]
            Lct = [[P.sb("cLc%d_%d" % (i, pp), [128, 128], F32) for pp in range(2)] for i in range(NW)]
            step = 0
            for di in range(2):
                mk = mks[di]
                for c in dir_chunks[di]:
                    I = ins[step % 2]
                    step += 1
                    own = c * 128 < OWN
                    cols = slice(c * 128, (c + 1) * 128)
                    P.dma("sp", I["ARh"][:, :, 0, :], AhT[di].rearrange("(ct p) l -> p ct l", p=128)[:, :, cols])
                    P.dma("sp", I["ARh"][:, :, 1, :], RhT[di].rearrange("(ct p) l -> p ct l", p=128)[:, :, cols])
                    P.dma("sp", I["Bt"][:], BtT[di].rearrange("(ct p) l -> p ct l", p=128)[:, :, cols])
                    P.dma("sp", I["Kt"][:], KtT[di].rearrange("(ct p) l -> p ct l", p=128)[:, :, cols])
                    P.dma("sp", I["Vt"][:], Vtok[c * 128:(c + 1) * 128, :])
                    for h in range(32):
                        ct, half = h // 2, h % 2
                        hp = slice(half * 64, half * 64 + 64)
                        AB, CZ = bk[h % 4], bk[4 + h % 4]
                        P.mm(AB[:, 0:256], I["Bt"][hp, ct, :], I["ARh"][hp, ct, :, :])
                        P.mm(AB[:, 256:512], I["Kt"][hp, ct, :], I["ARh"][hp, ct, :, :])
                        P.mm(CZ[:, 0:128], I["ARh"][hp, ct, 0, :], I["Bt"][hp, ct, :])
                        MN = MNs()
                        P.tt("dve", Xs[:, h, :], AB[:, 0:128], mk[:, 0:128], ALU.mult)
                        if own:
                            P.tt("dve", I["Nst"][:, h, :], AB[:, 128:256], mk[:, 128:256], ALU.mult)
                        P.tt("dve", MN[:], AB[:, 256:512], mk[:, 0:256], ALU.mult)
                        P.tt("dve", Ls[:, h, :], CZ[:, 0:128], mk[:, 256:384], ALU.mult)
                        hc = slice(h * 64, (h + 1) * 64)
                        P.mm(CZ[:, 128:192], MN[:, 0:128], I["Vt"][:, hc])
                        P.copy("act", I["Zst"][:, h, :], CZ[:, 128:192])
                        if own:
                            P.mm(CZ[hp, 256:384], I["Vt"][:, hc], MN[:, 128:256])
                            P.copy("act", I["Yst"][hp, ct, :], CZ[hp, 256:384])
                    for g0 in range(0, 32, NW):
                        hs = list(range(g0, g0 + NW))
                        for i, h in enumerate(hs):
                            P.mm(bk[i][:, 0:128], Ls[:, h, :], Xs[:, h, :])
                            P.mm(bk[NW + i][:, 0:128], Xs[:, h, :], Ls[:, h, :])
                        for i, h in enumerate(hs):
                            XG, Lc = XGt[i][0], Lct[i][0]
                            P.tt("dve", XG[:, 128:256], Xs[:, h, :], idf[:], ALU.add)
                            P.copy("act", XG[:, 0:128], bk[i][:, 0:128])
                            P.copy("act", Lc[:], bk[NW + i][:, 0:128])
                        for k in range(1, 7):
                            pc, pn = (k - 1) % 2, k % 2
                            if k < 6:
                                for i, h in enumerate(hs):
                                    XG, Lc = XGt[i][pc], Lct[i][pc]
                                    P.mm(bk[i][:, 0:256], Lc[:], XG[:, 0:256])
                                    P.mm(bk[NW + i][:, 0:128], XG[:, 0:128], Lc[:])
                                for i, h in enumerate(hs):
                                    XG, XG2, Lc2 = XGt[i][pc], XGt[i][pn], Lct[i][pn]
                                    P.copy("act", XG2[:, 0:128], bk[i][:, 0:128])
                                    P.tt("dve", XG2[:, 128:256], bk[i][:, 128:256], XG[:, 128:256], ALU.add)
                                    P.copy("act" if i % 2 else "dve", Lc2[:], bk[NW + i][:, 0:128])
                            else:
                                for i, h in enumerate(hs):
                                    P.mm(bk[i][:, 128:256], Lct[i][pc][:], XGt[i][pc][:, 128:256])
                                for i, h in enumerate(hs):
                                    P.tt("dve", I["Gst"][:, h, :], bk[i][:, 128:256], XGt[i][pc][:, 128:256], ALU.add)
                    P.dma("sp", Gs[di][c], I["Gst"][:].rearrange("p h i -> p (h i)"))
                    P.dma("sp", Z0s[di][c], I["Zst"][:].rearrange("p h i -> p (h i)"))
                    if own:
                        P.dma("sp", Ns[di][c], I["Nst"][:].rearrange("p h i -> p (h i)"))
                        P.dma("sp", Y0s[di][c], I["Yst"][:].rearrange("p h i -> p (h i)"))

    if on(6):
        with P.phase():
            ST = P.sb("ST", [128, 16, 64], F32)
            STz = P.sb("STz", [128, 16, 2, 64], BF16)
            wcs = P.sb("swc", [128, 16, NCH], F32)
            ins = []
            for i in range(2):
                ins.append(dict(
                    ARh=P.sb("sARh%d" % i, [128, 16, 2, 128], BF16), G=P.sb("sG%d" % i, [128, 32, 128], BF16),
                    N=P.sb("sN%d" % i, [128, 32, 128], BF16), Z0=P.sb("sZ%d" % i, [128, 32, 64], F32),
                    Y0=P.sb("sY%d" % i, [128, 16, 128], F32), Bt=P.sb("sBt%d" % i, [128, RW_W], BF16),
                    Kt=P.sb("sKt%d" % i, [128, RW_W], BF16), Vt=P.sb("sVt%d" % i, [128, RW_W], BF16)))
            Zb = P.sb("sZb", [128, 32, 64], BF16)
            Ub = P.sb("sUb", [128, 32, 64], BF16)
            yts = Rot([P.sb("syt%d" % i, [128, 16, 128], F32) for i in range(2)])
            pz = Rot([P.ps("spz%d" % i, [128, 512]) for i in range(2)])
            pu = Rot([P.ps("spu%d" % i, [128, 512]) for i in range(2)])
            py = Rot([P.ps("spy%d" % i, [128, 512]) for i in range(2)])
            psb = Rot([P.ps("sps%d" % i, [128, 512]) for i in range(2)])
            step = 0
            for di in range(2):
                P.memset("dve", ST[:], 0.0)
                P.memset("pool", STz[:], 0.0)
                P.dma("sp", wcs[:], Wc[di].rearrange("(ct p) c -> p ct c", p=128))
                for c in dir_chunks[di]:
                    I = ins[step % 2]
                    step += 1
                    own = c * 128 < OWN
                    cols = slice(c * 128, (c + 1) * 128)
                    P.dma("sp", I["ARh"][:, :, 0, :], AhT[di].rearrange("(ct p) l -> p ct l", p=128)[:, :, cols])
                    P.dma("sp", I["G"][:].rearrange("p h i -> p (h i)"), Gs[di][c])
                    P.dma("sp", I["Z0"][:].rearrange("p h i -> p (h i)"), Z0s[di][c])
                    P.dma("sp", I["Bt"][:], Btok[di][c * 128:(c + 1) * 128, :])
                    P.dma("sp", I["Kt"][:], Ktok[di][c * 128:(c + 1) * 128, :])
                    P.dma("sp", I["Vt"][:], Vtok[c * 128:(c + 1) * 128, :])
                    if own:
                        P.dma("sp", I["ARh"][:, :, 1, :], RhT[di].rearrange("(ct p) l -> p ct l", p=128)[:, :, cols])
                        P.dma("sp", I["N"][:].rearrange("p h i -> p (h i)"), Ns[di][c])
                        P.dma("sp", I["Y0"][:].rearrange("p h i -> p (h i)"), Y0s[di][c])
                    for g in range(4):
                        p_ = pz()
                        for hh in range(8):
                            h = g * 8 + hh
                            ct, hp = h // 2, slice((h % 2) * 64, (h % 2) * 64 + 64)
                            P.mm(p_[:, hh * 64:(hh + 1) * 64], I["ARh"][:, ct, 0, :], STz[:, ct, h % 2, :])
                        P.tt("dve", Zb[:, g * 8:(g + 1) * 8, :], p_[:].rearrange("p (h v) -> p h v", v=64), I["Z0"][:, g * 8:(g + 1) * 8, :], ALU.add)
                    for g in range(4):
                        p_ = pu()
                        for hh in range(8):
                            h = g * 8 + hh
                            P.mm(p_[:, hh * 64:(hh + 1) * 64], I["G"][:, h, :], Zb[:, h, :])
                        P.copy("act", Ub[:, g * 8:(g + 1) * 8, :], p_[:].rearrange("p (h v) -> p h v", v=64))
                    if own:
                        yt = yts()
                        for g in range(4):
                            p_ = py()
                            for c4 in range(4):
                                ct = g * 4 + c4
                                for half in range(2):
                                    h = ct * 2 + half
                                    hp = slice(half * 64, half * 64 + 64)
                                    P.mm(p_[hp, c4 * 128:(c4 + 1) * 128], STz[:, ct, half, :], I["ARh"][:, ct, 1, :], start=True, stop=False)
                                    P.mm(p_[hp, c4 * 128:(c4 + 1) * 128], Ub[:, h, :], I["N"][:, h, :], start=False, stop=True)
                            P.tt("dve", yt[:, g * 4:(g + 1) * 4, :], p_[:].rearrange("p (c i) -> p c i", i=128), I["Y0"][:, g * 4:(g + 1) * 4, :], ALU.add)
                        P.dma("sp", ysc[di].rearrange("(ct p) l -> p ct l", p=128)[:, :, cols], yt[:])
                    for g in range(2):
                        p_ = psb()
                        for c8 in range(8):
                            ct = g * 8 + c8
                            for half in range(2):
                                h = ct * 2 + half
                                hp = slice(half * 64, half * 64 + 64)
                                hc = slice(h * 64, (h + 1) * 64)
                                P.mm(p_[hp, c8 * 64:(c8 + 1) * 64], I["Bt"][:, hc], Ub[:, h, :], start=True, stop=False)
                                P.mm(p_[hp, c8 * 64:(c8 + 1) * 64], I["Kt"][:, hc], I["Vt"][:, hc], start=False, stop=True)
                        P.tt("dve", ST[:, g * 8:(g + 1) * 8, :], p_[:].rearrange("p (c v) -> p c v", v=64), ST[:, g * 8:(g + 1) * 8, :], ALU.add)
                    P.tt("dve", ST[:], ST[:], wcs[:, :, c:c + 1].to_broadcast([128, 16, 64]), ALU.mult)
                    P.copy("act", STz[0:64, :, 0, :], ST[0:64, :, :])
                    P.copy("act", STz[64:128, :, 1, :], ST[64:128, :, :])

    if on(7):
        with P.phase():
            chp = P.sb("chp", [128, 16, NPAR], F32)
            P.dma("sp", chp[:], IN("chp", [128, 16, NPAR])[:, :, :])
            bo = P.sb("bo", [128, 128], F32)
            P.dma("sp", bo[:], IN("bo", [128, 128])[:, :])
            bo64 = P.sb("bo64", [128, 128], F32)
            P.ts("dve", bo64[:], bo[:], 1.0 / 64, ALU.mult)
            g2b = P.sb("g2b", [128, 4, RW_W], BF16)
            g2d = IN("rw_g2", [480, RW_W])
            for gi in range(4):
                rn = 128 if gi < 3 else 96
                P.dma("pool", g2b[0:rn, gi, :], g2d[gi * 128:gi * 128 + rn, :])
            sgb = P.sb("sgb", [128, 4, OWN], BF16)
            P.dma("sp", sgb[:], sgT.rearrange("(g p) t -> p g t", p=128))
            pss = Rot([P.ps("op%d" % i, [128, 512]) for i in range(4)])
            ya = Rot([P.sb("ya%d" % i, [128, 512], F32) for i in range(2)])
            yb = Rot([P.sb("yb%d" % i, [128, 512], F32) for i in range(2)])
            bn_ = Rot([P.sb("obn%d" % i, [128, 512], F32) for i in range(2)])
            yc = P.sb("yc", [128, 512], F32)
            sq = P.sb("osq", [128, 512], F32)
            rsd = P.sb("rsd", [128, 512], F32)
            ob = Rot([P.sb("oob%d" % i, [128, 512], BF16) for i in range(2)])
            for ct in range(16):
                rows = slice(ct * 128, (ct + 1) * 128)
                for b0, bn in splits(0, OWN, 512):
                    y0, y1, bt = ya(), yb(), bn_()
                    P.dma("sp", y0[:, 0:bn], ysc[0][rows, b0:b0 + bn])
                    P.dma("sp", y1[:, 0:bn], ysc[1][rows, b0:b0 + bn])
                    P.dma("sp", bt[:, 0:bn], bonusT[rows, b0:b0 + bn])
                    P.tt("dve", y0[:, 0:bn], y0[:, 0:bn], y1[:, 0:bn], ALU.add)
                    pm_ = pss()
                    P.mm(pm_[:, 0:bn], bo64[:], y0[:, 0:bn])
                    P.tt("dve", yc[:, 0:bn], y0[:, 0:bn], pm_[:, 0:bn], ALU.subtract)
                    P.tt("pool", sq[:, 0:bn], yc[:, 0:bn], yc[:, 0:bn], ALU.mult)
                    pv = pss()
                    P.mm(pv[:, 0:bn], bo64[:], sq[:, 0:bn])
                    P.ts("dve", rsd[:, 0:bn], pv[:, 0:bn], GN_EPS, ALU.add)
                    P.act(rsd[:, 0:bn], rsd[:, 0:bn], AF.Sqrt)
                    P.recip(rsd[:, 0:bn], rsd[:, 0:bn])
                    P.tt("dve", yc[:, 0:bn], yc[:, 0:bn], rsd[:, 0:bn], ALU.mult)
                    P.ts("dve", yc[:, 0:bn], yc[:, 0:bn], chp[:, ct, PLNW:PLNW + 1], ALU.mult, chp[:, ct, PLNB:PLNB + 1], ALU.add)
                    P.tt("dve", yc[:, 0:bn], yc[:, 0:bn], bt[:, 0:bn], ALU.add)
                    pg = pss()
                    for gi in range(4):
                        rn = 128 if gi < 3 else 96
                        P.mm(pg[:, 0:bn], g2b[0:rn, gi, rows], sgb[0:rn, gi, b0:b0 + bn], start=(gi == 0), stop=(gi == 3))
                    o_ = ob()
                    P.tt("dve", o_[:, 0:bn], yc[:, 0:bn], pg[:, 0:bn], ALU.mult)
                    P.dma("sp", oT[DA_W + ct * 128:DA_W + (ct + 1) * 128, b0:b0 + bn], o_[:, 0:bn])

    if on(8):
        with P.phase():
            w_out = IN("w_out", [D, D])
            wv = w_out.rearrange("(k p) n -> p k n", p=128)
            oTv = oT.rearrange("(k p) t -> p k t", p=128)
            oTb = P.sb("oTb", [128, KC, 1024], BF16)
            wbs = Rot([P.sb("wo%d" % i, [128, KC, 512], BF16) for i in range(2)])
            pss = Rot([P.ps("wp%d" % i, [128, 512]) for i in range(4)])
            obs = Rot([P.sb("wob%d" % i, [128, 512], F32) for i in range(3)])
            for t0, tn in splits(0, OWN, 1024):
                P.dma("sp", oTb[:, :, 0:tn], oTv[:, :, t0:t0 + tn])
                for c0, cn in splits(0, D, 512):
                    wb = wbs()
                    P.dma("pool", wb[:], wv[:, :, c0:c0 + cn])
                    for tt0, _ in splits(0, tn, 128):
                        ps = pss()
                        for kc in range(KC):
                            P.mm(ps[:], oTb[:, kc, tt0:tt0 + 128], wb[:, kc, :], start=(kc == 0), stop=(kc == KC - 1))
                        o_ = obs()
                        P.copy("act", o_[:], ps[:])
                        P.dma("sp", olat[t0 + tt0:t0 + tt0 + 128, c0:c0 + cn], o_[:])

    if on(9):
        with P.phase():
            x_l = IN("x_l", [L, D])
            idf, idb = load_ident(P)
            G2 = P.sb("G2", [128, D], F32)
            A2 = P.sb("A2", [128, D], F32)
            Sh2 = P.sb("Sh2", [128, D], F32)
            tmp = P.sb("tmp", [128, D], F32)
            P.dma("sp", G2[:], modscr[0:1, 2 * D:3 * D].partition_broadcast(128))
            P.dma("sp", tmp[:], IN("g_post_attn", [1, D])[0:1, :].partition_broadcast(128))
            P.tt("dve", G2[:], G2[:], tmp[:], ALU.mult)
            P.dma("sp", Sh2[:], modscr[0:1, 3 * D:4 * D].partition_broadcast(128))
            P.dma("sp", A2[:], modscr[0:1, 4 * D:5 * D].partition_broadcast(128))
            P.dma("sp", tmp[:], IN("g_pre_ffn", [1, D])[0:1, :].partition_broadcast(128))
            P.stt(A2[:], A2[:], 1.0, tmp[:], ALU.add, ALU.mult)
            rwf = P.sb("rwf", [128, KC, NE], F32)
            P.dma("sp", rwf[:], IN("router_w", [D, NE]).rearrange("(k p) e -> p k e", p=128))
            rbias = P.sb("rbias", [128, NE], F32)
            P.dma("sp", rbias[:], IN("router_bias", [1, NE])[0:1, :].partition_broadcast(128))
            xt = P.sb("xt", [128, D], F32)
            ol = P.sb("ol", [128, D], F32)
            x1 = P.sb("x1", [128, D], F32)
            hf = ol
            hb = P.sb("hb", [128, D], BF16)
            junk = P.sb("junk", [128, D], BF16)
            hTf = P.sb("hTf", [128, KC, 128], F32)
            hTb = P.sb("hTb", [128, KC, 128], BF16)
            st = P.sb("st", [128, 8], F32)
            ptb = Rot([P.ps("rtb%d" % i, [128, 1024], BF16) for i in range(2)])
            ptf = Rot([P.ps("rtf%d" % i, [128, 512]) for i in range(3)])
            prr = P.ps("prr", [128, 512])
            R = {n: P.sb("r_" + n, [128, NE], F32) for n in ("sc", "bi", "eq", "mk", "mb", "sel", "ga")}
            r8 = {n: P.sb("r8_" + n, [128, 8], F32) for n in ("m1", "m2", "gs", "srt", "gm", "pen", "srt2", "den")}
            gT = P.sb("gT", [128, 128], F32)
            for ti in range(OWN // 128):
                rowsl = slice(ti * 128, (ti + 1) * 128)
                P.dma("sp", xt[:], x_l[rowsl, :])
                P.dma("sp", ol[:], olat[rowsl, :])
                rms_stats(P, st, ol[:], junk[:], D)
                P.stt(tmp[:], ol[:], st[:, 3:4], G2[:], ALU.mult, ALU.mult)
                P.tt("pool", x1[:], tmp[:], xt[:], ALU.add)
                P.dma("sp", x1s[rowsl, :], x1[:])
                rms_stats(P, st[:, 4:8], x1[:], junk[:], D)
                P.stt(tmp[:], x1[:], st[:, 7:8], A2[:], ALU.mult, ALU.mult)
                P.tt("dve", hf[:], tmp[:], Sh2[:], ALU.add)
                P.copy("pool", hb[:], hf[:])
                for q4 in range(4):
                    pt = ptb()
                    for k8 in range(8):
                        kc = q4 * 8 + k8
                        P.tr(pt[:, k8 * 128:(k8 + 1) * 128], hb[:, kc * 128:(kc + 1) * 128], idb[:])
                    P.copy("act", hTb[:, q4 * 8:(q4 + 1) * 8, :], pt[:].rearrange("p (k t) -> p k t", k=8))
                P.dma("sp", hTs.rearrange("(k p) t -> p k t", p=128)[:, :, rowsl], hTb[:])
                for q8 in range(8):
                    pt = ptf()
                    for k4 in range(4):
                        kc = q8 * 4 + k4
                        P.tr(pt[:, k4 * 128:(k4 + 1) * 128], hf[:, kc * 128:(kc + 1) * 128], idf[:])
                    P.copy("act" if q8 % 2 else "dve", hTf[:, q8 * 4:(q8 + 1) * 4, :], pt[:].rearrange("p (k t) -> p k t", k=4))
                for kc in range(KC):
                    P.mm(prr[:, 0:NE], hTf[:, kc, :], rwf[:, kc, :], start=(kc == 0), stop=(kc == KC - 1))
                P.act(R["sc"][:], prr[:, 0:NE], AF.Sigmoid)
                P.tt("dve", R["bi"][:], R["sc"][:], rbias[:], ALU.add)
                bv = R["bi"][:].rearrange("p (g s) -> p g s", s=GS)
                P.red(r8["m1"][:], bv, ALU.max)
                P.tt("dve", R["eq"][:].rearrange("p (g s) -> p g s", s=GS), bv, r8["m1"][:].unsqueeze(2).to_broadcast([128, NG, GS]), ALU.is_equal)
                P.stt(R["mk"][:], R["eq"][:], -1e9, R["bi"][:], ALU.mult, ALU.add)
                P.red(r8["m2"][:], R["mk"][:].rearrange("p (g s) -> p g s", s=GS), ALU.max)
                P.tt("dve", r8["gs"][:], r8["m1"][:], r8["m2"][:], ALU.add)
                P.max8(r8["srt"][:], r8["gs"][:])
                P.ts("dve", r8["gm"][:], r8["gs"][:], r8["srt"][:, 3:4], ALU.is_ge)
                P.ts("dve", r8["pen"][:], r8["gm"][:], -1.0, ALU.add, 1e9, ALU.mult)
                P.tt("dve", R["mb"][:].rearrange("p (g s) -> p g s", s=GS), bv, r8["pen"][:].unsqueeze(2).to_broadcast([128, NG, GS]), ALU.add)
                P.max8(r8["srt2"][:], R["mb"][:])
                P.ts("dve", R["sel"][:], R["mb"][:], r8["srt2"][:, 5:6], ALU.is_ge)
                P.tt("dve", R["ga"][:], R["sc"][:], R["sel"][:], ALU.mult)
                P.red(r8["den"][:, 0:1], R["ga"][:], ALU.add)
                P.recip(r8["den"][:, 1:2], r8["den"][:, 0:1])
                P.ts("dve", R["ga"][:], R["ga"][:], r8["den"][:, 1:2], ALU.mult, 2.5, ALU.mult)
                pt = ptf()
                P.tr(pt[0:NE, 0:128], R["ga"][:], idf[:])
                P.copy("act", gT[0:NE, :], pt[0:NE, 0:128])
                P.dma("sp", gscT[:, rowsl], gT[0:NE, :])

    if on(10):
        with P.phase():
            w1a = IN("w1all", [NE1, D, FF])
            w3a = IN("w3all", [NE1, D, FF])
            hTv = hTs.rearrange("(k p) t -> p k t", p=128)
            hTb = P.sb("ehT", [128, KC, 1024], BF16)
            w1s = Rot([P.sb("ew1%d" % i, [128, KC, 256], BF16) for i in range(2)])
            w3s = Rot([P.sb("ew3%d" % i, [128, KC, 256], BF16) for i in range(2)])
            gbs = Rot([P.sb("egb%d" % i, [128, 1024], F32) for i in range(2)])
            p1s = Rot([P.ps("ep1%d" % i, [128, 512]) for i in range(3)])
            p3s = Rot([P.ps("ep3%d" % i, [128, 512]) for i in range(3)])
            sls = Rot([P.sb("esl%d" % i, [128, 512], F32) for i in range(2)])
            hms = Rot([P.sb("ehm%d" % i, [128, 512], F32) for i in range(2)])
            hos = Rot([P.sb("eho%d" % i, [128, 512], BF16) for i in range(3)])
            for t0, tn in splits(0, OWN, 1024):
                P.dma("sp", hTb[:, :, 0:tn], hTv[:, :, t0:t0 + tn])
                for e in range(NE1):
                    gb = None
                    if e < NE:
                        gb = gbs()
                        P.dma("sp", gb[:, 0:tn], gscT[e:e + 1, t0:t0 + tn].partition_broadcast(128))
                    for f0 in (0, 256):
                        w1, w3 = w1s(), w3s()
                        P.dma("pool", w1[:], w1a[e].rearrange("(k p) f -> p k f", p=128)[:, :, f0:f0 + 256])
                        P.dma("pool", w3[:], w3a[e].rearrange("(k p) f -> p k f", p=128)[:, :, f0:f0 + 256])
                        for fh in range(2):
                            fs = slice(fh * 128, (fh + 1) * 128)
                            for s0, sn in splits(0, tn, 512):
                                p1, p3 = p1s(), p3s()
                                for kc in range(KC):
                                    P.mm(p1[:, 0:sn], w1[:, kc, fs], hTb[:, kc, s0:s0 + sn], start=(kc == 0), stop=(kc == KC - 1))
                                for kc in range(KC):
                                    P.mm(p3[:, 0:sn], w3[:, kc, fs], hTb[:, kc, s0:s0 + sn], start=(kc == 0), stop=(kc == KC - 1))
                                sl, ho = sls(), hos()
                                P.act(sl[:, 0:sn], p1[:, 0:sn], AF.Silu)
                                if gb is None:
                                    P.tt("dve", ho[:, 0:sn], sl[:, 0:sn], p3[:, 0:sn], ALU.mult)
                                else:
                                    hm = hms()
                                    P.tt("dve", hm[:, 0:sn], sl[:, 0:sn], p3[:, 0:sn], ALU.mult)
                                    P.tt("dve", ho[:, 0:sn], hm[:, 0:sn], gb[:, s0:s0 + sn], ALU.mult)
                                r0 = e * FF + f0 + fh * 128
                                P.dma("sp", hbs[r0:r0 + 128, t0 + s0:t0 + s0 + sn], ho[:, 0:sn])

    if on(11):
        with P.phase():
            w2a = IN("w2all", [NE1 * FF, D])
            NCK = NE1 * FF // 128
            GK = 20
            hbv = hbs.rearrange("(c p) t -> p c t", p=128)
            w2v = w2a.rearrange("(c p) n -> p c n", p=128)
            hbg = Rot([P.sb("dhb%d" % i, [128, GK, 1024], BF16) for i in range(2)])
            wbs = Rot([P.sb("dw%d" % i, [128, GK, 512], BF16) for i in range(2)])
            yacc = P.sb("yacc", [128, 8, 512], F32)
            pss = Rot([P.ps("dp%d" % i, [128, 512]) for i in range(4)])
            for t0, tn in splits(0, OWN, 1024):
                ntt = tn // 128
                for c0, cn in splits(0, D, 512):
                    for gi, (k0, kn) in enumerate(splits(0, NCK, GK)):
                        hg, wb = hbg(), wbs()
                        P.dma("sp", hg[:, 0:kn, 0:tn], hbv[:, k0:k0 + kn, t0:t0 + tn])
                        P.dma("pool", wb[:, 0:kn, :], w2v[:, k0:k0 + kn, c0:c0 + cn])
                        for tt in range(ntt):
                            ps = pss()
                            for k in range(kn):
                                P.mm(ps[:], hg[:, k, tt * 128:(tt + 1) * 128], wb[:, k, :], start=(k == 0), stop=(k == kn - 1))
                            if gi == 0:
                                P.copy("act", yacc[:, tt, :], ps[:])
                            else:
                                P.tt("dve", yacc[:, tt, :], ps[:], yacc[:, tt, :], ALU.add)
                    P.dma("sp", ymoe[t0:t0 + tn, c0:c0 + cn].rearrange("(t p) n -> p t n", p=128), yacc[:, 0:ntt, :])

    if on(12):
        with P.phase():
            G5 = P.sb("G5", [128, D], F32)
            tmp = P.sb("tmp", [128, D], F32)
            P.dma("sp", G5[:], modscr[0:1, 5 * D:6 * D].partition_broadcast(128))
            P.dma("sp", tmp[:], IN("g_post_ffn", [1, D])[0:1, :].partition_broadcast(128))
            P.tt("dve", G5[:], G5[:], tmp[:], ALU.mult)
            yms = Rot([P.sb("ym%d" % i, [128, D], F32) for i in range(2)])
            x1b = Rot([P.sb("x1b%d" % i, [128, D], F32) for i in range(2)])
            ots = Rot([P.sb("ot%d" % i, [128, D], F32) for i in range(2)])
            junk = P.sb("junk", [128, D], BF16)
            sts = Rot([P.sb("fst%d" % i, [128, 4], F32) for i in range(2)])
            for ti in range(OWN // 128):
                rowsl = slice(ti * 128, (ti + 1) * 128)
                ym, x1, ot, st = yms(), x1b(), ots(), sts()
                P.dma("sp", ym[:], ymoe[rowsl, :])
                P.dma("sp", x1[:], x1s[rowsl, :])
                rms_stats(P, st, ym[:], junk[:], D)
                P.stt(tmp[:], ym[:], st[:, 3:4], G5[:], ALU.mult, ALU.mult)
                P.tt("pool", ot[:], tmp[:], x1[:], ALU.add)
                P.dma("sp", out_d[rowsl, :], ot[:])
    elif cfg.get("dummy_out", True):
        with P.phase():
            z = P.sb("z", [128, 64], F32)
            P.memset("dve", z[:], 0.0)
            P.dma("sp", out_d[0:128, 0:64], z[:])
    P.close()
    P.in_names = in_names
    return nc, P


def qk_perm():
    idx = np.arange(IN_COLS)
    blk = np.concatenate([np.arange(0, 128, 2), np.arange(1, 128, 2)])
    for base in range(0, 2 * DA_W, 128):
        idx[base:base + 128] = base + blk
    return idx


def rope_tables(cfg, j):
    S, CTX, GW = cfg["S"], cfg["CTX"], cfg["GW"]
    L = S + CTX
    l = np.arange(S)
    t = l if j == 0 else (S - 1 - l)
    row = (t // GW).astype(np.float32)
    col = (t % GW).astype(np.float32)
    inv = np.power(np.float32(10000.0), -np.arange(32, dtype=np.float32) / np.float32(32)).astype(np.float32)
    ang = np.concatenate([row[:, None] * inv, col[:, None] * inv], axis=-1).astype(np.float32)
    cos, sin = np.cos(ang).astype(np.float32), np.sin(ang).astype(np.float32)
    cosT = np.ones((128, L), np.float32)
    sinT = np.zeros((128, L), np.float32)
    cosT[0:64, :S] = cos.T
    cosT[64:128, :S] = cos.T
    sinT[0:64, :S] = -sin.T
    sinT[64:128, :S] = sin.T
    return cosT, sinT


def chunk_masks():
    i = np.arange(128)
    m = np.zeros((2, 128, 384), np.float32)
    r, c = i[:, None], i[None, :]
    m[0, :, 0:128] = (c > r)
    m[0, :, 128:256] = (c >= r)
    m[0, :, 256:384] = (r > c)
    m[1, :, 0:128] = (c < r)
    m[1, :, 128:256] = (c <= r)
    m[1, :, 256:384] = (r < c)
    return m


def host_inputs(inputs, cfg, names=None):
    S, CTX = cfg["S"], cfg["CTX"]
    g = lambda k: np.asarray(inputs[k]) if k in inputs else None
    want = (lambda n: True) if names is None else (lambda n: n in names)
    shared = {}
    if want("w_in"):
        shared["w_in"] = np.ascontiguousarray(g("w_in")[0][:, qk_perm()])
    for k in ("w_mod", "w_out", "rw_g2", "router_w"):
        if want(k):
            shared[k] = np.ascontiguousarray(g(k)[0])
    for k in ("g_pre_attn", "g_post_attn", "g_pre_ffn", "g_post_ffn", "da_subln", "router_bias"):
        if want(k):
            shared[k] = np.ascontiguousarray(g(k).reshape(1, -1))
    if want("b_mod"):
        shared["b_mod"] = np.ascontiguousarray(g("b_mod")[0][None, :])
    if want("da_lambda"):
        shared["da_lambda"] = np.ascontiguousarray(g("da_lambda")[0].reshape(1, 512))
    if want("w1all"):
        shared["w1all"] = np.concatenate([g("exp_w1")[0], g("sh_w1")], axis=0)
    if want("w3all"):
        shared["w3all"] = np.concatenate([g("exp_w3")[0], g("sh_w3")], axis=0)
    if want("w2all"):
        e2 = g("exp_w2")[0]
        shared["w2all"] = np.concatenate([e2.reshape(-1, D), g("sh_w2")[0]], axis=0)
    shared["ident"] = np.eye(128, dtype=np.float32)
    pm = np.zeros((128, 128), np.float32)
    for m in range(128):
        pm[(m + 64) % 128, m] = 1.0
    shared["pm"] = pm
    bo = np.zeros((128, 128), np.float32)
    bo[:64, :64] = 1.0
    bo[64:, 64:] = 1.0
    shared["bo"] = bo
    shared["masks"] = chunk_masks()
    maps = []
    for c in range(cfg.get("NCORES", 8)):
        b, j = c // 2, c % 2
        m = dict(shared)
        if want("x_l"):
            xb, cb = g("x")[b], g("ctx")[b]
            if j == 1:
                xb, cb = xb[::-1], cb[::-1]
            m["x_l"] = np.ascontiguousarray(np.concatenate([xb, cb], axis=0))
        if want("cvec"):
            cv = np.stack([g("c")[b], g("c_ctx")], axis=-1)
            m["cvec"] = np.ascontiguousarray(cv.reshape(KC, 128, 2).transpose(1, 0, 2))
        if want("cosT") or want("sinT"):
            m["cosT"], m["sinT"] = rope_tables(cfg, j)
        dsel = [j, 1 - j]
        if want("shAB"):
            sh = g("rw_shift")[0]
            ab = np.zeros((NRT * 128, 2), np.float32)
            ab[:RW_COLS, 0] = sh[dsel[0]]
            ab[:RW_COLS, 1] = sh[dsel[1]]
            m["shAB"] = np.ascontiguousarray(ab.reshape(NRT, 128, 2).transpose(1, 0, 2))
        if want("chp"):
            cp = np.zeros((RW_W, NPAR), np.float32)
            cp[:, PK_K] = g("rw_k_k")[0]
            cp[:, PK_A] = g("rw_k_a")[0]
            cp[:, PR_K] = g("rw_r_k")[0].reshape(-1)
            cp[:, PLNW] = g("rw_ln_w")[0]
            cp[:, PLNB] = g("rw_ln_b")[0]
            for i in range(2):
                cp[:, PW0 + i] = g("rw_w0")[0][dsel[i]]
                cp[:, PA0 + i] = g("rw_a0")[0][dsel[i]]
            m["chp"] = np.ascontiguousarray(cp.reshape(16, 128, NPAR).transpose(1, 0, 2))
        if want("rw_w2d"):
            m["rw_w2d"] = np.ascontiguousarray(g("rw_w2")[0][dsel])
        if want("rw_a2d"):
            m["rw_a2d"] = np.ascontiguousarray(g("rw_a2")[0][dsel])
        if names is not None:
            m = {k: v for k, v in m.items() if k in names}
        maps.append(m)
    return maps


def assemble(results, cfg):
    S = cfg["S"]
    OWN = S // 2
    out = np.zeros((cfg["B"], S, D), np.float32)
    for c, r in enumerate(results):
        b, j = c // 2, c % 2
        o = r["out"]
        if j == 0:
            out[b, :OWN] = o
        else:
            out[b, S - 1 - np.arange(OWN)] = o
    return out


def kernel(**inputs):
    cfg = FULL_CFG
    nc, P = build_program(cfg)
    maps = host_inputs(inputs, cfg, names=set(P.in_names))
    res = run_bass_kernel_spmd(nc, maps, core_ids=list(range(8)))
    return assemble(res.results, cfg)
```
